# Optimizing a Trainium2 kernel written in Bass

```python
import jax, jax.numpy as jnp
from jax import lax
import numpy as np

D_MODEL = 2048
BATCH = 2
SEQ = 8192
DEPTH = 1

D_MIX = D_MODEL
LRU_WIDTH = D_MIX // 2
CONF_WIDTH = D_MIX - LRU_WIDTH
LRU_HEADS = 8
LRU_HEAD_DIM = LRU_WIDTH // LRU_HEADS
LRU_CONV_WIDTH = 4
LRU_C = 8.0
CONF_GROUPS = 8
CONF_KERNEL = 31
IN_COLS = 2 * LRU_WIDTH + 2 * CONF_WIDTH
PEER_HEADS = 8
PEER_N_KEYS = 128
PEER_N_EXPERTS = PEER_N_KEYS * PEER_N_KEYS
PEER_TOPK = 16
PEER_QUERY_DIM = 256
PEER_HALF = PEER_QUERY_DIM // 2
PEER_BLOCK = 128
N_MOD = 6
EPS = 1e-6

kernel_name = "hymba_rglru_conformer_peer_block"


def rms_norm(x, g):
    xf = x.astype(jnp.float32)
    y = xf * lax.rsqrt(jnp.mean(xf * xf, axis=-1, keepdims=True) + EPS)
    return (y * g.astype(jnp.float32)).astype(x.dtype)


def layer_norm(x, g, b):
    xf = x.astype(jnp.float32)
    mu = jnp.mean(xf, axis=-1, keepdims=True)
    var = jnp.mean(jnp.square(xf - mu), axis=-1, keepdims=True)
    y = (xf - mu) * lax.rsqrt(var + EPS)
    return (y * g.astype(jnp.float32) + b.astype(jnp.float32)).astype(x.dtype)


def modulate(h, shift, scale):
    return h * (1.0 + scale[:, None, :]) + shift[:, None, :]


def causal_depthwise_conv(x, w, b):
    k = w.shape[0]
    y = lax.conv_general_dilated(
        x, w[:, None, :].astype(x.dtype), window_strides=(1,), padding=[(k - 1, 0)],
        dimension_numbers=('NWC', 'WIO', 'NWC'), feature_group_count=x.shape[-1])
    return y + b


def _linear_recurrence_combine(e1, e2):
    a1, b1 = e1
    a2, b2 = e2
    return a1 * a2, a2 * b1 + b2


def rg_lru(x, wa, ba, wx, bx, lam):
    bsz, seq, width = x.shape
    xh = x.reshape(bsz, seq, LRU_HEADS, LRU_HEAD_DIM)
    r = jax.nn.sigmoid(jnp.einsum('bshi,hij->bshj', xh, wa).reshape(bsz, seq, width) + ba)
    i = jax.nn.sigmoid(jnp.einsum('bshi,hij->bshj', xh, wx).reshape(bsz, seq, width) + bx)
    log_a = -LRU_C * jax.nn.softplus(-lam.astype(jnp.float32)) * r.astype(jnp.float32)
    a = jnp.exp(log_a)
    mult = jnp.sqrt(-jnp.expm1(2.0 * log_a))
    u = mult * (i * x).astype(jnp.float32)
    _, h = lax.associative_scan(_linear_recurrence_combine, (a, u), axis=1)
    return h.astype(x.dtype)


def hybrid_mixer(h, w_in, lru_conv_w, lru_conv_b, lru_wa, lru_ba, lru_wx, lru_bx, lru_lambda,
                 conf_dw_w, conf_dw_b, conf_ln_g, conf_ln_b, w_out):
    proj = h @ w_in
    lru_x, lru_g, conf_v, conf_g = jnp.split(
        proj, [LRU_WIDTH, 2 * LRU_WIDTH, 2 * LRU_WIDTH + CONF_WIDTH], axis=-1)
    xl = causal_depthwise_conv(lru_x, lru_conv_w, lru_conv_b)
    y_lru = rg_lru(xl, lru_wa, lru_ba, lru_wx, lru_bx, lru_lambda) * jax.nn.gelu(lru_g)
    z = conf_v * jax.nn.sigmoid(conf_g)
    z = causal_depthwise_conv(z, conf_dw_w, conf_dw_b)
    z = jax.nn.silu(layer_norm(z, conf_ln_g, conf_ln_b))
    return jnp.concatenate([y_lru, z], axis=-1) @ w_out


def peer_ffn(h, wq, subkeys, u_table, v_table):
    bsz, seq, d = h.shape
    t = bsz * seq
    hf = h.reshape(t, d)
    q = (hf @ wq).reshape(t, PEER_HEADS, 2, PEER_HALF)
    scores = jnp.einsum('thpd,hpnd->thpn', q.astype(jnp.float32), subkeys.astype(jnp.float32))
    s1, i1 = lax.top_k(scores[:, :, 0], PEER_TOPK)
    s2, i2 = lax.top_k(scores[:, :, 1], PEER_TOPK)
    cand = (s1[..., :, None] + s2[..., None, :]).reshape(t, PEER_HEADS, PEER_TOPK * PEER_TOPK)
    cs, ci = lax.top_k(cand, PEER_TOPK)
    row = jnp.take_along_axis(i1, ci // PEER_TOPK, axis=-1)
    col = jnp.take_along_axis(i2, ci % PEER_TOPK, axis=-1)
    expert_idx = row * PEER_N_KEYS + col
    gates = jax.nn.softmax(cs, axis=-1).astype(h.dtype)
    n_blk = t // PEER_BLOCK

    def expert_block(args):
        hb, ib, gb = args
        u = jnp.take(u_table, ib, axis=0)
        act = jnp.einsum('cd,chkd->chk', hb, u)
        w = gb * jax.nn.gelu(act)
        v = jnp.take(v_table, ib, axis=0)
        return jnp.einsum('chk,chkd->cd', w, v)

    out = lax.map(expert_block, (hf.reshape(n_blk, PEER_BLOCK, d),
                                 expert_idx.reshape(n_blk, PEER_BLOCK, PEER_HEADS, PEER_TOPK),
                                 gates.reshape(n_blk, PEER_BLOCK, PEER_HEADS, PEER_TOPK)))
    return out.reshape(bsz, seq, d)


def setup_inputs(seed: int = 0) -> dict:
    key = jax.random.key(seed)
    ks = jax.random.split(key, 26)
    f32 = jnp.float32
    nrm = lambda k, shape, s: jax.random.normal(k, shape, f32) * s
    gain = lambda k, n: 1.0 + 0.02 * jax.random.normal(k, (DEPTH, n), f32)
    u_a = jax.random.uniform(ks[14], (DEPTH, LRU_WIDTH), f32, 0.9, 0.999)
    s_l = u_a ** (1.0 / LRU_C)
    lru_lambda = jnp.log(s_l) - jnp.log1p(-s_l)
    return {
        "x": jax.random.normal(ks[0], (BATCH, SEQ, D_MODEL), f32),
        "c": jax.random.normal(ks[1], (BATCH, D_MODEL), f32),
        "w_mod": nrm(ks[2], (DEPTH, D_MODEL, N_MOD * D_MODEL), 0.2 * D_MODEL ** -0.5),
        "b_mod": nrm(ks[3], (DEPTH, N_MOD * D_MODEL), 0.02),
        "g_pre_mix": gain(ks[4], D_MODEL),
        "g_post_mix": gain(ks[5], D_MODEL),
        "g_pre_ffn": gain(ks[6], D_MODEL),
        "g_post_ffn": gain(ks[7], D_MODEL),
        "w_in": nrm(ks[8], (DEPTH, D_MODEL, IN_COLS), D_MODEL ** -0.5),
        "lru_conv_w": nrm(ks[9], (DEPTH, LRU_CONV_WIDTH, LRU_WIDTH), LRU_CONV_WIDTH ** -0.5),
        "lru_conv_b": nrm(ks[10], (DEPTH, LRU_WIDTH), 0.02),
        "lru_wa": nrm(ks[11], (DEPTH, LRU_HEADS, LRU_HEAD_DIM, LRU_HEAD_DIM), LRU_HEAD_DIM ** -0.5),
        "lru_ba": nrm(ks[12], (DEPTH, LRU_WIDTH), 0.02),
        "lru_wx": nrm(ks[13], (DEPTH, LRU_HEADS, LRU_HEAD_DIM, LRU_HEAD_DIM), LRU_HEAD_DIM ** -0.5),
        "lru_bx": nrm(ks[15], (DEPTH, LRU_WIDTH), 0.02),
        "lru_lambda": lru_lambda,
        "conf_dw_w": nrm(ks[16], (DEPTH, CONF_KERNEL, CONF_WIDTH), CONF_KERNEL ** -0.5),
        "conf_dw_b": nrm(ks[17], (DEPTH, CONF_WIDTH), 0.02),
        "conf_ln_g": gain(ks[18], CONF_WIDTH),
        "conf_ln_b": nrm(ks[19], (DEPTH, CONF_WIDTH), 0.02),
        "w_out": nrm(ks[20], (DEPTH, D_MIX, D_MODEL), D_MIX ** -0.5),
        "peer_wq": nrm(ks[21], (DEPTH, D_MODEL, PEER_HEADS * PEER_QUERY_DIM), D_MODEL ** -0.5),
        "peer_subkeys": nrm(ks[22], (DEPTH, PEER_HEADS, 2, PEER_N_KEYS, PEER_HALF), PEER_HALF ** -0.5),
        "peer_u": nrm(ks[23], (DEPTH, PEER_N_EXPERTS, D_MODEL), D_MODEL ** -0.5),
        "peer_v": nrm(ks[24], (DEPTH, PEER_N_EXPERTS, D_MODEL), PEER_HEADS ** -0.5),
    }


def reference(x, c, w_mod, b_mod, g_pre_mix, g_post_mix, g_pre_ffn, g_post_ffn, w_in,
              lru_conv_w, lru_conv_b, lru_wa, lru_ba, lru_wx, lru_bx, lru_lambda,
              conf_dw_w, conf_dw_b, conf_ln_g, conf_ln_b, w_out,
              peer_wq, peer_subkeys, peer_u, peer_v):
    c_act = jax.nn.silu(c)
    for l in range(DEPTH):
        mod = c_act @ w_mod[l] + b_mod[l]
        sh1, sc1, gt1, sh2, sc2, gt2 = jnp.split(mod, N_MOD, axis=-1)
        h = modulate(rms_norm(x, g_pre_mix[l]), sh1, sc1)
        y = hybrid_mixer(h, w_in[l], lru_conv_w[l], lru_conv_b[l], lru_wa[l], lru_ba[l],
                         lru_wx[l], lru_bx[l], lru_lambda[l], conf_dw_w[l], conf_dw_b[l],
                         conf_ln_g[l], conf_ln_b[l], w_out[l])
        x = x + gt1[:, None, :] * rms_norm(y, g_post_mix[l])
        h = modulate(rms_norm(x, g_pre_ffn[l]), sh2, sc2)
        y = peer_ffn(h, peer_wq[l], peer_subkeys[l], peer_u[l], peer_v[l])
        x = x + gt2[:, None, :] * rms_norm(y, g_post_ffn[l])
    return x
```

```python
import numpy as np
from contextlib import ExitStack
import concourse.bass as bass
import concourse.mybir as mybir
from concourse.bass_utils import run_bass_kernel_spmd

F32 = mybir.dt.float32
BF16 = mybir.dt.bfloat16
U32 = mybir.dt.uint32
AF = mybir.ActivationFunctionType
ALU = mybir.AluOpType
AX = mybir.AxisListType

D = 2048
KC = 16
T = 2048
NPRE = 6144
TB = 1024
EPS = 1e-6
NEXP_CH = 128
RG = 4
HB = 1024
F_GROUPS = 0
F_MODE = ''

C_C = 0
C_LW = 16
C_LB = 48
C_BA = 56
C_BX = 64
C_LAM = 72
C_CW = 80
C_CB = 328
C_LG = 336
C_LNB = 344
C_FL = 352
NCOL = 358


class _Op:
    __slots__ = ("eng", "fn", "deps", "need_inc", "dma", "semkey", "token")


class Phase:
    ENG = ("sp", "pool", "act", "dve", "pe")

    gpool = None

    def __init__(self, nc, name):
        self.nc = nc
        self.name = name
        self.ops = []
        self.lw = {}
        self.rd = {}

    def op(self, eng, fn, reads=(), writes=(), dma=False, semkey=None):
        o = _Op()
        o.eng = eng
        o.fn = fn
        o.dma = dma
        o.need_inc = dma
        o.token = None
        o.semkey = semkey if semkey is not None else (("dma", writes[0]) if dma else None)
        deps = []
        for k in reads:
            w = self.lw.get(k)
            if w is not None:
                deps.append(w)
        for k in writes:
            w = self.lw.get(k)
            if w is not None:
                deps.append(w)
            deps.extend(self.rd.get(k, ()))
        o.deps = []
        seen = set()
        for d in deps:
            if id(d) not in seen and d is not o:
                seen.add(id(d))
                o.deps.append(d)
                d.need_inc = True
        for k in writes:
            self.lw[k] = o
            self.rd[k] = []
        for k in reads:
            self.rd.setdefault(k, []).append(o)
        self.ops.append(o)
        return o

    def emit(self):
        nc = self.nc
        gp = self.gpool
        local = {}
        for o in self.ops:
            if o.dma:
                k = o.semkey
                local[k] = local.get(k, 0) + 16
                o.token = (k, local[k])
            elif o.need_inc:
                k = ("eng", o.eng)
                local[k] = local.get(k, 0) + 1
                o.token = (k, local[k])
        swkeys = set(o.semkey for o in self.ops if o.dma and o.eng == "pool")
        slot = {}
        nhw = nsw = 0
        for k in local:
            if k[0] == "eng":
                slot[k] = self.ENG.index(k[1])
                continue
            lst = gp["sw"] if k in swkeys else gp["hw"]
            n_ = nsw if k in swkeys else nhw
            if n_ >= len(lst):
                lst.append(None)
            if k in swkeys:
                nsw += 1
            else:
                nhw += 1
            slot[k] = ("sw" if k in swkeys else "hw", n_)
        def getslot(sl):
            if isinstance(sl, int):
                while len(gp["sems"]) < 5:
                    i = len(gp["sems"])
                    gp["sems"].append(gp["stack"].enter_context(nc.semaphore(f"gsem{i}")))
                    gp["counts"].append(0)
                return sl
            kind, n_ = sl
            lst = gp[kind]
            if lst[n_] is None:
                i = len(gp["sems"])
                gp["sems"].append(gp["stack"].enter_context(nc.semaphore(f"gsem{i}")))
                gp["counts"].append(0)
                lst[n_] = i
            return lst[n_]
        getslot(0)
        slot = {k: getslot(v) for k, v in slot.items()}
        base = {k: gp["counts"][slot[k]] for k in local}
        sems = {k: gp["sems"][slot[k]] for k in local}
        by_eng = {e: [o for o in self.ops if o.eng == e] for e in self.ENG}
        with nc.Block() as block:
            reg = {"sp": block.sync, "pool": block.gpsimd, "act": block.scalar,
                   "dve": block.vector, "pe": block.tensor}
            for ename in self.ENG:
                ops = by_eng[ename]

                def body(e, ops=ops):
                    waited = {}
                    for o in ops:
                        need = {}
                        for d in o.deps:
                            if d.eng == "pe" and o.eng == "pe" and not d.dma and not o.dma:
                                continue
                            s, v = d.token
                            if need.get(s, 0) < v:
                                need[s] = v
                        for s, v in need.items():
                            if waited.get(s, 0) < v:
                                e.wait_ge(sems[s], base[s] + v)
                                waited[s] = v
                        ins = o.fn(e)
                        if o.dma:
                            ins.then_inc(sems[o.semkey], 16)
                        elif o.need_inc:
                            ins.then_inc(sems[("eng", o.eng)], 1)
                    for s, v in local.items():
                        if waited.get(s, 0) < v:
                            e.wait_ge(sems[s], base[s] + v)
                reg[ename](body)
        for k, v in local.items():
            gp["counts"][slot[k]] += v


def _col(cols, c):
    return cols[:, c:c + 1]


def build_program(stop_after=None):
    nc = bass.Bass("TRN2", target_bir_lowering=False)

    def din(name, shape, dt=F32):
        return nc.dram_tensor(name, list(shape), dt, kind="ExternalInput").ap()

    x_main = din("x_main", [T, D])
    x_pre = din("x_pre", [NPRE, D])
    cols_d = din("cols", [128, NCOL])
    ident_d = din("ident", [128, 128])
    iota_d = din("iota", [128, 128])
    w_mod = din("w_mod", [D, 6 * D])
    b_mod = din("b_mod", [6 * D])
    g_pre_mix = din("g_pre_mix", [D])
    g_post_mix = din("g_post_mix", [D])
    g_pre_ffn = din("g_pre_ffn", [D])
    g_post_ffn = din("g_post_ffn", [D])
    w_in = din("w_in", [D, 2 * D])
    lru_wa = din("lru_wa", [8, 128, 128])
    lru_wx = din("lru_wx", [8, 128, 128])
    w_out = din("w_out", [D, D])
    peer_wq = din("peer_wq", [D, D])
    peer_sk = din("peer_sk", [16, 128, 128])
    peer_u = din("peer_u", [16384, D])
    peer_v = din("peer_v", [16384, D])
    out = nc.dram_tensor("out", [T, D], F32, kind="ExternalOutput").ap()

    modbc_d = nc.dram_tensor("modbc_d", [128, 6 * D], F32).ap()
    catT_d = nc.dram_tensor("catT_d", [16, 128, T], BF16).ap()
    x1_d = nc.dram_tensor("x1_d", [T, D], F32).ap()
    h2T_d = nc.dram_tensor("h2T_d", [16, 128, T], BF16).ap()
    slots_d = nc.dram_tensor("slots_d", [3, 128, T], F32).ap()
    G_ds = [nc.dram_tensor(f"G_d{i}", [128, 4, T, 4], BF16).ap() for i in range(8)]

    dbg = None
    if stop_after is not None:
        dbg = nc.dram_tensor("dbg", [T, D], F32, kind="ExternalOutput").ap()

    with ExitStack() as outer:
        Phase.gpool = {"sems": [], "counts": [], "stack": outer, "hw": [], "sw": []}

        def sb(name, shape, dt=F32):
            return outer.enter_context(nc.sbuf_tensor("g_" + name, list(shape), dt))
        cols = sb("cols", [128, NCOL])
        ident = sb("ident", [128, 128])
        iota = sb("iota", [128, 128])
        ones = sb("ones", [128, 128])
        ident_bf = sb("ident_bf", [128, 128], BF16)
        iota_bf = sb("iota_bf", [128, 128], BF16)
        carry = sb("carry", [128, 8, 3])
        zcarry = sb("zcarry", [128, 8, 30])
        state = sb("state", [128, 8])
        clc = sb("clc", [128, 16])

        with ExitStack() as ph_es:
            ph = Phase(nc, "A")
            def sbp(name, shape, dt=F32, es=ph_es):
                return es.enter_context(nc.sbuf_tensor("a_" + name, list(shape), dt))
            def psp(name, shape, es=ph_es):
                return es.enter_context(nc.psum_tensor("a_" + name, list(shape), F32))
            caT = sbp("caT", [128, 16])
            caTb = sbp("caTb", [128, 16, 128])
            wm = [sbp(f"wm{i}", [128, 16, 512]) for i in range(2)]
            bmb = [sbp(f"bmb{i}", [128, 512]) for i in range(2)]
            gb = [sbp(f"gb{i}", [128, 512]) for i in range(2)]
            mo = [sbp(f"mo{i}", [128, 512]) for i in range(2)]
            tmpA = sbp("tmpA", [128, 16])
            mps = [psp(f"mps{i}", [128, 512]) for i in range(2)]

            ph.op("sp", lambda e: e.dma_start(out=cols[:], in_=cols_d), writes=["cols"], dma=True)
            ph.op("sp", lambda e: e.dma_start(out=ident[:], in_=ident_d), writes=["ident"], dma=True)
            ph.op("sp", lambda e: e.dma_start(out=iota[:], in_=iota_d), writes=["iota"], dma=True)
            ph.op("pool", lambda e: e.memset(ones[:], 1.0), writes=["ones"])
            ph.op("dve", lambda e: e.tensor_copy(out=ident_bf[:], in_=ident[:]), reads=["ident"], writes=["ident_bf"])
            ph.op("dve", lambda e: e.tensor_copy(out=iota_bf[:], in_=iota[:]), reads=["iota"], writes=["iota_bf"])
            ph.op("pool", lambda e: e.memset(carry[:], 0.0), writes=["carry"])
            ph.op("pool", lambda e: e.memset(zcarry[:], 0.0), writes=["zcarry"])
            ph.op("pool", lambda e: e.memset(state[:], 0.0), writes=["state"])
            ph.op("act", lambda e: e.activation(out=caT[:], in_=cols[:, C_C:C_C + 16], func=AF.Silu),
                  reads=["cols"], writes=["caT"])
            ph.op("act", lambda e: e.activation(out=tmpA[:, 0:8], in_=cols[:, C_LAM:C_LAM + 8], func=AF.Exp, scale=-1.0),
                  reads=["cols"], writes=["tmpA"])
            ph.op("act", lambda e: e.activation(out=tmpA[:, 8:16], in_=tmpA[:, 0:8], func=AF.Ln, bias=1.0),
                  reads=["tmpA"], writes=["tmpA2"])
            ph.op("dve", lambda e: e.tensor_scalar(out=clc[:, 0:8], in0=tmpA[:, 8:16], scalar1=-8.0, scalar2=None, op0=ALU.mult),
                  reads=["tmpA2"], writes=["clc0"])
            ph.op("dve", lambda e: e.tensor_scalar(out=clc[:, 8:16], in0=tmpA[:, 8:16], scalar1=-16.0, scalar2=None, op0=ALU.mult),
                  reads=["tmpA2"], writes=["clc1"])
            for k in range(16):
                ph.op("dve", lambda e, k=k: e.tensor_copy(out=caTb[:, k, :], in_=caT[:, k:k + 1].to_broadcast([128, 128])),
                      reads=["caT"], writes=[("caTb", k)])
            gsrc = {1: g_pre_mix, 2: g_post_mix, 4: g_pre_ffn, 5: g_post_ffn}
            for n in range(24):
                s = n % 2
                sec = n // 4
                cb = (n % 4) * 512
                ph.op("sp", lambda e, n=n, s=s: e.dma_start(
                    out=wm[s][:], in_=w_mod.rearrange("(k p) c -> p k c", p=128)[:, :, n * 512:(n + 1) * 512]),
                    writes=[("wm", s)], dma=True)
                ph.op("sp", lambda e, n=n, s=s: e.dma_start(
                    out=bmb[s][:], in_=b_mod[n * 512:(n + 1) * 512].partition_broadcast(128)),
                    writes=[("bmb", s)], dma=True)
                if sec in gsrc:
                    ph.op("sp", lambda e, s=s, sec=sec, cb=cb: e.dma_start(
                        out=gb[s][:], in_=gsrc[sec][cb:cb + 512].partition_broadcast(128)),
                        writes=[("gb", s)], dma=True)

                def mm(e, s=s):
                    last = None
                    for k in range(16):
                        last = e.matmul(mps[s][:], lhsT=caTb[:, k, :], rhs=wm[s][:, k, :], start=(k == 0), stop=(k == 15))
                    return last
                ph.op("pe", mm, reads=[("wm", s)] + [("caTb", k) for k in range(16)], writes=[("mps", s)])
                if sec in (0, 3):
                    ph.op("dve", lambda e, s=s: e.tensor_tensor(out=mo[s][:], in0=mps[s][:], in1=bmb[s][:], op=ALU.add),
                          reads=[("mps", s), ("bmb", s)], writes=[("mo", s)])
                else:
                    ph.op("dve", lambda e, s=s: e.tensor_tensor(out=bmb[s][:], in0=mps[s][:], in1=bmb[s][:], op=ALU.add),
                          reads=[("mps", s), ("bmb", s)], writes=[("bmb", s)])
                    if sec in (1, 4):
                        ph.op("dve", lambda e, s=s: e.scalar_tensor_tensor(out=mo[s][:], in0=bmb[s][:], scalar=1.0, in1=gb[s][:],
                                                                          op0=ALU.add, op1=ALU.mult),
                              reads=[("bmb", s), ("gb", s)], writes=[("mo", s)])
                    else:
                        ph.op("dve", lambda e, s=s: e.tensor_tensor(out=mo[s][:], in0=bmb[s][:], in1=gb[s][:], op=ALU.mult),
                              reads=[("bmb", s), ("gb", s)], writes=[("mo", s)])
                ph.op("sp", lambda e, n=n, s=s: e.dma_start(out=modbc_d[:, n * 512:(n + 1) * 512], in_=mo[s][:]),
                      reads=[("mo", s)], writes=[("modbc_d", n)], dma=True, semkey=("st", "mo", s))
            ph.emit()

        if stop_after == "A":
            _dbg_copy(nc, dbg, modbc_d[:, 0:D], rows=128)
            return nc

        def mixer_phase(name, xsrc, nblocks, prefix):
            with ExitStack() as ph_es:
                ph = Phase(nc, name)
                def sbp(nm, shape, dt=F32):
                    return ph_es.enter_context(nc.sbuf_tensor(name + "_" + nm, list(shape), dt))
                def psp(nm, shape):
                    return ph_es.enter_context(nc.psum_tensor(name + "_" + nm, list(shape), F32))
                N = TB
                A1bc = sbp("A1bc", [128, D])
                sh1bc = sbp("sh1bc", [128, D])
                xt = [sbp(f"xt{i}", [128, D]) for i in range(2)]
                xn = [sbp(f"xn{i}", [128, D]) for i in range(2)]
                junk = sbp("junk", [128, D], BF16)
                ss = sbp("ss", [128, 4])
                rs = sbp("rs", [128, 4])
                hT = sbp("hT", [128, 16, N], BF16)
                wsl = [sbp(f"wsl{i}", [128, 16, 128], BF16) for i in range(4)]
                wga = sbp("wga", [128, 8, 128])
                wgx = sbp("wgx", [128, 8, 128])
                xpad = sbp("xpad", [128, N + 3])
                cA = sbp("cA", [128, N])
                cB = sbp("cB", [128, N])
                xl = sbp("xl", [128, N])
                rr = sbp("rr", [128, N])
                ig = sbp("ig", [128, N])
                aa = sbp("aa", [128, N])
                a2 = sbp("a2", [128, N])
                uu = sbp("uu", [128, N])
                hs = sbp("hs", [128, N])
                tp = [psp(f"tp{i}", [128, 512]) for i in range(2)]
                pp = psp("pp", [128, N])
                gpa = psp("gpa", [128, N])
                gpx = psp("gpx", [128, N])
                if not prefix:
                    zpad = sbp("zpad", [128, N + 30])
                    cz = sbp("cz", [128, 8, N])
                    gel = sbp("gel", [128, N])
                    catc = [sbp(f"catc{i}", [128, N], BF16) for i in range(2)]
                    mean = aa
                    rstd = a2
                    sqt = [rr, ig]
                zs = sbp("zs", [128, 32])
                zsg = sbp("zsg", [128, 32])

                ph.op("sp", lambda e: e.dma_start(out=A1bc[:], in_=modbc_d[:, D:2 * D]), writes=["A1bc"], dma=True)
                ph.op("sp", lambda e: e.dma_start(out=sh1bc[:], in_=modbc_d[:, 0:D]), writes=["sh1bc"], dma=True)
                ph.op("sp", lambda e: e.dma_start(out=wga[:], in_=lru_wa.rearrange("h i j -> i h j")), writes=["wga"], dma=True)
                ph.op("sp", lambda e: e.dma_start(out=wgx[:], in_=lru_wx.rearrange("h i j -> i h j")), writes=["wgx"], dma=True)

                plan = []
                for b_ in range(nblocks):
                    for i_ in range(8):
                        plan.append(i_ * 128)
                        if not prefix:
                            plan.append(1024 + i_ * 128)
                    if (prefix and b_ == nblocks - 1) or not prefix:
                        for i_ in range(8):
                            plan.append(2048 + i_ * 128)
                            plan.append(3072 + i_ * 128)
                wctr = [0, 0]

                def issue_loads():
                    while wctr[0] < len(plan) and wctr[0] < wctr[1] + 3:
                        n_ = wctr[0]
                        s_ = n_ % 4
                        c0 = plan[n_]
                        wctr[0] += 1
                        ph.op("pool", lambda e, s_=s_, c0=c0: e.dma_start(
                            out=wsl[s_][:], in_=w_in.rearrange("(k p) c -> p k c", p=128)[:, :, c0:c0 + 128]),
                            writes=[("wsl", s_)], dma=True)

                def load_slice(c0):
                    issue_loads()
                    n_ = wctr[1]
                    assert plan[n_] == c0, (n_, plan[n_], c0)
                    wctr[1] += 1
                    return n_ % 4

                def proj(dst, dname, s, t0, n):
                    for g0 in range(0, n, 512):
                        gn = min(512, n - g0)

                        def mm(e, g0=g0, gn=gn):
                            last = None
                            for k in range(16):
                                last = e.matmul(dst[:, g0:g0 + gn], lhsT=wsl[s][:, k, :], rhs=hT[:, k, t0 + g0:t0 + g0 + gn],
                                                start=(k == 0), stop=(k == 15))
                            return last
                        ph.op("pe", mm, reads=[("wsl", s), "hT"], writes=[(dname, g0)])
                    issue_loads()

                tctr = [0]
                for b in range(nblocks):
                    for t in range(N // 128):
                        s = tctr[0] % 2
                        tctr[0] += 1
                        r0 = b * N + t * 128
                        ph.op("sp", lambda e, s=s, r0=r0: e.dma_start(out=xt[s][:], in_=xsrc[r0:r0 + 128, :]),
                              writes=[("xt", s)], dma=True)
                        ph.op("act", lambda e, s=s: e.activation(out=junk[:], in_=xt[s][:], func=AF.Square, accum_out=ss[:, s:s + 1]),
                              reads=[("xt", s)], writes=["junk", ("ss", s)])
                        ph.op("act", lambda e, s=s: e.activation(out=rs[:, s:s + 1], in_=ss[:, s:s + 1], func=AF.Sqrt, scale=1.0 / D, bias=EPS),
                              reads=[("ss", s)], writes=[("rs", s)])
                        ph.op("dve", lambda e, s=s: e.reciprocal(out=rs[:, 2 + s:3 + s], in_=rs[:, s:s + 1]),
                              reads=[("rs", s)], writes=[("rs2", s)])
                        ph.op("dve", lambda e, s=s: e.scalar_tensor_tensor(out=xn[s][:], in0=xt[s][:], scalar=rs[:, 2 + s:3 + s], in1=A1bc[:],
                                                                          op0=ALU.mult, op1=ALU.mult),
                              reads=[("xt", s), ("rs2", s), "A1bc"], writes=[("xn", s)])
                        ph.op("pool", lambda e, s=s: e.tensor_tensor(out=xn[s][:], in0=xn[s][:], in1=sh1bc[:], op=ALU.add),
                              reads=[("xn", s), "sh1bc"], writes=[("xn", s)])
                        for kg in range(4):
                            bk = kg % 2

                            def trs(e, s=s, kg=kg, bk=bk):
                                last = None
                                for kk in range(4):
                                    k = 4 * kg + kk
                                    last = e.transpose(out=tp[bk][:, kk * 128:(kk + 1) * 128], in_=xn[s][:, k * 128:(k + 1) * 128],
                                                       identity=ident[:])
                                return last
                            ph.op("pe", trs, reads=[("xn", s), "ident"], writes=[("tp", bk)])
                            eng = "act" if kg % 2 == 0 else "dve"
                            if eng == "act":
                                ph.op("act", lambda e, kg=kg, bk=bk, t=t: e.activation(
                                    out=hT[:, 4 * kg:4 * kg + 4, t * 128:(t + 1) * 128],
                                    in_=tp[bk][:].rearrange("p (a c) -> p a c", a=4), func=AF.Copy),
                                    reads=[("tp", bk)], writes=["hT"])
                            else:
                                ph.op("dve", lambda e, kg=kg, bk=bk, t=t: e.tensor_copy(
                                    out=hT[:, 4 * kg:4 * kg + 4, t * 128:(t + 1) * 128],
                                    in_=tp[bk][:].rearrange("p (a c) -> p a c", a=4)),
                                    reads=[("tp", bk)], writes=["hT"])
                    fl = _col(cols, C_FL + b) if prefix else None
                    for i in range(8):
                        sx = load_slice(i * 128)
                        if not prefix:
                            sg_ = load_slice(1024 + i * 128)
                        proj(pp, "pp", sx, 0, N)
                        ph.op("act", lambda e: e.activation(out=xpad[:, 3:3 + N], in_=pp[:], func=AF.Copy),
                              reads=[("pp", 0), ("pp", 512)], writes=["xpad"])
                        ph.op("pool", lambda e, i=i: e.tensor_copy(out=xpad[:, 0:3], in_=carry[:, i, :]),
                              reads=[("carry", i), "carry"], writes=["xpadc"])
                        wcol = lambda j, i=i: _col(cols, C_LW + j * 8 + i)
                        ph.op("dve", lambda e, i=i: e.tensor_scalar(out=cA[:], in0=xpad[:, 0:N], scalar1=_col(cols, C_LW + i),
                                                                   scalar2=_col(cols, C_LB + i), op0=ALU.mult, op1=ALU.add),
                              reads=["xpad", "xpadc", "cols"], writes=["cA"])
                        ph.op("dve", lambda e, i=i: e.scalar_tensor_tensor(out=cB[:], in0=xpad[:, 1:1 + N], scalar=_col(cols, C_LW + 8 + i),
                                                                          in1=cA[:], op0=ALU.mult, op1=ALU.add),
                              reads=["xpad", "xpadc", "cA"], writes=["cB"])
                        ph.op("dve", lambda e, i=i: e.scalar_tensor_tensor(out=cA[:], in0=xpad[:, 2:2 + N], scalar=_col(cols, C_LW + 16 + i),
                                                                          in1=cB[:], op0=ALU.mult, op1=ALU.add),
                              reads=["xpad", "xpadc", "cB"], writes=["cA"])
                        ph.op("dve", lambda e, i=i: e.scalar_tensor_tensor(out=xl[:], in0=xpad[:, 3:3 + N], scalar=_col(cols, C_LW + 24 + i),
                                                                          in1=cA[:], op0=ALU.mult, op1=ALU.add),
                              reads=["xpad", "cA"], writes=["xl"])
                        if prefix:
                            ph.op("pool", lambda e, i=i, fl=fl: e.tensor_scalar(out=carry[:, i, :], in0=xpad[:, N:N + 3], scalar1=fl,
                                                                               scalar2=None, op0=ALU.mult),
                                  reads=["xpad"], writes=[("carry", i)])
                        else:
                            ph.op("pool", lambda e, i=i: e.tensor_copy(out=carry[:, i, :], in_=xpad[:, N:N + 3]),
                                  reads=["xpad"], writes=[("carry", i)])
                        for (wg, gp, nm) in ((wga, gpa, "gpa"), (wgx, gpx, "gpx")):
                            for g0 in (0, 512):
                                ph.op("pe", lambda e, wg=wg, gp=gp, g0=g0, i=i: e.matmul(gp[:, g0:g0 + 512], lhsT=wg[:, i, :], rhs=xl[:, g0:g0 + 512],
                                                                                    start=True, stop=True),
                                      reads=["xl", "wga", "wgx"], writes=[(nm, g0)])
                        ph.op("act", lambda e, i=i: e.activation(out=rr[:], in_=gpa[:], func=AF.Sigmoid, bias=_col(cols, C_BA + i)),
                              reads=[("gpa", 0), ("gpa", 512)], writes=["rr"])
                        ph.op("act", lambda e, i=i: e.activation(out=ig[:], in_=gpx[:], func=AF.Sigmoid, bias=_col(cols, C_BX + i)),
                              reads=[("gpx", 0), ("gpx", 512)], writes=["ig"])
                        ph.op("act", lambda e, i=i: e.activation(out=aa[:], in_=rr[:], func=AF.Exp, scale=clc[:, i:i + 1]),
                              reads=["rr"], writes=["aa"])
                        ph.op("act", lambda e, i=i: e.activation(out=a2[:], in_=rr[:], func=AF.Exp, scale=clc[:, 8 + i:9 + i]),
                              reads=["rr"], writes=["a2"])
                        ph.op("act", lambda e: e.activation(out=a2[:], in_=a2[:], func=AF.Sqrt, scale=-1.0, bias=1.0),
                              reads=["a2"], writes=["a2"])
                        ph.op("pool", lambda e: e.tensor_tensor(out=uu[:], in0=ig[:], in1=xl[:], op=ALU.mult),
                              reads=["ig", "xl"], writes=["uu"])
                        ph.op("pool", lambda e: e.tensor_tensor(out=uu[:], in0=uu[:], in1=a2[:], op=ALU.mult),
                              reads=["uu", "a2"], writes=["uu"])
                        ph.op("dve", lambda e, i=i: e.tensor_tensor_scan(out=hs[:], data0=aa[:], data1=uu[:], initial=state[:, i:i + 1],
                                                                        op0=ALU.mult, op1=ALU.add),
                              reads=["aa", "uu", ("state", i), "state"], writes=["hs"])
                        if prefix:
                            ph.op("pool", lambda e, i=i, fl=fl: e.tensor_scalar(out=state[:, i:i + 1], in0=hs[:, N - 1:N], scalar1=fl,
                                                                               scalar2=None, op0=ALU.mult),
                                  reads=["hs"], writes=[("state", i)])
                        else:
                            ph.op("pool", lambda e, i=i: e.tensor_copy(out=state[:, i:i + 1], in_=hs[:, N - 1:N]),
                                  reads=["hs"], writes=[("state", i)])
                            proj(gpa, "gpa", sg_, 0, N)
                            ph.op("act", lambda e: e.activation(out=gel[:], in_=gpa[:], func=AF.Gelu_apprx_tanh),
                                  reads=[("gpa", 0), ("gpa", 512)], writes=["gel"])
                            cs_ = i % 2
                            ph.op("dve", lambda e, cs_=cs_: e.tensor_tensor(out=catc[cs_][:], in0=hs[:], in1=gel[:], op=ALU.mult),
                                  reads=["hs", "gel"], writes=[("catc", cs_)])
                            ph.op("sp", lambda e, cs_=cs_, i=i, b=b: e.dma_start(out=catT_d[i, :, b * N:(b + 1) * N], in_=catc[cs_][:]),
                                  reads=[("catc", cs_)], writes=[("catT_d", i, b)], dma=True, semkey=("st", "catc", cs_))
                    if prefix and b == nblocks - 1:
                        for i in range(8):
                            sv = load_slice(2048 + i * 128)
                            sg2 = load_slice(3072 + i * 128)
                            proj(pp, "pp", sv, N - 32, 32)
                            proj(gpa, "gpa", sg2, N - 32, 32)
                            ph.op("act", lambda e: e.activation(out=zsg[:], in_=gpa[:, 0:32], func=AF.Sigmoid),
                                  reads=[("gpa", 0)], writes=["zsg"])
                            ph.op("dve", lambda e: e.tensor_tensor(out=zs[:], in0=pp[:, 0:32], in1=zsg[:], op=ALU.mult),
                                  reads=[("pp", 0), "zsg"], writes=["zs"])
                            ph.op("pool", lambda e, i=i, fl=fl: e.tensor_scalar(out=zcarry[:, i, :], in0=zs[:, 2:32], scalar1=fl,
                                                                               scalar2=None, op0=ALU.mult),
                                  reads=["zs"], writes=[("zcarry", i)])
                    if not prefix:
                        for i in range(8):
                            sv = load_slice(2048 + i * 128)
                            sg2 = load_slice(3072 + i * 128)
                            proj(pp, "pp", sv, 0, N)
                            proj(gpx, "gpx", sg2, 0, N)
                            ph.op("act", lambda e: e.activation(out=gel[:], in_=gpx[:], func=AF.Sigmoid),
                                  reads=[("gpx", 0), ("gpx", 512)], writes=["gel"])
                            ph.op("dve", lambda e: e.tensor_tensor(out=zpad[:, 30:30 + N], in0=pp[:], in1=gel[:], op=ALU.mult),
                                  reads=[("pp", 0), ("pp", 512), "gel"], writes=["zpad"])
                            ph.op("pool", lambda e, i=i: e.tensor_copy(out=zpad[:, 0:30], in_=zcarry[:, i, :]),
                                  reads=[("zcarry", i), "zcarry"], writes=["zpadc"])
                            ph.op("dve", lambda e, i=i: e.tensor_scalar(out=cA[:], in0=zpad[:, 0:N], scalar1=_col(cols, C_CW + i),
                                                                       scalar2=_col(cols, C_CB + i), op0=ALU.mult, op1=ALU.add),
                                  reads=["zpad", "zpadc"], writes=["cA"])
                            bufs = [(cA, "cA"), (cB, "cB")]
                            for j in range(1, 31):
                                src, sn = bufs[(j - 1) % 2]
                                if j == 30:
                                    o_ap, dn = cz[:, i, :], ("cz", i)
                                else:
                                    o_ap, dn = bufs[j % 2][0][:], bufs[j % 2][1]
                                ph.op("dve", lambda e, i=i, j=j, src=src, o_ap=o_ap: e.scalar_tensor_tensor(
                                    out=o_ap, in0=zpad[:, j:j + N], scalar=_col(cols, C_CW + j * 8 + i), in1=src[:],
                                    op0=ALU.mult, op1=ALU.add),
                                    reads=["zpad", "zpadc", sn], writes=[dn])
                            ph.op("pool", lambda e, i=i: e.tensor_copy(out=zcarry[:, i, :], in_=zpad[:, N:N + 30]),
                                  reads=["zpad"], writes=[("zcarry", i)])
                            q_ = i % 2
                            ph.op("act", lambda e, i=i, q_=q_: e.activation(out=sqt[q_][:], in_=cz[:, i, :], func=AF.Square),
                                  reads=[("cz", i)], writes=[("rr", "ig")[q_]])
                            for g0 in (0, 512):
                                ph.op("pe", lambda e, i=i, g0=g0: e.matmul(gpa[:, g0:g0 + 512], lhsT=ones[:], rhs=cz[:, i, g0:g0 + 512],
                                                                          start=(i == 0), stop=(i == 7)),
                                      reads=[("cz", i), "ones"], writes=[("gpa", g0)])
                            for g0 in (0, 512):
                                ph.op("pe", lambda e, i=i, g0=g0, q_=q_: e.matmul(tp[g0 // 512][:], lhsT=ones[:], rhs=sqt[q_][:, g0:g0 + 512],
                                                                                 start=(i == 0), stop=(i == 7)),
                                      reads=[("rr", "ig")[q_], "ones"], writes=[("tp", g0 // 512)])
                        ph.op("act", lambda e: e.activation(out=mean[:], in_=gpa[:], func=AF.Copy, scale=1.0 / 1024.0),
                              reads=[("gpa", 0), ("gpa", 512)], writes=["aa"])
                        for g in range(2):
                            ph.op("act", lambda e, g=g: e.activation(out=rstd[:, g * 512:(g + 1) * 512], in_=tp[g][:], func=AF.Copy, scale=1.0 / 1024.0),
                                  reads=[("tp", g)], writes=["a2"])
                        ph.op("dve", lambda e: e.tensor_tensor(out=cA[:], in0=mean[:], in1=mean[:], op=ALU.mult),
                              reads=["aa"], writes=["cA"])
                        ph.op("dve", lambda e: e.tensor_tensor(out=rstd[:], in0=rstd[:], in1=cA[:], op=ALU.subtract),
                              reads=["a2", "cA"], writes=["a2"])
                        ph.op("act", lambda e: e.activation(out=rstd[:], in_=rstd[:], func=AF.Sqrt, bias=EPS),
                              reads=["a2"], writes=["a2"])
                        ph.op("dve", lambda e: e.reciprocal(out=rstd[:], in_=rstd[:]), reads=["a2"], writes=["a2"])
                        for i in range(8):
                            ph.op("pool", lambda e, i=i: e.tensor_tensor(out=cA[:], in0=cz[:, i, :], in1=mean[:], op=ALU.subtract),
                                  reads=[("cz", i), "aa"], writes=["cA"])
                            ph.op("dve", lambda e: e.tensor_tensor(out=cB[:], in0=cA[:], in1=rstd[:], op=ALU.mult),
                                  reads=["cA", "a2"], writes=["cB"])
                            cs_ = i % 2
                            ph.op("act", lambda e, i=i, cs_=cs_: e.activation(out=catc[cs_][:], in_=cB[:], func=AF.Silu,
                                                                             scale=_col(cols, C_LG + i), bias=_col(cols, C_LNB + i)),
                                  reads=["cB"], writes=[("catc", cs_)])
                            ph.op("sp", lambda e, cs_=cs_, i=i, b=b: e.dma_start(out=catT_d[8 + i, :, b * N:(b + 1) * N], in_=catc[cs_][:]),
                                  reads=[("catc", cs_)], writes=[("catT_d", 8 + i, b)], dma=True, semkey=("st", "catc", cs_))
                ph.emit()

        mixer_phase("P", x_pre, NPRE // TB, True)
        mixer_phase("M", x_main, T // TB, False)
        if stop_after == "M":
            return nc

        with ExitStack() as ph_es:
            ph = Phase(nc, "C")
            def sbp(nm, shape, dt=F32):
                return ph_es.enter_context(nc.sbuf_tensor("c_" + nm, list(shape), dt))
            def psp(nm, shape):
                return ph_es.enter_context(nc.psum_tensor("c_" + nm, list(shape), F32))
            wo = sbp("wo", [128, 16, D], BF16)
            gg = sbp("gg", [128, D])
            ct = [sbp(f"ct{i}", [128, 16, 128], BF16) for i in range(2)]
            xt = [sbp(f"cxt{i}", [128, D]) for i in range(2)]
            yo = [sbp(f"yo{i}", [128, D]) for i in range(2)]
            junk = sbp("cjunk", [128, D], BF16)
            ss = sbp("css", [128, 4])
            rs = sbp("crs", [128, 4])
            yp = [psp(f"yp{i}", [128, D]) for i in range(2)]
            for k in range(16):
                ph.op("pool", lambda e, k=k: e.dma_start(out=wo[:, k, :], in_=w_out[k * 128:(k + 1) * 128, :]),
                      writes=[("wo", k)], dma=True)
            ph.op("sp", lambda e: e.dma_start(out=gg[:], in_=modbc_d[:, 2 * D:3 * D]), writes=["gg"], dma=True)
            for t in range(T // 128):
                s = t % 2
                ph.op("sp", lambda e, s=s, t=t: e.dma_start(out=ct[s][:], in_=catT_d[:, :, t * 128:(t + 1) * 128].rearrange("k p n -> p k n")),
                      writes=[("ct", s)], dma=True)
                ph.op("sp", lambda e, s=s, t=t: e.dma_start(out=xt[s][:], in_=x_main[t * 128:(t + 1) * 128, :]),
                      writes=[("xt", s)], dma=True)

                def mm(e, s=s):
                    last = None
                    for g in range(4):
                        for k in range(16):
                            last = e.matmul(yp[s][:, g * 512:(g + 1) * 512], lhsT=ct[s][:, k, :], rhs=wo[:, k, g * 512:(g + 1) * 512],
                                            start=(k == 0), stop=(k == 15))
                    return last
                ph.op("pe", mm, reads=[("ct", s)] + [("wo", k) for k in range(16)], writes=[("yp", s)])
                ph.op("act", lambda e, s=s: e.activation(out=junk[:], in_=yp[s][:], func=AF.Square, accum_out=ss[:, s:s + 1]),
                      reads=[("yp", s)], writes=["junk", ("ss", s)])
                ph.op("act", lambda e, s=s: e.activation(out=rs[:, s:s + 1], in_=ss[:, s:s + 1], func=AF.Sqrt, scale=1.0 / D, bias=EPS),
                      reads=[("ss", s)], writes=[("rs", s)])
                ph.op("dve", lambda e, s=s: e.reciprocal(out=rs[:, 2 + s:3 + s], in_=rs[:, s:s + 1]),
                      reads=[("rs", s)], writes=[("rs2", s)])
                ph.op("dve", lambda e, s=s: e.scalar_tensor_tensor(out=yo[s][:], in0=yp[s][:], scalar=rs[:, 2 + s:3 + s], in1=gg[:],
                                                                  op0=ALU.mult, op1=ALU.mult),
                      reads=[("yp", s), ("rs2", s), "gg"], writes=[("yo", s)])
                ph.op("pool", lambda e, s=s: e.tensor_tensor(out=yo[s][:], in0=yo[s][:], in1=xt[s][:], op=ALU.add),
                      reads=[("yo", s), ("xt", s)], writes=[("yo", s)])
                ph.op("sp", lambda e, s=s, t=t: e.dma_start(out=x1_d[t * 128:(t + 1) * 128, :], in_=yo[s][:]),
                      reads=[("yo", s)], writes=[("x1_d", t)], dma=True, semkey=("st", "yo", s))
            ph.emit()
        if stop_after == "C":
            _dbg_copy(nc, dbg, x1_d, rows=T)
            return nc

        with ExitStack() as ph_es:
            ph = Phase(nc, "D")
            def sbp(nm, shape, dt=F32):
                return ph_es.enter_context(nc.sbuf_tensor("d_" + nm, list(shape), dt))
            def psp(nm, shape, dt=F32):
                return ph_es.enter_context(nc.psum_tensor("d_" + nm, list(shape), dt))
            A2bc = sbp("A2bc", [128, D])
            sh2bc = sbp("sh2bc", [128, D])
            xt = [sbp(f"xt{i}", [128, D]) for i in range(2)]
            xn = [sbp(f"xn{i}", [128, D]) for i in range(2)]
            junk = sbp("junk", [128, D], BF16)
            ss = sbp("ss", [128, 4])
            rs = sbp("rs", [128, 4])
            h2T = sbp("h2T", [128, 16, 512], BF16)
            wqs = [sbp(f"wqs{i}", [128, 16, 128], BF16) for i in range(4)]
            qT = sbp("qT", [128, 16, 512])
            skT = sbp("skT", [128, 16, 128])
            S = sbp("S", [128, 16, 128])
            S2 = sbp("S2", [128, 16, 128])
            vals = sbp("vals", [128, 16, 16])
            idx = sbp("idx", [128, 16, 16], U32)
            idxf = sbp("idxf", [128, 16, 16])
            cand = sbp("cand", [128, 8, 256])
            cand2 = sbp("cand2", [128, 8, 256])
            cval = sbp("cval", [128, 8, 16])
            cidx = sbp("cidx", [128, 8, 16], U32)
            cif = sbp("cif", [128, 8, 16])
            ex = sbp("ex", [128, 8, 16])
            esum = sbp("esum", [128, 8])
            e1 = sbp("e1", [128, 8, 16, 16])
            e2 = sbp("e2", [128, 8, 16, 16])
            i_f = sbp("i_f", [128, 8, 16])
            jjf = sbp("jjf", [128, 8, 16])
            slot = sbp("slot", [128, 3, 128])
            slT = [sbp(f"slT{i}", [128, 3, 128]) for i in range(2)]
            k16 = sbp("k16", [128, 3, 16])
            tp = [psp(f"tp{i}", [128, 512]) for i in range(2)]
            qp = [psp(f"qp{i}", [128, 512]) for i in range(2)]
            scp = psp("scp", [128, 2048])

            ph.op("sp", lambda e: e.dma_start(out=A2bc[:], in_=modbc_d[:, 4 * D:5 * D]), writes=["A2bc"], dma=True)
            ph.op("sp", lambda e: e.dma_start(out=sh2bc[:], in_=modbc_d[:, 3 * D:4 * D]), writes=["sh2bc"], dma=True)
            ph.op("sp", lambda e: e.dma_start(out=S[:], in_=peer_sk.rearrange("j n d -> n j d")), writes=["S"], dma=True)
            for jg in range(4):
                def trs(e, jg=jg):
                    last = None
                    for jj in range(4):
                        last = e.transpose(out=tp[jg % 2][:, jj * 128:(jj + 1) * 128], in_=S[:, 4 * jg + jj, :], identity=ident[:])
                    return last
                ph.op("pe", trs, reads=["S"], writes=[("tp", jg % 2)])
                ph.op("act", lambda e, jg=jg: e.activation(out=skT[:, 4 * jg:4 * jg + 4, :],
                                                          in_=tp[jg % 2][:].rearrange("p (a c) -> p a c", a=4), func=AF.Copy),
                      reads=[("tp", jg % 2)], writes=["skT"])
            ph.op("dve", lambda e: e.tensor_copy(out=k16[:, 0, :], in_=iota[:, 0:16]), writes=["k16a"])
            ph.op("dve", lambda e: e.tensor_scalar(out=k16[:, 1, :], in0=iota[:, 0:16], scalar1=16.0, scalar2=None, op0=ALU.mult),
                  writes=["k16b"])
            ph.op("dve", lambda e: e.tensor_scalar(out=k16[:, 2, :], in0=iota[:, 0:16], scalar1=16.0, scalar2=16.0, op0=ALU.mult, op1=ALU.add),
                  writes=["k16c"])

            wplan = [j for g in range(4) for j in range(16)]
            wctr = [0, 0]

            def issue_wq():
                while wctr[0] < len(wplan) and wctr[0] < wctr[1] + 3:
                    n_ = wctr[0]
                    wctr[0] += 1
                    ph.op("pool", lambda e, s_=n_ % 4, j=wplan[n_]: e.dma_start(
                        out=wqs[s_][:], in_=peer_wq.rearrange("(k p) c -> p k c", p=128)[:, :, j * 128:(j + 1) * 128]),
                        writes=[("wqs", n_ % 4)], dma=True)

            tctr = 0
            for g in range(4):
                for t in range(4):
                    s = tctr % 2
                    tctr += 1
                    r0 = g * 512 + t * 128
                    ph.op("sp", lambda e, s=s, r0=r0: e.dma_start(out=xt[s][:], in_=x1_d[r0:r0 + 128, :]), writes=[("xt", s)], dma=True)
                    ph.op("act", lambda e, s=s: e.activation(out=junk[:], in_=xt[s][:], func=AF.Square, accum_out=ss[:, s:s + 1]),
                          reads=[("xt", s)], writes=["junk", ("ss", s)])
                    ph.op("act", lambda e, s=s: e.activation(out=rs[:, s:s + 1], in_=ss[:, s:s + 1], func=AF.Sqrt, scale=1.0 / D, bias=EPS),
                          reads=[("ss", s)], writes=[("rs", s)])
                    ph.op("dve", lambda e, s=s: e.reciprocal(out=rs[:, 2 + s:3 + s], in_=rs[:, s:s + 1]), reads=[("rs", s)], writes=[("rs2", s)])
                    ph.op("dve", lambda e, s=s: e.scalar_tensor_tensor(out=xn[s][:], in0=xt[s][:], scalar=rs[:, 2 + s:3 + s], in1=A2bc[:],
                                                                      op0=ALU.mult, op1=ALU.mult),
                          reads=[("xt", s), ("rs2", s), "A2bc"], writes=[("xn", s)])
                    ph.op("pool", lambda e, s=s: e.tensor_tensor(out=xn[s][:], in0=xn[s][:], in1=sh2bc[:], op=ALU.add),
                          reads=[("xn", s), "sh2bc"], writes=[("xn", s)])
                    for kg in range(4):
                        bk = kg % 2

                        def trs(e, s=s, kg=kg, bk=bk):
                            last = None
                            for kk in range(4):
                                k = 4 * kg + kk
                                last = e.transpose(out=tp[bk][:, kk * 128:(kk + 1) * 128], in_=xn[s][:, k * 128:(k + 1) * 128], identity=ident[:])
                            return last
                        ph.op("pe", trs, reads=[("xn", s)], writes=[("tp", bk)])
                        if kg % 2 == 0:
                            ph.op("act", lambda e, kg=kg, bk=bk, t=t: e.activation(
                                out=h2T[:, 4 * kg:4 * kg + 4, t * 128:(t + 1) * 128],
                                in_=tp[bk][:].rearrange("p (a c) -> p a c", a=4), func=AF.Copy),
                                reads=[("tp", bk)], writes=["h2T"])
                        else:
                            ph.op("dve", lambda e, kg=kg, bk=bk, t=t: e.tensor_copy(
                                out=h2T[:, 4 * kg:4 * kg + 4, t * 128:(t + 1) * 128],
                                in_=tp[bk][:].rearrange("p (a c) -> p a c", a=4)),
                                reads=[("tp", bk)], writes=["h2T"])
                ph.op("sp", lambda e, g=g: e.dma_start(out=h2T_d[:, :, g * 512:(g + 1) * 512].rearrange("k p n -> p k n"), in_=h2T[:]),
                      reads=["h2T"], writes=[("h2T_d", g)], dma=True, semkey=("st", "h2T"))
                for j in range(16):
                    issue_wq()
                    sl = wctr[1] % 4
                    wctr[1] += 1

                    def mm(e, sl=sl, j=j):
                        last = None
                        for k in range(16):
                            last = e.matmul(qp[j % 2][:], lhsT=wqs[sl][:, k, :], rhs=h2T[:, k, :], start=(k == 0), stop=(k == 15))
                        return last
                    ph.op("pe", mm, reads=[("wqs", sl), "h2T"], writes=[("qp", j % 2)])
                    issue_wq()
                    if j % 2 == 0:
                        ph.op("act", lambda e, j=j: e.activation(out=qT[:, j, :], in_=qp[j % 2][:], func=AF.Copy),
                              reads=[("qp", j % 2)], writes=[("qT", j)])
                    else:
                        ph.op("dve", lambda e, j=j: e.tensor_copy(out=qT[:, j, :], in_=qp[j % 2][:]),
                              reads=[("qp", j % 2)], writes=[("qT", j)])
                for t in range(4):
                    tok0 = g * 512 + t * 128

                    def scm(e, t=t):
                        last = None
                        for j in range(16):
                            last = e.matmul(scp[:, j * 128:(j + 1) * 128], lhsT=qT[:, j, t * 128:(t + 1) * 128], rhs=skT[:, j, :],
                                            start=True, stop=True)
                        return last
                    ph.op("pe", scm, reads=[("qT", j) for j in range(16)] + ["skT"], writes=["scp"])
                    ph.op("act", lambda e: e.activation(out=S[:].rearrange("p a b -> p (a b)"), in_=scp[:], func=AF.Copy),
                          reads=["scp"], writes=["S"])
                    for j in range(16):
                        ph.op("dve", lambda e, j=j: e.max(out=vals[:, j, 0:8], in_=S[:, j, :]), reads=["S"], writes=[("v1", j)])
                    for j in range(16):
                        ph.op("dve", lambda e, j=j: e.match_replace(out=S2[:, j, :], in_to_replace=vals[:, j, 0:8], in_values=S[:, j, :], imm_value=-1e30),
                              reads=["S", ("v1", j)], writes=[("S2", j)])
                    for j in range(16):
                        ph.op("dve", lambda e, j=j: e.max(out=vals[:, j, 8:16], in_=S2[:, j, :]), reads=[("S2", j)], writes=[("v2", j)])
                    for j in range(16):
                        ph.op("dve", lambda e, j=j: e.max_index(out=idx[:, j, 0:8], in_max=vals[:, j, 0:8], in_values=S[:, j, :]),
                              reads=["S", ("v1", j)], writes=[("i1", j)])
                    for j in range(16):
                        ph.op("dve", lambda e, j=j: e.max_index(out=idx[:, j, 8:16], in_max=vals[:, j, 8:16], in_values=S2[:, j, :]),
                              reads=[("S2", j), ("v2", j)], writes=[("i2", j)])
                    allv = [("v1", j) for j in range(16)] + [("v2", j) for j in range(16)]
                    alli = [("i1", j) for j in range(16)] + [("i2", j) for j in range(16)]
                    ph.op("pool", lambda e: e.tensor_copy(out=idxf[:], in_=idx[:]), reads=alli, writes=["idxf"])
                    v4 = vals[:].rearrange("p (h q) k -> p h q k", q=2)
                    ph.op("pool", lambda e, v4=v4: e.tensor_tensor(
                        out=cand[:].rearrange("p h (a b) -> p h a b", a=16),
                        in0=v4[:, :, 0, :].unsqueeze(3).to_broadcast([128, 8, 16, 16]),
                        in1=v4[:, :, 1, :].unsqueeze(2).to_broadcast([128, 8, 16, 16]), op=ALU.add),
                        reads=allv, writes=["cand"])
                    for h in range(8):
                        ph.op("dve", lambda e, h=h: e.max(out=cval[:, h, 0:8], in_=cand[:, h, :]), reads=["cand"], writes=[("c1", h)])
                    for h in range(8):
                        ph.op("dve", lambda e, h=h: e.match_replace(out=cand2[:, h, :], in_to_replace=cval[:, h, 0:8], in_values=cand[:, h, :], imm_value=-1e30),
                              reads=["cand", ("c1", h)], writes=[("cand2", h)])
                    for h in range(8):
                        ph.op("dve", lambda e, h=h: e.max(out=cval[:, h, 8:16], in_=cand2[:, h, :]), reads=[("cand2", h)], writes=[("c2", h)])
                    for h in range(8):
                        ph.op("dve", lambda e, h=h: e.max_index(out=cidx[:, h, 0:8], in_max=cval[:, h, 0:8], in_values=cand[:, h, :]),
                              reads=["cand", ("c1", h)], writes=[("ci1", h)])
                    for h in range(8):
                        ph.op("dve", lambda e, h=h: e.max_index(out=cidx[:, h, 8:16], in_max=cval[:, h, 8:16], in_values=cand2[:, h, :]),
                              reads=[("cand2", h), ("c2", h)], writes=[("ci2", h)])
                    allc = [("c1", h) for h in range(8)] + [("c2", h) for h in range(8)]
                    allci = [("ci1", h) for h in range(8)] + [("ci2", h) for h in range(8)]
                    ph.op("pool", lambda e: e.tensor_tensor(out=ex[:], in0=cval[:], in1=cval[:, :, 0:1].to_broadcast([128, 8, 16]), op=ALU.subtract),
                          reads=allc, writes=["ex"])
                    ph.op("act", lambda e: e.activation(out=ex[:], in_=ex[:], func=AF.Exp), reads=["ex"], writes=["ex"])
                    ph.op("dve", lambda e: e.tensor_reduce(out=esum[:], in_=ex[:], axis=AX.X, op=ALU.add), reads=["ex"], writes=["esum"])
                    ph.op("dve", lambda e: e.reciprocal(out=esum[:], in_=esum[:]), reads=["esum"], writes=["esum"])
                    ph.op("pool", lambda e: e.tensor_tensor(out=slot[:, 2, :].rearrange("p (h k) -> p h k", h=8), in0=ex[:],
                                                            in1=esum[:].unsqueeze(2).to_broadcast([128, 8, 16]), op=ALU.mult),
                          reads=["ex", "esum"], writes=["slot_g"])
                    ph.op("pool", lambda e: e.tensor_copy(out=cif[:], in_=cidx[:]), reads=allci, writes=["cif"])
                    cb = cif[:].unsqueeze(3).to_broadcast([128, 8, 16, 16])
                    lo = k16[:, 1, :].unsqueeze(1).unsqueeze(1).to_broadcast([128, 8, 16, 16])
                    hi = k16[:, 2, :].unsqueeze(1).unsqueeze(1).to_broadcast([128, 8, 16, 16])
                    io = k16[:, 0, :].unsqueeze(1).unsqueeze(1).to_broadcast([128, 8, 16, 16])
                    i4 = idxf[:].rearrange("p (h q) k -> p h q k", q=2)
                    ph.op("dve", lambda e, cb=cb, lo=lo: e.tensor_tensor(out=e1[:], in0=cb, in1=lo, op=ALU.is_ge), reads=["cif", "k16b"], writes=["e1"])
                    ph.op("dve", lambda e, cb=cb, hi=hi: e.tensor_tensor(out=e2[:], in0=cb, in1=hi, op=ALU.is_ge), reads=["cif", "k16c"], writes=["e2"])
                    ph.op("dve", lambda e: e.tensor_tensor(out=e1[:], in0=e1[:], in1=e2[:], op=ALU.subtract), reads=["e1", "e2"], writes=["e1"])
                    ph.op("pool", lambda e, i4=i4: e.tensor_tensor(out=e2[:], in0=e1[:], in1=i4[:, :, 0, :].unsqueeze(2).to_broadcast([128, 8, 16, 16]), op=ALU.mult),
                          reads=["e1", "idxf"], writes=["e2"])
                    ph.op("dve", lambda e: e.tensor_reduce(out=slot[:, 0, :].rearrange("p (h k) -> p h k", h=8), in_=e2[:], axis=AX.X, op=ALU.add),
                          reads=["e2"], writes=["slot_r"])
                    ph.op("pool", lambda e, io=io: e.tensor_tensor(out=e2[:], in0=e1[:], in1=io, op=ALU.mult), reads=["e1", "k16a"], writes=["e2"])
                    ph.op("dve", lambda e: e.tensor_reduce(out=i_f[:], in_=e2[:], axis=AX.X, op=ALU.add), reads=["e2"], writes=["i_f"])
                    ph.op("dve", lambda e: e.scalar_tensor_tensor(out=jjf[:], in0=i_f[:], scalar=-16.0, in1=cif[:], op0=ALU.mult, op1=ALU.add),
                          reads=["i_f", "cif"], writes=["jjf"])
                    ph.op("dve", lambda e, io=io: e.tensor_tensor(out=e1[:], in0=jjf[:].unsqueeze(3).to_broadcast([128, 8, 16, 16]), in1=io, op=ALU.is_equal),
                          reads=["jjf", "k16a"], writes=["e1"])
                    ph.op("pool", lambda e, i4=i4: e.tensor_tensor(out=e2[:], in0=e1[:], in1=i4[:, :, 1, :].unsqueeze(2).to_broadcast([128, 8, 16, 16]), op=ALU.mult),
                          reads=["e1", "idxf"], writes=["e2"])
                    ph.op("dve", lambda e: e.tensor_reduce(out=slot[:, 1, :].rearrange("p (h k) -> p h k", h=8), in_=e2[:], axis=AX.X, op=ALU.add),
                          reads=["e2"], writes=["slot_c"])
                    st_ = t % 2

                    def trs(e):
                        last = None
                        for q in range(3):
                            last = e.transpose(out=tp[0][:, q * 128:(q + 1) * 128], in_=slot[:, q, :], identity=ident[:])
                        return last
                    ph.op("pe", trs, reads=["slot_r", "slot_c", "slot_g"], writes=[("tp", 0)])
                    ph.op("act", lambda e, st_=st_: e.activation(out=slT[st_][:], in_=tp[0][:, 0:384].rearrange("p (a c) -> p a c", a=3), func=AF.Copy),
                          reads=[("tp", 0)], writes=[("slT", st_)])
                    ph.op("sp", lambda e, st_=st_, tok0=tok0: e.dma_start(out=slots_d[:, :, tok0:tok0 + 128].rearrange("q p n -> p q n"), in_=slT[st_][:]),
                          reads=[("slT", st_)], writes=[("slots_d", tok0)], dma=True, semkey=("st", "slT", st_))
            ph.emit()
        if stop_after == "D":
            _dbg_copy3(nc, dbg, slots_d)
            return nc

        with ExitStack() as ph_es:
            ph = Phase(nc, "E")
            def sbp(nm, shape, dt=F32):
                return ph_es.enter_context(nc.sbuf_tensor("e_" + nm, list(shape), dt))
            def psp(nm, shape, dt=F32):
                return ph_es.enter_context(nc.psum_tensor("e_" + nm, list(shape), dt))
            slT = [sbp(f"slT{i}", [128, 3, 128]) for i in range(2)]
            Ab = [sbp(f"Ab{i}", [128, 128], BF16) for i in range(8)]
            Bb = [sbp(f"Bb{i}", [128, 128], BF16) for i in range(8)]
            Gs = [sbp(f"Gs{i}", [128, 32, 128, 4], BF16) for i in range(2)]
            Gp = [psp(f"Gp{i}", [128, 512]) for i in range(4)]
            for t in range(T // 128):
                s = t % 2
                ph.op("sp", lambda e, s=s, t=t: e.dma_start(out=slT[s][:], in_=slots_d[:, :, t * 128:(t + 1) * 128].rearrange("q p n -> p q n")),
                      writes=[("slT", s)], dma=True)
                for n4 in range(32):
                    bk = n4 % 4
                    for q in range(4):
                        n = n4 * 4 + q
                        a_ = n % 8
                        ph.op("dve", lambda e, s=s, n=n, a_=a_: e.tensor_scalar(out=Ab[a_][:], in0=iota_bf[:], scalar1=slT[s][:, 0, n:n + 1],
                                                                               scalar2=slT[s][:, 2, n:n + 1], op0=ALU.is_equal, op1=ALU.mult),
                              reads=[("slT", s)], writes=[("Ab", a_)])
                        ph.op("dve", lambda e, s=s, n=n, a_=a_: e.tensor_scalar(out=Bb[a_][:], in0=iota_bf[:], scalar1=slT[s][:, 1, n:n + 1],
                                                                               scalar2=None, op0=ALU.is_equal),
                              reads=[("slT", s)], writes=[("Bb", a_)])
                        ph.op("pe", lambda e, a_=a_, bk=bk, q=q: e.matmul(Gp[bk][:, q * 128:(q + 1) * 128], lhsT=Bb[a_][:], rhs=Ab[a_][:], start=True, stop=True),
                              reads=[("Ab", a_), ("Bb", a_)], writes=[("Gp", bk)])
                    ph.op("act", lambda e, s=s, n4=n4, bk=bk: e.activation(
                        out=Gs[s][:, :, n4 * 4:n4 * 4 + 4, :],
                        in_=Gp[bk][:].rearrange("p (q g r) -> p g q r", q=4, g=32, r=4), func=AF.Copy),
                        reads=[("Gp", bk)], writes=[("Gs", s)])
                for gq in range(4):
                    for g2 in range(2):
                        gi = gq * 2 + g2
                        ph.op("sp", lambda e, s=s, t=t, gi=gi: e.dma_start(out=G_ds[gi][:, :, t * 128:(t + 1) * 128, :],
                                                                          in_=Gs[s][:, gi * 4:(gi + 1) * 4, :, :]),
                              reads=[("Gs", s)], writes=[("G_d", t, gi)], dma=True, semkey=("st", "Gs", s, gq))
            ph.emit()

        if stop_after == "E":
            return nc
        y2_d = nc.dram_tensor("y2_d", [T, D], F32).ap()
        RGV = 2
        for hb in range(T // HB):
            with ExitStack() as ph_es:
                ph = Phase(nc, f"F{hb}")
                def sbp(nm, shape, dt=F32):
                    return ph_es.enter_context(nc.sbuf_tensor(f"f{hb}_" + nm, list(shape), dt))
                def psp(nm, shape, dt=F32):
                    return ph_es.enter_context(nc.psum_tensor(f"f{hb}_" + nm, list(shape), dt))
                h2T = sbp("h2T", [128, 16, HB], BF16)
                acc = sbp("acc", [128, HB // 128, D])
                ub = [sbp(f"ub{i}", [128, D], BF16) for i in range(4)]
                uT = [sbp(f"uT{i}", [128, 16, 128], BF16) for i in range(4)]
                vb = [sbp(f"vb{i}", [128, D], BF16) for i in range(4)]
                Gg = [sbp(f"Gg{i}", [128, HB, 4], BF16) for i in range(2)]
                gl = [sbp(f"gl{i}", [128, HB]) for i in range(2)]
                W = [sbp(f"W{i}", [128, HB], BF16) for i in range(4)]
                tpu = [psp(f"tpu{i}", [128, 1024], BF16) for i in range(2)]
                actp = psp("actp", [128, HB])
                yp = [psp(f"yp{i}", [128, 1024]) for i in range(2)]
                ph.op("sp", lambda e: e.dma_start(out=h2T[:], in_=h2T_d[:, :, hb * HB:(hb + 1) * HB].rearrange("k p n -> p k n")),
                      writes=["h2T"], dma=True)
                nch = (F_GROUPS * RGV) if F_GROUPS else NEXP_CH
                ngroups = nch // RGV
                ypc = [0]

                def loadu(r):
                    if r >= nch:
                        return
                    rg, q4 = divmod(r, 4)
                    if q4 == 0:
                        ph.op("sp", lambda e, gs_=rg % 2, rg=rg: e.dma_start(out=Gg[gs_][:], in_=G_ds[rg // 4][:, rg % 4, hb * HB:(hb + 1) * HB, :]),
                              writes=[("Gg", rg % 2)], dma=True)
                    ph.op("pool", lambda e, r=r: e.dma_start(out=ub[r % 4][:], in_=peer_u[r * 128:(r + 1) * 128, :]), writes=[("ub", r % 4)], dma=True)

                def loadv(r):
                    if r >= nch:
                        return
                    ph.op("pool", lambda e, r=r: e.dma_start(out=vb[r % 4][:], in_=peer_v[r * 128:(r + 1) * 128, :]), writes=[("vb", r % 4)], dma=True)

                def front(r):
                    if r >= nch or F_MODE == 'l':
                        return
                    u_ = r % 4
                    for kg in range(2):
                        def trs(e, u_=u_, kg=kg):
                            last = None
                            for kk in range(8):
                                k = 8 * kg + kk
                                last = e.transpose(out=tpu[kg][:, kk * 128:(kk + 1) * 128], in_=ub[u_][:, k * 128:(k + 1) * 128], identity=ident_bf[:])
                            return last
                        ph.op("pe", trs, reads=[("ub", u_)], writes=[("tpu", kg)])
                        if kg == 0:
                            ph.op("act", lambda e, u_=u_, kg=kg: e.activation(
                                out=uT[u_][:, 8 * kg:8 * kg + 8, :], in_=tpu[kg][:].rearrange("p (a c) -> p a c", a=8), func=AF.Copy),
                                reads=[("tpu", kg)], writes=[("uT", u_, kg)])
                        else:
                            ph.op("dve", lambda e, u_=u_, kg=kg: e.tensor_copy(
                                out=uT[u_][:, 8 * kg:8 * kg + 8, :], in_=tpu[kg][:].rearrange("p (a c) -> p a c", a=8)),
                                reads=[("tpu", kg)], writes=[("uT", u_, kg)])

                def mid(r):
                    if F_MODE == 'l':
                        return
                    u_ = r % 4
                    s2 = r % 2
                    rg, q4 = divmod(r, 4)
                    gs_ = rg % 2
                    for tg in range(HB // 512):
                        def mm(e, u_=u_, tg=tg):
                            last = None
                            for k in range(16):
                                last = e.matmul(actp[:, tg * 512:(tg + 1) * 512], lhsT=uT[u_][:, k, :], rhs=h2T[:, k, tg * 512:(tg + 1) * 512],
                                                start=(k == 0), stop=(k == 15))
                            return last
                        ph.op("pe", mm, reads=[("uT", u_, 0), ("uT", u_, 1), "h2T"], writes=[("actp", tg)])
                    ph.op("act", lambda e, s2=s2: e.activation(out=gl[s2][:], in_=actp[:], func=AF.Gelu_apprx_tanh),
                          reads=[("actp", tg) for tg in range(HB // 512)], writes=[("gl", s2)])
                    ph.op("dve", lambda e, s2=s2, u_=u_, gs_=gs_, q4=q4: e.tensor_tensor(out=W[u_][:], in0=gl[s2][:], in1=Gg[gs_][:, :, q4], op=ALU.mult),
                          reads=[("gl", s2), ("Gg", gs_)], writes=[("W", u_)])

                def vphase(grp):
                    if F_MODE == 'l':
                        return
                    wbase = (grp % 2) * RGV
                    for t in range(HB // 128):
                        for dh in range(2):
                            yb = ypc[0] % 2
                            ypc[0] += 1

                            def vmm(e, t=t, dh=dh, yb=yb, wbase=wbase):
                                last = None
                                for dg in range(2):
                                    c0 = dh * 1024 + dg * 512
                                    for qq in range(RGV):
                                        last = e.matmul(yp[yb][:, dg * 512:(dg + 1) * 512], lhsT=W[wbase + qq][:, t * 128:(t + 1) * 128],
                                                        rhs=vb[wbase + qq][:, c0:c0 + 512], start=(qq == 0), stop=(qq == RGV - 1))
                                return last
                            ph.op("pe", vmm, reads=[("W", wbase + qq) for qq in range(RGV)] + [("vb", wbase + qq) for qq in range(RGV)],
                                  writes=[("yp", yb)])
                            if grp == 0:
                                ph.op("dve", lambda e, t=t, dh=dh, yb=yb: e.tensor_copy(out=acc[:, t, dh * 1024:(dh + 1) * 1024], in_=yp[yb][:]),
                                      reads=[("yp", yb)], writes=[("acc", t, dh)])
                            else:
                                ph.op("dve", lambda e, t=t, dh=dh, yb=yb: e.tensor_tensor(out=acc[:, t, dh * 1024:(dh + 1) * 1024], in0=yp[yb][:],
                                                                                        in1=acc[:, t, dh * 1024:(dh + 1) * 1024], op=ALU.add),
                                      reads=[("yp", yb), ("acc", t, dh)], writes=[("acc", t, dh)])

                for r in range(4):
                    loadu(r)
                front(0)
                front(1)
                for grp in range(ngroups):
                    r0, r1 = 2 * grp, 2 * grp + 1
                    mid(r0)
                    loadv(r0)
                    loadv(r1)
                    loadu(r0 + 4)
                    loadu(r1 + 4)
                    front(r0 + 2)
                    front(r1 + 2)
                    if grp > 0:
                        vphase(grp - 1)
                    mid(r1)
                vphase(ngroups - 1)
                for t in range(HB // 128 if F_MODE == '' else 0):
                    ph.op("sp", lambda e, t=t: e.dma_start(out=y2_d[hb * HB + t * 128:hb * HB + (t + 1) * 128, :], in_=acc[:, t, :]),
                          reads=[("acc", t, 0), ("acc", t, 1)], writes=[("y2_d", t)], dma=True, semkey=("st", "acc", t % 2))
                ph.emit()
        if stop_after == "F":
            _dbg_copy(nc, dbg, y2_d, rows=T)
            return nc

        with ExitStack() as ph_es:
            ph = Phase(nc, "G")
            def sbp(nm, shape, dt=F32):
                return ph_es.enter_context(nc.sbuf_tensor("gq_" + nm, list(shape), dt))
            gg = sbp("gg", [128, D])
            yt = [sbp(f"yt{i}", [128, D]) for i in range(2)]
            xt = [sbp(f"xt{i}", [128, D]) for i in range(2)]
            junk = sbp("junk", [128, D], BF16)
            ss = sbp("ss", [128, 4])
            rs = sbp("rs", [128, 4])
            ph.op("sp", lambda e: e.dma_start(out=gg[:], in_=modbc_d[:, 5 * D:6 * D]), writes=["gg"], dma=True)
            for t in range(T // 128):
                s = t % 2
                ph.op("sp", lambda e, s=s, t=t: e.dma_start(out=yt[s][:], in_=y2_d[t * 128:(t + 1) * 128, :]), writes=[("yt", s)], dma=True)
                ph.op("sp", lambda e, s=s, t=t: e.dma_start(out=xt[s][:], in_=x1_d[t * 128:(t + 1) * 128, :]), writes=[("xt", s)], dma=True)
                ph.op("act", lambda e, s=s: e.activation(out=junk[:], in_=yt[s][:], func=AF.Square, accum_out=ss[:, s:s + 1]),
                      reads=[("yt", s)], writes=["junk", ("ss", s)])
                ph.op("act", lambda e, s=s: e.activation(out=rs[:, s:s + 1], in_=ss[:, s:s + 1], func=AF.Sqrt, scale=1.0 / D, bias=EPS),
                      reads=[("ss", s)], writes=[("rs", s)])
                ph.op("dve", lambda e, s=s: e.reciprocal(out=rs[:, 2 + s:3 + s], in_=rs[:, s:s + 1]), reads=[("rs", s)], writes=[("rs2", s)])
                ph.op("dve", lambda e, s=s: e.scalar_tensor_tensor(out=yt[s][:], in0=yt[s][:], scalar=rs[:, 2 + s:3 + s], in1=gg[:],
                                                                  op0=ALU.mult, op1=ALU.mult),
                      reads=[("yt", s), ("rs2", s), "gg"], writes=[("yt", s)])
                ph.op("pool", lambda e, s=s: e.tensor_tensor(out=yt[s][:], in0=yt[s][:], in1=xt[s][:], op=ALU.add),
                      reads=[("yt", s), ("xt", s)], writes=[("yt", s)])
                ph.op("sp", lambda e, s=s, t=t: e.dma_start(out=out[t * 128:(t + 1) * 128, :], in_=yt[s][:]),
                      reads=[("yt", s)], writes=[("out", t)], dma=True, semkey=("st", "yt", s))
            ph.emit()
    return nc


def _dbg_copy3(nc, dbg, slots_d):
    with ExitStack() as es:
        buf = es.enter_context(nc.sbuf_tensor("dbgbuf3", [128, T], F32))
        s1 = es.enter_context(nc.semaphore("dbg3_s1"))
        with nc.Block() as block:
            @block.sync
            def _(e):
                n = 0
                for q in range(3):
                    e.dma_start(out=buf[:], in_=slots_d[q]).then_inc(s1, 16)
                    n += 16
                    e.wait_ge(s1, n)
                    e.dma_start(out=dbg[q * 128:(q + 1) * 128, :], in_=buf[:]).then_inc(s1, 16)
                    n += 16
                    e.wait_ge(s1, n)


def _dbg_copy(nc, dbg, src, rows):
    with ExitStack() as es:
        buf = es.enter_context(nc.sbuf_tensor("dbgbuf", [128, D], F32))
        s1 = es.enter_context(nc.semaphore("dbg_s1"))
        with nc.Block() as block:
            @block.sync
            def _(e):
                n = 0
                for t in range(rows // 128):
                    e.dma_start(out=buf[:], in_=src[t * 128:(t + 1) * 128, :]).then_inc(s1, 16)
                    n += 16
                    e.wait_ge(s1, n)
                    e.dma_start(out=dbg[t * 128:(t + 1) * 128, :], in_=buf[:]).then_inc(s1, 16)
                    n += 16
                    e.wait_ge(s1, n)


def _make_cols(inp, b, j):
    cols = np.zeros((128, NCOL), np.float32)

    def put(c0, vec):
        v = np.asarray(vec, np.float32).reshape(-1, 128)
        cols[:, c0:c0 + v.shape[0]] = v.T
    put(C_C, inp["c"][b])
    for tap in range(4):
        put(C_LW + tap * 8, inp["lru_conv_w"][0, tap])
    put(C_LB, inp["lru_conv_b"][0])
    put(C_BA, inp["lru_ba"][0])
    put(C_BX, inp["lru_bx"][0])
    put(C_LAM, inp["lru_lambda"][0])
    for tap in range(31):
        put(C_CW + tap * 8, inp["conf_dw_w"][0, tap])
    put(C_CB, inp["conf_dw_b"][0])
    put(C_LG, inp["conf_ln_g"][0])
    put(C_LNB, inp["conf_ln_b"][0])
    nvalid_blocks = (T * j) // TB
    for blk in range(NPRE // TB):
        cols[:, C_FL + blk] = 1.0 if blk >= (NPRE // TB - nvalid_blocks) else 0.0
    return cols


def make_in_maps(inp):
    x = np.ascontiguousarray(inp["x"], dtype=np.float32)
    shared = {
        "ident": np.eye(128, dtype=np.float32),
        "iota": np.tile(np.arange(128, dtype=np.float32)[None, :], (128, 1)),
        "w_mod": np.ascontiguousarray(inp["w_mod"][0]),
        "b_mod": np.ascontiguousarray(inp["b_mod"][0]),
        "g_pre_mix": np.ascontiguousarray(inp["g_pre_mix"][0]),
        "g_post_mix": np.ascontiguousarray(inp["g_post_mix"][0]),
        "g_pre_ffn": np.ascontiguousarray(inp["g_pre_ffn"][0]),
        "g_post_ffn": np.ascontiguousarray(inp["g_post_ffn"][0]),
        "w_in": np.ascontiguousarray(inp["w_in"][0]),
        "lru_wa": np.ascontiguousarray(inp["lru_wa"][0]),
        "lru_wx": np.ascontiguousarray(inp["lru_wx"][0]),
        "w_out": np.ascontiguousarray(inp["w_out"][0]),
        "peer_wq": np.ascontiguousarray(inp["peer_wq"][0]),
        "peer_sk": np.ascontiguousarray(inp["peer_subkeys"][0].reshape(16, 128, 128)),
        "peer_u": np.ascontiguousarray(inp["peer_u"][0]),
        "peer_v": np.ascontiguousarray(inp["peer_v"][0]),
    }
    maps = []
    for c in range(8):
        b, j = divmod(c, 4)
        m = dict(shared)
        m["x_main"] = np.ascontiguousarray(x[b, T * j:T * (j + 1)])
        pre = np.zeros((NPRE, D), np.float32)
        nv = T * j
        if nv:
            pre[NPRE - nv:] = x[b, 0:nv]
        m["x_pre"] = pre
        m["cols"] = _make_cols(inp, b, j)
        maps.append(m)
    return maps


def kernel(**inputs):
    nc = build_program()
    maps = make_in_maps(inputs)
    res = run_bass_kernel_spmd(nc, maps, core_ids=list(range(8)))
    out = np.empty((2, 8192, D), np.float32)
    for c in range(8):
        b, j = divmod(c, 4)
        out[b, T * j:T * (j + 1)] = res.results[c]["out"]
    return out
```

```python
import numpy as np
from contextlib import ExitStack
import concourse.bass as bass
import concourse.mybir as mybir
from concourse.bass_utils import run_bass_kernel_spmd

F32 = mybir.dt.float32
BF16 = mybir.dt.bfloat16
U32 = mybir.dt.uint32
AF = mybir.ActivationFunctionType
ALU = mybir.AluOpType
AX = mybir.AxisListType

D = 2048
KC = 16
T = 2048
NPRE = 6144
TB = 1024
EPS = 1e-6
NEXP_CH = 128
RG = 4
HB = 1024
F_GROUPS = 0
F_MODE = ''

C_C = 0
C_LW = 16
C_LB = 48
C_BA = 56
C_BX = 64
C_LAM = 72
C_CW = 80
C_CB = 328
C_LG = 336
C_LNB = 344
C_FL = 352
NCOL = 358


class _Op:
    __slots__ = ("eng", "fn", "deps", "need_inc", "dma", "semkey", "token")


class Phase:
    ENG = ("sp", "pool", "act", "dve", "pe")

    gpool = None

    def __init__(self, nc, name):
        self.nc = nc
        self.name = name
        self.ops = []
        self.lw = {}
        self.rd = {}

    def op(self, eng, fn, reads=(), writes=(), dma=False, semkey=None):
        o = _Op()
        o.eng = eng
        o.fn = fn
        o.dma = dma
        o.need_inc = dma
        o.token = None
        o.semkey = semkey if semkey is not None else (("dma", writes[0]) if dma else None)
        deps = []
        for k in reads:
            w = self.lw.get(k)
            if w is not None:
                deps.append(w)
        for k in writes:
            w = self.lw.get(k)
            if w is not None:
                deps.append(w)
            deps.extend(self.rd.get(k, ()))
        o.deps = []
        seen = set()
        for d in deps:
            if id(d) not in seen and d is not o:
                seen.add(id(d))
                o.deps.append(d)
                d.need_inc = True
        for k in writes:
            self.lw[k] = o
            self.rd[k] = []
        for k in reads:
            self.rd.setdefault(k, []).append(o)
        self.ops.append(o)
        return o

    def emit(self):
        nc = self.nc
        gp = self.gpool
        local = {}
        for o in self.ops:
            if o.dma:
                k = o.semkey
                local[k] = local.get(k, 0) + 16
                o.token = (k, local[k])
            elif o.need_inc:
                k = ("eng", o.eng)
                local[k] = local.get(k, 0) + 1
                o.token = (k, local[k])
        swkeys = set(o.semkey for o in self.ops if o.dma and o.eng == "pool")
        slot = {}
        nhw = nsw = 0
        for k in local:
            if k[0] == "eng":
                slot[k] = self.ENG.index(k[1])
                continue
            lst = gp["sw"] if k in swkeys else gp["hw"]
            n_ = nsw if k in swkeys else nhw
            if n_ >= len(lst):
                lst.append(None)
            if k in swkeys:
                nsw += 1
            else:
                nhw += 1
            slot[k] = ("sw" if k in swkeys else "hw", n_)
        def getslot(sl):
            if isinstance(sl, int):
                while len(gp["sems"]) < 5:
                    i = len(gp["sems"])
                    gp["sems"].append(gp["stack"].enter_context(nc.semaphore(f"gsem{i}")))
                    gp["counts"].append(0)
                return sl
            kind, n_ = sl
            lst = gp[kind]
            if lst[n_] is None:
                i = len(gp["sems"])
                gp["sems"].append(gp["stack"].enter_context(nc.semaphore(f"gsem{i}")))
                gp["counts"].append(0)
                lst[n_] = i
            return lst[n_]
        getslot(0)
        slot = {k: getslot(v) for k, v in slot.items()}
        base = {k: gp["counts"][slot[k]] for k in local}
        sems = {k: gp["sems"][slot[k]] for k in local}
        by_eng = {e: [o for o in self.ops if o.eng == e] for e in self.ENG}
        with nc.Block() as block:
            reg = {"sp": block.sync, "pool": block.gpsimd, "act": block.scalar,
                   "dve": block.vector, "pe": block.tensor}
            for ename in self.ENG:
                ops = by_eng[ename]

                def body(e, ops=ops):
                    waited = {}
                    for o in ops:
                        need = {}
                        for d in o.deps:
                            if d.eng == "pe" and o.eng == "pe" and not d.dma and not o.dma:
                                continue
                            s, v = d.token
                            if need.get(s, 0) < v:
                                need[s] = v
                        for s, v in need.items():
                            if waited.get(s, 0) < v:
                                e.wait_ge(sems[s], base[s] + v)
                                waited[s] = v
                        ins = o.fn(e)
                        if o.dma:
                            ins.then_inc(sems[o.semkey], 16)
                        elif o.need_inc:
                            ins.then_inc(sems[("eng", o.eng)], 1)
                    for s, v in local.items():
                        if waited.get(s, 0) < v:
                            e.wait_ge(sems[s], base[s] + v)
                reg[ename](body)
        for k, v in local.items():
            gp["counts"][slot[k]] += v


def _col(cols, c):
    return cols[:, c:c + 1]


def build_program(stop_after=None):
    nc = bass.Bass("TRN2", target_bir_lowering=False)

    def din(name, shape, dt=F32):
        return nc.dram_tensor(name, list(shape), dt, kind="ExternalInput").ap()

    x_main = din("x_main", [T, D])
    x_pre = din("x_pre", [NPRE, D])
    cols_d = din("cols", [128, NCOL])
    ident_d = din("ident", [128, 128])
    iota_d = din("iota", [128, 128])
    w_mod = din("w_mod", [D, 6 * D])
    b_mod = din("b_mod", [6 * D])
    g_pre_mix = din("g_pre_mix", [D])
    g_post_mix = din("g_post_mix", [D])
    g_pre_ffn = din("g_pre_ffn", [D])
    g_post_ffn = din("g_post_ffn", [D])
    w_in = din("w_in", [D, 2 * D])
    lru_wa = din("lru_wa", [8, 128, 128])
    lru_wx = din("lru_wx", [8, 128, 128])
    w_out = din("w_out", [D, D])
    peer_wq = din("peer_wq", [D, D])
    peer_sk = din("peer_sk", [16, 128, 128])
    peer_u = din("peer_u", [16384, D])
    peer_v = din("peer_v", [16384, D])
    out = nc.dram_tensor("out", [T, D], F32, kind="ExternalOutput").ap()

    modbc_d = nc.dram_tensor("modbc_d", [128, 6 * D], F32).ap()
    catT_d = nc.dram_tensor("catT_d", [16, 128, T], BF16).ap()
    x1_d = nc.dram_tensor("x1_d", [T, D], F32).ap()
    h2T_d = nc.dram_tensor("h2T_d", [16, 128, T], BF16).ap()
    slots_d = nc.dram_tensor("slots_d", [3, 128, T], F32).ap()
    G_ds = [nc.dram_tensor(f"G_d{i}", [128, 4, T, 4], BF16).ap() for i in range(8)]

    dbg = None
    if stop_after is not None:
        dbg = nc.dram_tensor("dbg", [T, D], F32, kind="ExternalOutput").ap()

    with ExitStack() as outer:
        Phase.gpool = {"sems": [], "counts": [], "stack": outer, "hw": [], "sw": []}

        def sb(name, shape, dt=F32):
            return outer.enter_context(nc.sbuf_tensor("g_" + name, list(shape), dt))
        cols = sb("cols", [128, NCOL])
        ident = sb("ident", [128, 128])
        iota = sb("iota", [128, 128])
        ones = sb("ones", [128, 128])
        ident_bf = sb("ident_bf", [128, 128], BF16)
        iota_bf = sb("iota_bf", [128, 128], BF16)
        carry = sb("carry", [128, 8, 3])
        zcarry = sb("zcarry", [128, 8, 30])
        state = sb("state", [128, 8])
        clc = sb("clc", [128, 16])

        with ExitStack() as ph_es:
            ph = Phase(nc, "A")
            def sbp(name, shape, dt=F32, es=ph_es):
                return es.enter_context(nc.sbuf_tensor("a_" + name, list(shape), dt))
            def psp(name, shape, es=ph_es):
                return es.enter_context(nc.psum_tensor("a_" + name, list(shape), F32))
            caT = sbp("caT", [128, 16])
            caTb = sbp("caTb", [128, 16, 128])
            wm = [sbp(f"wm{i}", [128, 16, 512]) for i in range(2)]
            bmb = [sbp(f"bmb{i}", [128, 512]) for i in range(2)]
            gb = [sbp(f"gb{i}", [128, 512]) for i in range(2)]
            mo = [sbp(f"mo{i}", [128, 512]) for i in range(2)]
            tmpA = sbp("tmpA", [128, 16])
            mps = [psp(f"mps{i}", [128, 512]) for i in range(2)]

            ph.op("sp", lambda e: e.dma_start(out=cols[:], in_=cols_d), writes=["cols"], dma=True)
            ph.op("sp", lambda e: e.dma_start(out=ident[:], in_=ident_d), writes=["ident"], dma=True)
            ph.op("sp", lambda e: e.dma_start(out=iota[:], in_=iota_d), writes=["iota"], dma=True)
            ph.op("pool", lambda e: e.memset(ones[:], 1.0), writes=["ones"])
            ph.op("dve", lambda e: e.tensor_copy(out=ident_bf[:], in_=ident[:]), reads=["ident"], writes=["ident_bf"])
            ph.op("dve", lambda e: e.tensor_copy(out=iota_bf[:], in_=iota[:]), reads=["iota"], writes=["iota_bf"])
            ph.op("pool", lambda e: e.memset(carry[:], 0.0), writes=["carry"])
            ph.op("pool", lambda e: e.memset(zcarry[:], 0.0), writes=["zcarry"])
            ph.op("pool", lambda e: e.memset(state[:], 0.0), writes=["state"])
            ph.op("act", lambda e: e.activation(out=caT[:], in_=cols[:, C_C:C_C + 16], func=AF.Silu),
                  reads=["cols"], writes=["caT"])
            ph.op("act", lambda e: e.activation(out=tmpA[:, 0:8], in_=cols[:, C_LAM:C_LAM + 8], func=AF.Exp, scale=-1.0),
                  reads=["cols"], writes=["tmpA"])
            ph.op("act", lambda e: e.activation(out=tmpA[:, 8:16], in_=tmpA[:, 0:8], func=AF.Ln, bias=1.0),
                  reads=["tmpA"], writes=["tmpA2"])
            ph.op("dve", lambda e: e.tensor_scalar(out=clc[:, 0:8], in0=tmpA[:, 8:16], scalar1=-8.0, scalar2=None, op0=ALU.mult),
                  reads=["tmpA2"], writes=["clc0"])
            ph.op("dve", lambda e: e.tensor_scalar(out=clc[:, 8:16], in0=tmpA[:, 8:16], scalar1=-16.0, scalar2=None, op0=ALU.mult),
                  reads=["tmpA2"], writes=["clc1"])
            for k in range(16):
                ph.op("dve", lambda e, k=k: e.tensor_copy(out=caTb[:, k, :], in_=caT[:, k:k + 1].to_broadcast([128, 128])),
                      reads=["caT"], writes=[("caTb", k)])
            gsrc = {1: g_pre_mix, 2: g_post_mix, 4: g_pre_ffn, 5: g_post_ffn}
            for n in range(24):
                s = n % 2
                sec = n // 4
                cb = (n % 4) * 512
                ph.op("sp", lambda e, n=n, s=s: e.dma_start(
                    out=wm[s][:], in_=w_mod.rearrange("(k p) c -> p k c", p=128)[:, :, n * 512:(n + 1) * 512]),
                    writes=[("wm", s)], dma=True)
                ph.op("sp", lambda e, n=n, s=s: e.dma_start(
                    out=bmb[s][:], in_=b_mod[n * 512:(n + 1) * 512].partition_broadcast(128)),
                    writes=[("bmb", s)], dma=True)
                if sec in gsrc:
                    ph.op("sp", lambda e, s=s, sec=sec, cb=cb: e.dma_start(
                        out=gb[s][:], in_=gsrc[sec][cb:cb + 512].partition_broadcast(128)),
                        writes=[("gb", s)], dma=True)

                def mm(e, s=s):
                    last = None
                    for k in range(16):
                        last = e.matmul(mps[s][:], lhsT=caTb[:, k, :], rhs=wm[s][:, k, :], start=(k == 0), stop=(k == 15))
                    return last
                ph.op("pe", mm, reads=[("wm", s)] + [("caTb", k) for k in range(16)], writes=[("mps", s)])
                if sec in (0, 3):
                    ph.op("dve", lambda e, s=s: e.tensor_tensor(out=mo[s][:], in0=mps[s][:], in1=bmb[s][:], op=ALU.add),
                          reads=[("mps", s), ("bmb", s)], writes=[("mo", s)])
                else:
                    ph.op("dve", lambda e, s=s: e.tensor_tensor(out=bmb[s][:], in0=mps[s][:], in1=bmb[s][:], op=ALU.add),
                          reads=[("mps", s), ("bmb", s)], writes=[("bmb", s)])
                    if sec in (1, 4):
                        ph.op("dve", lambda e, s=s: e.scalar_tensor_tensor(out=mo[s][:], in0=bmb[s][:], scalar=1.0, in1=gb[s][:],
                                                                          op0=ALU.add, op1=ALU.mult),
                              reads=[("bmb", s), ("gb", s)], writes=[("mo", s)])
                    else:
                        ph.op("dve", lambda e, s=s: e.tensor_tensor(out=mo[s][:], in0=bmb[s][:], in1=gb[s][:], op=ALU.mult),
                              reads=[("bmb", s), ("gb", s)], writes=[("mo", s)])
                ph.op("sp", lambda e, n=n, s=s: e.dma_start(out=modbc_d[:, n * 512:(n + 1) * 512], in_=mo[s][:]),
                      reads=[("mo", s)], writes=[("modbc_d", n)], dma=True, semkey=("st", "mo", s))
            ph.emit()

        if stop_after == "A":
            _dbg_copy(nc, dbg, modbc_d[:, 0:D], rows=128)
            return nc

        def mixer_phase(name, xsrc, nblocks, prefix):
            with ExitStack() as ph_es:
                ph = Phase(nc, name)
                def sbp(nm, shape, dt=F32):
                    return ph_es.enter_context(nc.sbuf_tensor(name + "_" + nm, list(shape), dt))
                def psp(nm, shape):
                    return ph_es.enter_context(nc.psum_tensor(name + "_" + nm, list(shape), F32))
                N = TB
                A1bc = sbp("A1bc", [128, D])
                sh1bc = sbp("sh1bc", [128, D])
                xt = [sbp(f"xt{i}", [128, D]) for i in range(2)]
                xn = [sbp(f"xn{i}", [128, D]) for i in range(2)]
                junk = sbp("junk", [128, D], BF16)
                ss = sbp("ss", [128, 4])
                rs = sbp("rs", [128, 4])
                hT = sbp("hT", [128, 16, N], BF16)
                wsl = [sbp(f"wsl{i}", [128, 16, 128], BF16) for i in range(4)]
                wga = sbp("wga", [128, 8, 128])
                wgx = sbp("wgx", [128, 8, 128])
                NBK = dict(xpad=2, xl=3, rr=1, ig=2, aa=2, a2=2, uu=1, hs=1) if prefix else dict(xpad=1, xl=1, rr=1, ig=1, aa=1, a2=1, uu=1, hs=1)
                xpadS = [sbp(f"xpad{j}", [128, N + 3]) for j in range(NBK["xpad"])]
                xpad = xpadS[0]
                cA = sbp("cA", [128, N])
                cB = sbp("cB", [128, N])
                xlS = [sbp(f"xl{j}", [128, N]) for j in range(NBK["xl"])]
                xl = xlS[0]
                rrS = [sbp(f"rr{j}", [128, N]) for j in range(NBK["rr"])]
                rr = rrS[0]
                igS = [sbp(f"ig{j}", [128, N]) for j in range(NBK["ig"])]
                ig = igS[0]
                aaS = [sbp(f"aa{j}", [128, N]) for j in range(NBK["aa"])]
                aa = aaS[0]
                a2S = [sbp(f"a2{j}", [128, N]) for j in range(NBK["a2"])]
                a2 = a2S[0]
                uuS = [sbp(f"uu{j}", [128, N]) for j in range(NBK["uu"])]
                uu = uuS[0]
                hsS = [sbp(f"hs{j}", [128, N]) for j in range(NBK["hs"])]
                hs = hsS[0]
                tp = [psp(f"tp{i}", [128, 512]) for i in range(2)]
                pp = psp("pp", [128, N])
                gpa = psp("gpa", [128, N])
                gpx = psp("gpx", [128, N])
                if not prefix:
                    zpad = sbp("zpad", [128, N + 30])
                    cz = sbp("cz", [128, 8, N])
                    gel = sbp("gel", [128, N])
                    catc = [sbp(f"catc{i}", [128, N], BF16) for i in range(2)]
                    mean = aa
                    rstd = a2
                    sqt = [rr, ig]
                zs = sbp("zs", [128, 32])
                zsg = sbp("zsg", [128, 32])

                ph.op("sp", lambda e: e.dma_start(out=A1bc[:], in_=modbc_d[:, D:2 * D]), writes=["A1bc"], dma=True)
                ph.op("sp", lambda e: e.dma_start(out=sh1bc[:], in_=modbc_d[:, 0:D]), writes=["sh1bc"], dma=True)
                ph.op("sp", lambda e: e.dma_start(out=wga[:], in_=lru_wa.rearrange("h i j -> i h j")), writes=["wga"], dma=True)
                ph.op("sp", lambda e: e.dma_start(out=wgx[:], in_=lru_wx.rearrange("h i j -> i h j")), writes=["wgx"], dma=True)

                plan = []
                for b_ in range(nblocks):
                    for i_ in range(8):
                        plan.append(i_ * 128)
                        if not prefix:
                            plan.append(1024 + i_ * 128)
                    if (prefix and b_ == nblocks - 1) or not prefix:
                        for i_ in range(8):
                            plan.append(2048 + i_ * 128)
                            plan.append(3072 + i_ * 128)
                wctr = [0, 0]

                def issue_loads():
                    while wctr[0] < len(plan) and wctr[0] < wctr[1] + 3:
                        n_ = wctr[0]
                        s_ = n_ % 4
                        c0 = plan[n_]
                        wctr[0] += 1
                        ph.op("pool", lambda e, s_=s_, c0=c0: e.dma_start(
                            out=wsl[s_][:], in_=w_in.rearrange("(k p) c -> p k c", p=128)[:, :, c0:c0 + 128]),
                            writes=[("wsl", s_)], dma=True)

                def load_slice(c0):
                    issue_loads()
                    n_ = wctr[1]
                    assert plan[n_] == c0, (n_, plan[n_], c0)
                    wctr[1] += 1
                    return n_ % 4

                def proj(dst, dname, s, t0, n):
                    for g0 in range(0, n, 512):
                        gn = min(512, n - g0)

                        def mm(e, g0=g0, gn=gn):
                            last = None
                            for k in range(16):
                                last = e.matmul(dst[:, g0:g0 + gn], lhsT=wsl[s][:, k, :], rhs=hT[:, k, t0 + g0:t0 + g0 + gn],
                                                start=(k == 0), stop=(k == 15))
                            return last
                        ph.op("pe", mm, reads=[("wsl", s), "hT"], writes=[(dname, g0)])
                    issue_loads()

                tctr = [0]
                for b in range(nblocks):
                    for t in range(N // 128):
                        s = tctr[0] % 2
                        tctr[0] += 1
                        r0 = b * N + t * 128
                        ph.op("sp", lambda e, s=s, r0=r0: e.dma_start(out=xt[s][:], in_=xsrc[r0:r0 + 128, :]),
                              writes=[("xt", s)], dma=True)
                        ph.op("act", lambda e, s=s: e.activation(out=junk[:], in_=xt[s][:], func=AF.Square, accum_out=ss[:, s:s + 1]),
                              reads=[("xt", s)], writes=["junk", ("ss", s)])
                        ph.op("act", lambda e, s=s: e.activation(out=rs[:, s:s + 1], in_=ss[:, s:s + 1], func=AF.Sqrt, scale=1.0 / D, bias=EPS),
                              reads=[("ss", s)], writes=[("rs", s)])
                        ph.op("dve", lambda e, s=s: e.reciprocal(out=rs[:, 2 + s:3 + s], in_=rs[:, s:s + 1]),
                              reads=[("rs", s)], writes=[("rs2", s)])
                        ph.op("dve", lambda e, s=s: e.scalar_tensor_tensor(out=xn[s][:], in0=xt[s][:], scalar=rs[:, 2 + s:3 + s], in1=A1bc[:],
                                                                          op0=ALU.mult, op1=ALU.mult),
                              reads=[("xt", s), ("rs2", s), "A1bc"], writes=[("xn", s)])
                        ph.op("pool", lambda e, s=s: e.tensor_tensor(out=xn[s][:], in0=xn[s][:], in1=sh1bc[:], op=ALU.add),
                              reads=[("xn", s), "sh1bc"], writes=[("xn", s)])
                        for kg in range(4):
                            bk = kg % 2

                            def trs(e, s=s, kg=kg, bk=bk):
                                last = None
                                for kk in range(4):
                                    k = 4 * kg + kk
                                    last = e.transpose(out=tp[bk][:, kk * 128:(kk + 1) * 128], in_=xn[s][:, k * 128:(k + 1) * 128],
                                                       identity=ident[:])
                                return last
                            ph.op("pe", trs, reads=[("xn", s), "ident"], writes=[("tp", bk)])
                            eng = "act" if kg % 2 == 0 else "dve"
                            if eng == "act":
                                ph.op("act", lambda e, kg=kg, bk=bk, t=t: e.activation(
                                    out=hT[:, 4 * kg:4 * kg + 4, t * 128:(t + 1) * 128],
                                    in_=tp[bk][:].rearrange("p (a c) -> p a c", a=4), func=AF.Copy),
                                    reads=[("tp", bk)], writes=["hT"])
                            else:
                                ph.op("dve", lambda e, kg=kg, bk=bk, t=t: e.tensor_copy(
                                    out=hT[:, 4 * kg:4 * kg + 4, t * 128:(t + 1) * 128],
                                    in_=tp[bk][:].rearrange("p (a c) -> p a c", a=4)),
                                    reads=[("tp", bk)], writes=["hT"])
                    fl = _col(cols, C_FL + b) if prefix else None

                    def st0(i, b=b, fl=fl):
                        j = i % NBK["xpad"]
                        sx = load_slice(i * 128)
                        proj(pp, "pp", sx, 0, N)
                        ph.op("act", lambda e, j=j: e.activation(out=xpadS[j][:, 3:3 + N], in_=pp[:], func=AF.Copy),
                              reads=[("pp", 0), ("pp", 512)], writes=[("xpad", j)])
                        ph.op("pool", lambda e, i=i, j=j: e.tensor_copy(out=xpadS[j][:, 0:3], in_=carry[:, i, :]),
                              reads=[("carry", i)], writes=[("xpadc", j)])

                    def st1(i, b=b, fl=fl):
                        jx = i % NBK["xpad"]
                        j = i % NBK["xl"]
                        xp = xpadS[jx]
                        rk = [("xpad", jx), ("xpadc", jx)]
                        ph.op("dve", lambda e, i=i, xp=xp: e.tensor_scalar(out=cA[:], in0=xp[:, 0:N], scalar1=_col(cols, C_LW + i),
                                                                          scalar2=_col(cols, C_LB + i), op0=ALU.mult, op1=ALU.add),
                              reads=rk, writes=["cA"])
                        ph.op("dve", lambda e, i=i, xp=xp: e.scalar_tensor_tensor(out=cB[:], in0=xp[:, 1:1 + N], scalar=_col(cols, C_LW + 8 + i),
                                                                                 in1=cA[:], op0=ALU.mult, op1=ALU.add),
                              reads=rk + ["cA"], writes=["cB"])
                        ph.op("dve", lambda e, i=i, xp=xp: e.scalar_tensor_tensor(out=cA[:], in0=xp[:, 2:2 + N], scalar=_col(cols, C_LW + 16 + i),
                                                                                 in1=cB[:], op0=ALU.mult, op1=ALU.add),
                              reads=rk + ["cB"], writes=["cA"])
                        ph.op("dve", lambda e, i=i, xp=xp, j=j: e.scalar_tensor_tensor(out=xlS[j][:], in0=xp[:, 3:3 + N], scalar=_col(cols, C_LW + 24 + i),
                                                                                      in1=cA[:], op0=ALU.mult, op1=ALU.add),
                              reads=rk + ["cA"], writes=[("xl", j)])
                        if prefix:
                            ph.op("pool", lambda e, i=i, fl=fl, xp=xp: e.tensor_scalar(out=carry[:, i, :], in0=xp[:, N:N + 3], scalar1=fl,
                                                                                      scalar2=None, op0=ALU.mult),
                                  reads=[("xpad", jx)], writes=[("carry", i)])
                        else:
                            ph.op("pool", lambda e, i=i, xp=xp: e.tensor_copy(out=carry[:, i, :], in_=xp[:, N:N + 3]),
                                  reads=[("xpad", jx)], writes=[("carry", i)])

                    def st2(i, b=b, fl=fl):
                        jl, jr, jg, ja, j2 = (i % NBK[k_] for k_ in ("xl", "rr", "ig", "aa", "a2"))
                        for (wg, gp, nm) in ((wga, gpa, "gpa"), (wgx, gpx, "gpx")):
                            for g0 in (0, 512):
                                ph.op("pe", lambda e, wg=wg, gp=gp, g0=g0, i=i, jl=jl: e.matmul(gp[:, g0:g0 + 512], lhsT=wg[:, i, :], rhs=xlS[jl][:, g0:g0 + 512],
                                                                                           start=True, stop=True),
                                      reads=[("xl", jl), "wga", "wgx"], writes=[(nm, g0)])
                        ph.op("act", lambda e, i=i, jr=jr: e.activation(out=rrS[jr][:], in_=gpa[:], func=AF.Sigmoid, bias=_col(cols, C_BA + i)),
                              reads=[("gpa", 0), ("gpa", 512)], writes=[("rr", jr)])
                        ph.op("act", lambda e, i=i, jg=jg: e.activation(out=igS[jg][:], in_=gpx[:], func=AF.Sigmoid, bias=_col(cols, C_BX + i)),
                              reads=[("gpx", 0), ("gpx", 512)], writes=[("ig", jg)])
                        ph.op("act", lambda e, i=i, jr=jr, ja=ja: e.activation(out=aaS[ja][:], in_=rrS[jr][:], func=AF.Exp, scale=clc[:, i:i + 1]),
                              reads=[("rr", jr)], writes=[("aa", ja)])
                        ph.op("act", lambda e, i=i, jr=jr, j2=j2: e.activation(out=a2S[j2][:], in_=rrS[jr][:], func=AF.Exp, scale=clc[:, 8 + i:9 + i]),
                              reads=[("rr", jr)], writes=[("a2", j2)])
                        ph.op("act", lambda e, j2=j2: e.activation(out=a2S[j2][:], in_=a2S[j2][:], func=AF.Sqrt, scale=-1.0, bias=1.0),
                              reads=[("a2", j2)], writes=[("a2", j2)])

                    def st3(i, b=b, fl=fl):
                        jl, jg, ja, j2, ju, jh = (i % NBK[k_] for k_ in ("xl", "ig", "aa", "a2", "uu", "hs"))
                        ph.op("pool", lambda e, jl=jl, jg=jg, ju=ju: e.tensor_tensor(out=uuS[ju][:], in0=igS[jg][:], in1=xlS[jl][:], op=ALU.mult),
                              reads=[("ig", jg), ("xl", jl)], writes=[("uu", ju)])
                        ph.op("pool", lambda e, j2=j2, ju=ju: e.tensor_tensor(out=uuS[ju][:], in0=uuS[ju][:], in1=a2S[j2][:], op=ALU.mult),
                              reads=[("uu", ju), ("a2", j2)], writes=[("uu", ju)])
                        ph.op("dve", lambda e, i=i, ja=ja, ju=ju, jh=jh: e.tensor_tensor_scan(out=hsS[jh][:], data0=aaS[ja][:], data1=uuS[ju][:],
                                                                                             initial=state[:, i:i + 1], op0=ALU.mult, op1=ALU.add),
                              reads=[("aa", ja), ("uu", ju), ("state", i)], writes=[("hs", jh)])
                        if prefix:
                            ph.op("pool", lambda e, i=i, fl=fl, jh=jh: e.tensor_scalar(out=state[:, i:i + 1], in0=hsS[jh][:, N - 1:N], scalar1=fl,
                                                                                      scalar2=None, op0=ALU.mult),
                                  reads=[("hs", jh)], writes=[("state", i)])
                        else:
                            ph.op("pool", lambda e, i=i, jh=jh: e.tensor_copy(out=state[:, i:i + 1], in_=hsS[jh][:, N - 1:N]),
                                  reads=[("hs", jh)], writes=[("state", i)])
                            sg_ = load_slice(1024 + i * 128)
                            proj(gpa, "gpa", sg_, 0, N)
                            ph.op("act", lambda e: e.activation(out=gel[:], in_=gpa[:], func=AF.Gelu_apprx_tanh),
                                  reads=[("gpa", 0), ("gpa", 512)], writes=["gel"])
                            cs_ = i % 2
                            ph.op("dve", lambda e, cs_=cs_, jh=jh: e.tensor_tensor(out=catc[cs_][:], in0=hsS[jh][:], in1=gel[:], op=ALU.mult),
                                  reads=[("hs", jh), "gel"], writes=[("catc", cs_)])
                            ph.op("sp", lambda e, cs_=cs_, i=i, b=b: e.dma_start(out=catT_d[i, :, b * N:(b + 1) * N], in_=catc[cs_][:]),
                                  reads=[("catc", cs_)], writes=[("catT_d", i, b)], dma=True, semkey=("st", "catc", cs_))

                    if prefix:
                        for s_ in range(8 + 3):
                            if s_ < 8:
                                st0(s_)
                            if 0 <= s_ - 1 < 8:
                                st1(s_ - 1)
                            if 0 <= s_ - 2 < 8:
                                st2(s_ - 2)
                            if 0 <= s_ - 3 < 8:
                                st3(s_ - 3)
                    else:
                        for i in range(8):
                            st0(i)
                            st1(i)
                            st2(i)
                            st3(i)
                    if prefix and b == nblocks - 1:
                        for i in range(8):
                            sv = load_slice(2048 + i * 128)
                            sg2 = load_slice(3072 + i * 128)
                            proj(pp, "pp", sv, N - 32, 32)
                            proj(gpa, "gpa", sg2, N - 32, 32)
                            ph.op("act", lambda e: e.activation(out=zsg[:], in_=gpa[:, 0:32], func=AF.Sigmoid),
                                  reads=[("gpa", 0)], writes=["zsg"])
                            ph.op("dve", lambda e: e.tensor_tensor(out=zs[:], in0=pp[:, 0:32], in1=zsg[:], op=ALU.mult),
                                  reads=[("pp", 0), "zsg"], writes=["zs"])
                            ph.op("pool", lambda e, i=i, fl=fl: e.tensor_scalar(out=zcarry[:, i, :], in0=zs[:, 2:32], scalar1=fl,
                                                                               scalar2=None, op0=ALU.mult),
                                  reads=["zs"], writes=[("zcarry", i)])
                    if not prefix:
                        for i in range(8):
                            sv = load_slice(2048 + i * 128)
                            sg2 = load_slice(3072 + i * 128)
                            proj(pp, "pp", sv, 0, N)
                            proj(gpx, "gpx", sg2, 0, N)
                            ph.op("act", lambda e: e.activation(out=gel[:], in_=gpx[:], func=AF.Sigmoid),
                                  reads=[("gpx", 0), ("gpx", 512)], writes=["gel"])
                            ph.op("dve", lambda e: e.tensor_tensor(out=zpad[:, 30:30 + N], in0=pp[:], in1=gel[:], op=ALU.mult),
                                  reads=[("pp", 0), ("pp", 512), "gel"], writes=["zpad"])
                            ph.op("pool", lambda e, i=i: e.tensor_copy(out=zpad[:, 0:30], in_=zcarry[:, i, :]),
                                  reads=[("zcarry", i), "zcarry"], writes=["zpadc"])
                            ph.op("dve", lambda e, i=i: e.tensor_scalar(out=cA[:], in0=zpad[:, 0:N], scalar1=_col(cols, C_CW + i),
                                                                       scalar2=_col(cols, C_CB + i), op0=ALU.mult, op1=ALU.add),
                                  reads=["zpad", "zpadc"], writes=["cA"])
                            bufs = [(cA, "cA"), (cB, "cB")]
                            for j in range(1, 31):
                                src, sn = bufs[(j - 1) % 2]
                                if j == 30:
                                    o_ap, dn = cz[:, i, :], ("cz", i)
                                else:
                                    o_ap, dn = bufs[j % 2][0][:], bufs[j % 2][1]
                                ph.op("dve", lambda e, i=i, j=j, src=src, o_ap=o_ap: e.scalar_tensor_tensor(
                                    out=o_ap, in0=zpad[:, j:j + N], scalar=_col(cols, C_CW + j * 8 + i), in1=src[:],
                                    op0=ALU.mult, op1=ALU.add),
                                    reads=["zpad", "zpadc", sn], writes=[dn])
                            ph.op("pool", lambda e, i=i: e.tensor_copy(out=zcarry[:, i, :], in_=zpad[:, N:N + 30]),
                                  reads=["zpad"], writes=[("zcarry", i)])
                            q_ = i % 2
                            ph.op("act", lambda e, i=i, q_=q_: e.activation(out=sqt[q_][:], in_=cz[:, i, :], func=AF.Square),
                                  reads=[("cz", i)], writes=[(("rr", 0), ("ig", 0))[q_]])
                            for g0 in (0, 512):
                                ph.op("pe", lambda e, i=i, g0=g0: e.matmul(gpa[:, g0:g0 + 512], lhsT=ones[:], rhs=cz[:, i, g0:g0 + 512],
                                                                          start=(i == 0), stop=(i == 7)),
                                      reads=[("cz", i), "ones"], writes=[("gpa", g0)])
                            for g0 in (0, 512):
                                ph.op("pe", lambda e, i=i, g0=g0, q_=q_: e.matmul(tp[g0 // 512][:], lhsT=ones[:], rhs=sqt[q_][:, g0:g0 + 512],
                                                                                 start=(i == 0), stop=(i == 7)),
                                      reads=[(("rr", 0), ("ig", 0))[q_], "ones"], writes=[("tp", g0 // 512)])
                        ph.op("act", lambda e: e.activation(out=mean[:], in_=gpa[:], func=AF.Copy, scale=1.0 / 1024.0),
                              reads=[("gpa", 0), ("gpa", 512)], writes=[("aa", 0)])
                        for g in range(2):
                            ph.op("act", lambda e, g=g: e.activation(out=rstd[:, g * 512:(g + 1) * 512], in_=tp[g][:], func=AF.Copy, scale=1.0 / 1024.0),
                                  reads=[("tp", g)], writes=[("a2", 0)])
                        ph.op("dve", lambda e: e.tensor_tensor(out=cA[:], in0=mean[:], in1=mean[:], op=ALU.mult),
                              reads=[("aa", 0)], writes=["cA"])
                        ph.op("dve", lambda e: e.tensor_tensor(out=rstd[:], in0=rstd[:], in1=cA[:], op=ALU.subtract),
                              reads=[("a2", 0), "cA"], writes=[("a2", 0)])
                        ph.op("act", lambda e: e.activation(out=rstd[:], in_=rstd[:], func=AF.Sqrt, bias=EPS),
                              reads=[("a2", 0)], writes=[("a2", 0)])
                        ph.op("dve", lambda e: e.reciprocal(out=rstd[:], in_=rstd[:]), reads=[("a2", 0)], writes=[("a2", 0)])
                        for i in range(8):
                            ph.op("pool", lambda e, i=i: e.tensor_tensor(out=cA[:], in0=cz[:, i, :], in1=mean[:], op=ALU.subtract),
                                  reads=[("cz", i), ("aa", 0)], writes=["cA"])
                            ph.op("dve", lambda e: e.tensor_tensor(out=cB[:], in0=cA[:], in1=rstd[:], op=ALU.mult),
                                  reads=["cA", ("a2", 0)], writes=["cB"])
                            cs_ = i % 2
                            ph.op("act", lambda e, i=i, cs_=cs_: e.activation(out=catc[cs_][:], in_=cB[:], func=AF.Silu,
                                                                             scale=_col(cols, C_LG + i), bias=_col(cols, C_LNB + i)),
                                  reads=["cB"], writes=[("catc", cs_)])
                            ph.op("sp", lambda e, cs_=cs_, i=i, b=b: e.dma_start(out=catT_d[8 + i, :, b * N:(b + 1) * N], in_=catc[cs_][:]),
                                  reads=[("catc", cs_)], writes=[("catT_d", 8 + i, b)], dma=True, semkey=("st", "catc", cs_))
                ph.emit()

        mixer_phase("P", x_pre, NPRE // TB, True)
        mixer_phase("M", x_main, T // TB, False)
        if stop_after == "M":
            return nc

        with ExitStack() as ph_es:
            ph = Phase(nc, "C")
            def sbp(nm, shape, dt=F32):
                return ph_es.enter_context(nc.sbuf_tensor("c_" + nm, list(shape), dt))
            def psp(nm, shape):
                return ph_es.enter_context(nc.psum_tensor("c_" + nm, list(shape), F32))
            wo = sbp("wo", [128, 16, D], BF16)
            gg = sbp("gg", [128, D])
            ct = [sbp(f"ct{i}", [128, 16, 128], BF16) for i in range(2)]
            xt = [sbp(f"cxt{i}", [128, D]) for i in range(2)]
            yo = [sbp(f"yo{i}", [128, D]) for i in range(2)]
            junk = sbp("cjunk", [128, D], BF16)
            ss = sbp("css", [128, 4])
            rs = sbp("crs", [128, 4])
            yp = [psp(f"yp{i}", [128, D]) for i in range(2)]
            for k in range(16):
                ph.op("pool", lambda e, k=k: e.dma_start(out=wo[:, k, :], in_=w_out[k * 128:(k + 1) * 128, :]),
                      writes=[("wo", k)], dma=True)
            ph.op("sp", lambda e: e.dma_start(out=gg[:], in_=modbc_d[:, 2 * D:3 * D]), writes=["gg"], dma=True)
            for t in range(T // 128):
                s = t % 2
                ph.op("sp", lambda e, s=s, t=t: e.dma_start(out=ct[s][:], in_=catT_d[:, :, t * 128:(t + 1) * 128].rearrange("k p n -> p k n")),
                      writes=[("ct", s)], dma=True)
                ph.op("sp", lambda e, s=s, t=t: e.dma_start(out=xt[s][:], in_=x_main[t * 128:(t + 1) * 128, :]),
                      writes=[("xt", s)], dma=True)

                def mm(e, s=s):
                    last = None
                    for g in range(4):
                        for k in range(16):
                            last = e.matmul(yp[s][:, g * 512:(g + 1) * 512], lhsT=ct[s][:, k, :], rhs=wo[:, k, g * 512:(g + 1) * 512],
                                            start=(k == 0), stop=(k == 15))
                    return last
                ph.op("pe", mm, reads=[("ct", s)] + [("wo", k) for k in range(16)], writes=[("yp", s)])
                ph.op("act", lambda e, s=s: e.activation(out=junk[:], in_=yp[s][:], func=AF.Square, accum_out=ss[:, s:s + 1]),
                      reads=[("yp", s)], writes=["junk", ("ss", s)])
                ph.op("act", lambda e, s=s: e.activation(out=rs[:, s:s + 1], in_=ss[:, s:s + 1], func=AF.Sqrt, scale=1.0 / D, bias=EPS),
                      reads=[("ss", s)], writes=[("rs", s)])
                ph.op("dve", lambda e, s=s: e.reciprocal(out=rs[:, 2 + s:3 + s], in_=rs[:, s:s + 1]),
                      reads=[("rs", s)], writes=[("rs2", s)])
                ph.op("dve", lambda e, s=s: e.scalar_tensor_tensor(out=yo[s][:], in0=yp[s][:], scalar=rs[:, 2 + s:3 + s], in1=gg[:],
                                                                  op0=ALU.mult, op1=ALU.mult),
                      reads=[("yp", s), ("rs2", s), "gg"], writes=[("yo", s)])
                ph.op("pool", lambda e, s=s: e.tensor_tensor(out=yo[s][:], in0=yo[s][:], in1=xt[s][:], op=ALU.add),
                      reads=[("yo", s), ("xt", s)], writes=[("yo", s)])
                ph.op("sp", lambda e, s=s, t=t: e.dma_start(out=x1_d[t * 128:(t + 1) * 128, :], in_=yo[s][:]),
                      reads=[("yo", s)], writes=[("x1_d", t)], dma=True, semkey=("st", "yo", s))
            ph.emit()
        if stop_after == "C":
            _dbg_copy(nc, dbg, x1_d, rows=T)
            return nc

        with ExitStack() as ph_es:
            ph = Phase(nc, "D")
            def sbp(nm, shape, dt=F32):
                return ph_es.enter_context(nc.sbuf_tensor("d_" + nm, list(shape), dt))
            def psp(nm, shape, dt=F32):
                return ph_es.enter_context(nc.psum_tensor("d_" + nm, list(shape), dt))
            A2bc = sbp("A2bc", [128, D])
            sh2bc = sbp("sh2bc", [128, D])
            xt = [sbp(f"xt{i}", [128, D]) for i in range(2)]
            xn = [sbp(f"xn{i}", [128, D]) for i in range(2)]
            junk = sbp("junk", [128, D], BF16)
            ss = sbp("ss", [128, 4])
            rs = sbp("rs", [128, 4])
            h2T = sbp("h2T", [128, 16, 512], BF16)
            wqs = [sbp(f"wqs{i}", [128, 16, 128], BF16) for i in range(4)]
            qT = sbp("qT", [128, 16, 512])
            skT = sbp("skT", [128, 16, 128])
            S = sbp("S", [128, 16, 128])
            S2 = sbp("S2", [128, 16, 128])
            vals = sbp("vals", [128, 16, 16])
            idx = sbp("idx", [128, 16, 16], U32)
            idxf = sbp("idxf", [128, 16, 16])
            cand = sbp("cand", [128, 8, 256])
            cand2 = sbp("cand2", [128, 8, 256])
            cval = sbp("cval", [128, 8, 16])
            cidx = sbp("cidx", [128, 8, 16], U32)
            cif = sbp("cif", [128, 8, 16])
            ex = sbp("ex", [128, 8, 16])
            esum = sbp("esum", [128, 8])
            e1 = sbp("e1", [128, 8, 16, 16])
            e2 = sbp("e2", [128, 8, 16, 16])
            i_f = sbp("i_f", [128, 8, 16])
            jjf = sbp("jjf", [128, 8, 16])
            slot = sbp("slot", [128, 3, 128])
            slT = [sbp(f"slT{i}", [128, 3, 128]) for i in range(2)]
            k16 = sbp("k16", [128, 3, 16])
            tp = [psp(f"tp{i}", [128, 512]) for i in range(2)]
            qp = [psp(f"qp{i}", [128, 512]) for i in range(2)]
            scp = psp("scp", [128, 2048])

            ph.op("sp", lambda e: e.dma_start(out=A2bc[:], in_=modbc_d[:, 4 * D:5 * D]), writes=["A2bc"], dma=True)
            ph.op("sp", lambda e: e.dma_start(out=sh2bc[:], in_=modbc_d[:, 3 * D:4 * D]), writes=["sh2bc"], dma=True)
            ph.op("sp", lambda e: e.dma_start(out=S[:], in_=peer_sk.rearrange("j n d -> n j d")), writes=["S"], dma=True)
            for jg in range(4):
                def trs(e, jg=jg):
                    last = None
                    for jj in range(4):
                        last = e.transpose(out=tp[jg % 2][:, jj * 128:(jj + 1) * 128], in_=S[:, 4 * jg + jj, :], identity=ident[:])
                    return last
                ph.op("pe", trs, reads=["S"], writes=[("tp", jg % 2)])
                ph.op("act", lambda e, jg=jg: e.activation(out=skT[:, 4 * jg:4 * jg + 4, :],
                                                          in_=tp[jg % 2][:].rearrange("p (a c) -> p a c", a=4), func=AF.Copy),
                      reads=[("tp", jg % 2)], writes=["skT"])
            ph.op("dve", lambda e: e.tensor_copy(out=k16[:, 0, :], in_=iota[:, 0:16]), writes=["k16a"])
            ph.op("dve", lambda e: e.tensor_scalar(out=k16[:, 1, :], in0=iota[:, 0:16], scalar1=16.0, scalar2=None, op0=ALU.mult),
                  writes=["k16b"])
            ph.op("dve", lambda e: e.tensor_scalar(out=k16[:, 2, :], in0=iota[:, 0:16], scalar1=16.0, scalar2=16.0, op0=ALU.mult, op1=ALU.add),
                  writes=["k16c"])

            wplan = [j for g in range(4) for j in range(16)]
            wctr = [0, 0]

            def issue_wq():
                while wctr[0] < len(wplan) and wctr[0] < wctr[1] + 3:
                    n_ = wctr[0]
                    wctr[0] += 1
                    ph.op("pool", lambda e, s_=n_ % 4, j=wplan[n_]: e.dma_start(
                        out=wqs[s_][:], in_=peer_wq.rearrange("(k p) c -> p k c", p=128)[:, :, j * 128:(j + 1) * 128]),
                        writes=[("wqs", n_ % 4)], dma=True)

            tctr = 0
            for g in range(4):
                for t in range(4):
                    s = tctr % 2
                    tctr += 1
                    r0 = g * 512 + t * 128
                    ph.op("sp", lambda e, s=s, r0=r0: e.dma_start(out=xt[s][:], in_=x1_d[r0:r0 + 128, :]), writes=[("xt", s)], dma=True)
                    ph.op("act", lambda e, s=s: e.activation(out=junk[:], in_=xt[s][:], func=AF.Square, accum_out=ss[:, s:s + 1]),
                          reads=[("xt", s)], writes=["junk", ("ss", s)])
                    ph.op("act", lambda e, s=s: e.activation(out=rs[:, s:s + 1], in_=ss[:, s:s + 1], func=AF.Sqrt, scale=1.0 / D, bias=EPS),
                          reads=[("ss", s)], writes=[("rs", s)])
                    ph.op("dve", lambda e, s=s: e.reciprocal(out=rs[:, 2 + s:3 + s], in_=rs[:, s:s + 1]), reads=[("rs", s)], writes=[("rs2", s)])
                    ph.op("dve", lambda e, s=s: e.scalar_tensor_tensor(out=xn[s][:], in0=xt[s][:], scalar=rs[:, 2 + s:3 + s], in1=A2bc[:],
                                                                      op0=ALU.mult, op1=ALU.mult),
                          reads=[("xt", s), ("rs2", s), "A2bc"], writes=[("xn", s)])
                    ph.op("pool", lambda e, s=s: e.tensor_tensor(out=xn[s][:], in0=xn[s][:], in1=sh2bc[:], op=ALU.add),
                          reads=[("xn", s), "sh2bc"], writes=[("xn", s)])
                    for kg in range(4):
                        bk = kg % 2

                        def trs(e, s=s, kg=kg, bk=bk):
                            last = None
                            for kk in range(4):
                                k = 4 * kg + kk
                                last = e.transpose(out=tp[bk][:, kk * 128:(kk + 1) * 128], in_=xn[s][:, k * 128:(k + 1) * 128], identity=ident[:])
                            return last
                        ph.op("pe", trs, reads=[("xn", s)], writes=[("tp", bk)])
                        if kg % 2 == 0:
                            ph.op("act", lambda e, kg=kg, bk=bk, t=t: e.activation(
                                out=h2T[:, 4 * kg:4 * kg + 4, t * 128:(t + 1) * 128],
                                in_=tp[bk][:].rearrange("p (a c) -> p a c", a=4), func=AF.Copy),
                                reads=[("tp", bk)], writes=["h2T"])
                        else:
                            ph.op("dve", lambda e, kg=kg, bk=bk, t=t: e.tensor_copy(
                                out=h2T[:, 4 * kg:4 * kg + 4, t * 128:(t + 1) * 128],
                                in_=tp[bk][:].rearrange("p (a c) -> p a c", a=4)),
                                reads=[("tp", bk)], writes=["h2T"])
                ph.op("sp", lambda e, g=g: e.dma_start(out=h2T_d[:, :, g * 512:(g + 1) * 512].rearrange("k p n -> p k n"), in_=h2T[:]),
                      reads=["h2T"], writes=[("h2T_d", g)], dma=True, semkey=("st", "h2T"))
                for j in range(16):
                    issue_wq()
                    sl = wctr[1] % 4
                    wctr[1] += 1

                    def mm(e, sl=sl, j=j):
                        last = None
                        for k in range(16):
                            last = e.matmul(qp[j % 2][:], lhsT=wqs[sl][:, k, :], rhs=h2T[:, k, :], start=(k == 0), stop=(k == 15))
                        return last
                    ph.op("pe", mm, reads=[("wqs", sl), "h2T"], writes=[("qp", j % 2)])
                    issue_wq()
                    if j % 2 == 0:
                        ph.op("act", lambda e, j=j: e.activation(out=qT[:, j, :], in_=qp[j % 2][:], func=AF.Copy),
                              reads=[("qp", j % 2)], writes=[("qT", j)])
                    else:
                        ph.op("dve", lambda e, j=j: e.tensor_copy(out=qT[:, j, :], in_=qp[j % 2][:]),
                              reads=[("qp", j % 2)], writes=[("qT", j)])
                for t in range(4):
                    tok0 = g * 512 + t * 128

                    def scm(e, t=t):
                        last = None
                        for j in range(16):
                            last = e.matmul(scp[:, j * 128:(j + 1) * 128], lhsT=qT[:, j, t * 128:(t + 1) * 128], rhs=skT[:, j, :],
                                            start=True, stop=True)
                        return last
                    ph.op("pe", scm, reads=[("qT", j) for j in range(16)] + ["skT"], writes=["scp"])
                    ph.op("act", lambda e: e.activation(out=S[:].rearrange("p a b -> p (a b)"), in_=scp[:], func=AF.Copy),
                          reads=["scp"], writes=["S"])
                    for j in range(16):
                        ph.op("dve", lambda e, j=j: e.max(out=vals[:, j, 0:8], in_=S[:, j, :]), reads=["S"], writes=[("v1", j)])
                    for j in range(16):
                        ph.op("dve", lambda e, j=j: e.match_replace(out=S2[:, j, :], in_to_replace=vals[:, j, 0:8], in_values=S[:, j, :], imm_value=-1e30),
                              reads=["S", ("v1", j)], writes=[("S2", j)])
                    for j in range(16):
                        ph.op("dve", lambda e, j=j: e.max(out=vals[:, j, 8:16], in_=S2[:, j, :]), reads=[("S2", j)], writes=[("v2", j)])
                    for j in range(16):
                        ph.op("dve", lambda e, j=j: e.max_index(out=idx[:, j, 0:8], in_max=vals[:, j, 0:8], in_values=S[:, j, :]),
                              reads=["S", ("v1", j)], writes=[("i1", j)])
                    for j in range(16):
                        ph.op("dve", lambda e, j=j: e.max_index(out=idx[:, j, 8:16], in_max=vals[:, j, 8:16], in_values=S2[:, j, :]),
                              reads=[("S2", j), ("v2", j)], writes=[("i2", j)])
                    allv = [("v1", j) for j in range(16)] + [("v2", j) for j in range(16)]
                    alli = [("i1", j) for j in range(16)] + [("i2", j) for j in range(16)]
                    ph.op("pool", lambda e: e.tensor_copy(out=idxf[:], in_=idx[:]), reads=alli, writes=["idxf"])
                    v4 = vals[:].rearrange("p (h q) k -> p h q k", q=2)
                    ph.op("pool", lambda e, v4=v4: e.tensor_tensor(
                        out=cand[:].rearrange("p h (a b) -> p h a b", a=16),
                        in0=v4[:, :, 0, :].unsqueeze(3).to_broadcast([128, 8, 16, 16]),
                        in1=v4[:, :, 1, :].unsqueeze(2).to_broadcast([128, 8, 16, 16]), op=ALU.add),
                        reads=allv, writes=["cand"])
                    for h in range(8):
                        ph.op("dve", lambda e, h=h: e.max(out=cval[:, h, 0:8], in_=cand[:, h, :]), reads=["cand"], writes=[("c1", h)])
                    for h in range(8):
                        ph.op("dve", lambda e, h=h: e.match_replace(out=cand2[:, h, :], in_to_replace=cval[:, h, 0:8], in_values=cand[:, h, :], imm_value=-1e30),
                              reads=["cand", ("c1", h)], writes=[("cand2", h)])
                    for h in range(8):
                        ph.op("dve", lambda e, h=h: e.max(out=cval[:, h, 8:16], in_=cand2[:, h, :]), reads=[("cand2", h)], writes=[("c2", h)])
                    for h in range(8):
                        ph.op("dve", lambda e, h=h: e.max_index(out=cidx[:, h, 0:8], in_max=cval[:, h, 0:8], in_values=cand[:, h, :]),
                              reads=["cand", ("c1", h)], writes=[("ci1", h)])
                    for h in range(8):
                        ph.op("dve", lambda e, h=h: e.max_index(out=cidx[:, h, 8:16], in_max=cval[:, h, 8:16], in_values=cand2[:, h, :]),
                              reads=[("cand2", h), ("c2", h)], writes=[("ci2", h)])
                    allc = [("c1", h) for h in range(8)] + [("c2", h) for h in range(8)]
                    allci = [("ci1", h) for h in range(8)] + [("ci2", h) for h in range(8)]
                    ph.op("pool", lambda e: e.tensor_tensor(out=ex[:], in0=cval[:], in1=cval[:, :, 0:1].to_broadcast([128, 8, 16]), op=ALU.subtract),
                          reads=allc, writes=["ex"])
                    ph.op("act", lambda e: e.activation(out=ex[:], in_=ex[:], func=AF.Exp), reads=["ex"], writes=["ex"])
                    ph.op("dve", lambda e: e.tensor_reduce(out=esum[:], in_=ex[:], axis=AX.X, op=ALU.add), reads=["ex"], writes=["esum"])
                    ph.op("dve", lambda e: e.reciprocal(out=esum[:], in_=esum[:]), reads=["esum"], writes=["esum"])
                    ph.op("pool", lambda e: e.tensor_tensor(out=slot[:, 2, :].rearrange("p (h k) -> p h k", h=8), in0=ex[:],
                                                            in1=esum[:].unsqueeze(2).to_broadcast([128, 8, 16]), op=ALU.mult),
                          reads=["ex", "esum"], writes=["slot_g"])
                    ph.op("pool", lambda e: e.tensor_copy(out=cif[:], in_=cidx[:]), reads=allci, writes=["cif"])
                    cb = cif[:].unsqueeze(3).to_broadcast([128, 8, 16, 16])
                    lo = k16[:, 1, :].unsqueeze(1).unsqueeze(1).to_broadcast([128, 8, 16, 16])
                    hi = k16[:, 2, :].unsqueeze(1).unsqueeze(1).to_broadcast([128, 8, 16, 16])
                    io = k16[:, 0, :].unsqueeze(1).unsqueeze(1).to_broadcast([128, 8, 16, 16])
                    i4 = idxf[:].rearrange("p (h q) k -> p h q k", q=2)
                    ph.op("dve", lambda e, cb=cb, lo=lo: e.tensor_tensor(out=e1[:], in0=cb, in1=lo, op=ALU.is_ge), reads=["cif", "k16b"], writes=["e1"])
                    ph.op("dve", lambda e, cb=cb, hi=hi: e.tensor_tensor(out=e2[:], in0=cb, in1=hi, op=ALU.is_ge), reads=["cif", "k16c"], writes=["e2"])
                    ph.op("dve", lambda e: e.tensor_tensor(out=e1[:], in0=e1[:], in1=e2[:], op=ALU.subtract), reads=["e1", "e2"], writes=["e1"])
                    ph.op("pool", lambda e, i4=i4: e.tensor_tensor(out=e2[:], in0=e1[:], in1=i4[:, :, 0, :].unsqueeze(2).to_broadcast([128, 8, 16, 16]), op=ALU.mult),
                          reads=["e1", "idxf"], writes=["e2"])
                    ph.op("dve", lambda e: e.tensor_reduce(out=slot[:, 0, :].rearrange("p (h k) -> p h k", h=8), in_=e2[:], axis=AX.X, op=ALU.add),
                          reads=["e2"], writes=["slot_r"])
                    ph.op("pool", lambda e, io=io: e.tensor_tensor(out=e2[:], in0=e1[:], in1=io, op=ALU.mult), reads=["e1", "k16a"], writes=["e2"])
                    ph.op("dve", lambda e: e.tensor_reduce(out=i_f[:], in_=e2[:], axis=AX.X, op=ALU.add), reads=["e2"], writes=["i_f"])
                    ph.op("dve", lambda e: e.scalar_tensor_tensor(out=jjf[:], in0=i_f[:], scalar=-16.0, in1=cif[:], op0=ALU.mult, op1=ALU.add),
                          reads=["i_f", "cif"], writes=["jjf"])
                    ph.op("dve", lambda e, io=io: e.tensor_tensor(out=e1[:], in0=jjf[:].unsqueeze(3).to_broadcast([128, 8, 16, 16]), in1=io, op=ALU.is_equal),
                          reads=["jjf", "k16a"], writes=["e1"])
                    ph.op("pool", lambda e, i4=i4: e.tensor_tensor(out=e2[:], in0=e1[:], in1=i4[:, :, 1, :].unsqueeze(2).to_broadcast([128, 8, 16, 16]), op=ALU.mult),
                          reads=["e1", "idxf"], writes=["e2"])
                    ph.op("dve", lambda e: e.tensor_reduce(out=slot[:, 1, :].rearrange("p (h k) -> p h k", h=8), in_=e2[:], axis=AX.X, op=ALU.add),
                          reads=["e2"], writes=["slot_c"])
                    st_ = t % 2

                    def trs(e):
                        last = None
                        for q in range(3):
                            last = e.transpose(out=tp[0][:, q * 128:(q + 1) * 128], in_=slot[:, q, :], identity=ident[:])
                        return last
                    ph.op("pe", trs, reads=["slot_r", "slot_c", "slot_g"], writes=[("tp", 0)])
                    ph.op("act", lambda e, st_=st_: e.activation(out=slT[st_][:], in_=tp[0][:, 0:384].rearrange("p (a c) -> p a c", a=3), func=AF.Copy),
                          reads=[("tp", 0)], writes=[("slT", st_)])
                    ph.op("sp", lambda e, st_=st_, tok0=tok0: e.dma_start(out=slots_d[:, :, tok0:tok0 + 128].rearrange("q p n -> p q n"), in_=slT[st_][:]),
                          reads=[("slT", st_)], writes=[("slots_d", tok0)], dma=True, semkey=("st", "slT", st_))
            ph.emit()
        if stop_after == "D":
            _dbg_copy3(nc, dbg, slots_d)
            return nc

        with ExitStack() as ph_es:
            ph = Phase(nc, "E")
            def sbp(nm, shape, dt=F32):
                return ph_es.enter_context(nc.sbuf_tensor("e_" + nm, list(shape), dt))
            def psp(nm, shape, dt=F32):
                return ph_es.enter_context(nc.psum_tensor("e_" + nm, list(shape), dt))
            slT = [sbp(f"slT{i}", [128, 3, 128]) for i in range(2)]
            Ab = [sbp(f"Ab{i}", [128, 128], BF16) for i in range(8)]
            Bb = [sbp(f"Bb{i}", [128, 128], BF16) for i in range(8)]
            Gs = [sbp(f"Gs{i}", [128, 32, 128, 4], BF16) for i in range(2)]
            Gp = [psp(f"Gp{i}", [128, 512]) for i in range(4)]
            for t in range(T // 128):
                s = t % 2
                ph.op("sp", lambda e, s=s, t=t: e.dma_start(out=slT[s][:], in_=slots_d[:, :, t * 128:(t + 1) * 128].rearrange("q p n -> p q n")),
                      writes=[("slT", s)], dma=True)
                for n4 in range(32):
                    bk = n4 % 4
                    for q in range(4):
                        n = n4 * 4 + q
                        a_ = n % 8
                        ph.op("dve", lambda e, s=s, n=n, a_=a_: e.tensor_scalar(out=Ab[a_][:], in0=iota_bf[:], scalar1=slT[s][:, 0, n:n + 1],
                                                                               scalar2=slT[s][:, 2, n:n + 1], op0=ALU.is_equal, op1=ALU.mult),
                              reads=[("slT", s)], writes=[("Ab", a_)])
                        ph.op("dve", lambda e, s=s, n=n, a_=a_: e.tensor_scalar(out=Bb[a_][:], in0=iota_bf[:], scalar1=slT[s][:, 1, n:n + 1],
                                                                               scalar2=None, op0=ALU.is_equal),
                              reads=[("slT", s)], writes=[("Bb", a_)])
                        ph.op("pe", lambda e, a_=a_, bk=bk, q=q: e.matmul(Gp[bk][:, q * 128:(q + 1) * 128], lhsT=Bb[a_][:], rhs=Ab[a_][:], start=True, stop=True),
                              reads=[("Ab", a_), ("Bb", a_)], writes=[("Gp", bk)])
                    ph.op("act", lambda e, s=s, n4=n4, bk=bk: e.activation(
                        out=Gs[s][:, :, n4 * 4:n4 * 4 + 4, :],
                        in_=Gp[bk][:].rearrange("p (q g r) -> p g q r", q=4, g=32, r=4), func=AF.Copy),
                        reads=[("Gp", bk)], writes=[("Gs", s)])
                for gq in range(4):
                    for g2 in range(2):
                        gi = gq * 2 + g2
                        ph.op("sp", lambda e, s=s, t=t, gi=gi: e.dma_start(out=G_ds[gi][:, :, t * 128:(t + 1) * 128, :],
                                                                          in_=Gs[s][:, gi * 4:(gi + 1) * 4, :, :]),
                              reads=[("Gs", s)], writes=[("G_d", t, gi)], dma=True, semkey=("st", "Gs", s, gq))
            ph.emit()

        if stop_after == "E":
            return nc
        y2_d = nc.dram_tensor("y2_d", [T, D], F32).ap()
        RGV = 2
        for hb in range(T // HB):
            with ExitStack() as ph_es:
                ph = Phase(nc, f"F{hb}")
                def sbp(nm, shape, dt=F32):
                    return ph_es.enter_context(nc.sbuf_tensor(f"f{hb}_" + nm, list(shape), dt))
                def psp(nm, shape, dt=F32):
                    return ph_es.enter_context(nc.psum_tensor(f"f{hb}_" + nm, list(shape), dt))
                h2T = sbp("h2T", [128, 16, HB], BF16)
                acc = sbp("acc", [128, HB // 128, D])
                ub = [sbp(f"ub{i}", [128, D], BF16) for i in range(4)]
                uT = [sbp(f"uT{i}", [128, 16, 128], BF16) for i in range(4)]
                vb = [sbp(f"vb{i}", [128, D], BF16) for i in range(4)]
                Gg = [sbp(f"Gg{i}", [128, HB, 4], BF16) for i in range(2)]
                gl = [sbp(f"gl{i}", [128, HB]) for i in range(2)]
                W = [sbp(f"W{i}", [128, HB], BF16) for i in range(4)]
                tpu = [psp(f"tpu{i}", [128, 1024], BF16) for i in range(2)]
                actp = psp("actp", [128, HB])
                yp = [psp(f"yp{i}", [128, 1024]) for i in range(2)]
                ph.op("sp", lambda e: e.dma_start(out=h2T[:], in_=h2T_d[:, :, hb * HB:(hb + 1) * HB].rearrange("k p n -> p k n")),
                      writes=["h2T"], dma=True)
                nch = (F_GROUPS * RGV) if F_GROUPS else NEXP_CH
                ngroups = nch // RGV
                ypc = [0]

                def loadu(r):
                    if r >= nch:
                        return
                    rg, q4 = divmod(r, 4)
                    if q4 == 0:
                        ph.op("sp", lambda e, gs_=rg % 2, rg=rg: e.dma_start(out=Gg[gs_][:], in_=G_ds[rg // 4][:, rg % 4, hb * HB:(hb + 1) * HB, :]),
                              writes=[("Gg", rg % 2)], dma=True)
                    ph.op("pool", lambda e, r=r: e.dma_start(out=ub[r % 4][:], in_=peer_u[r * 128:(r + 1) * 128, :]), writes=[("ub", r % 4)], dma=True)

                def loadv(r):
                    if r >= nch:
                        return
                    ph.op("pool", lambda e, r=r: e.dma_start(out=vb[r % 4][:], in_=peer_v[r * 128:(r + 1) * 128, :]), writes=[("vb", r % 4)], dma=True)

                def front(r):
                    if r >= nch or F_MODE == 'l':
                        return
                    u_ = r % 4
                    for kg in range(2):
                        def trs(e, u_=u_, kg=kg):
                            last = None
                            for kk in range(8):
                                k = 8 * kg + kk
                                last = e.transpose(out=tpu[kg][:, kk * 128:(kk + 1) * 128], in_=ub[u_][:, k * 128:(k + 1) * 128], identity=ident_bf[:])
                            return last
                        ph.op("pe", trs, reads=[("ub", u_)], writes=[("tpu", kg)])
                        if kg == 0:
                            ph.op("act", lambda e, u_=u_, kg=kg: e.activation(
                                out=uT[u_][:, 8 * kg:8 * kg + 8, :], in_=tpu[kg][:].rearrange("p (a c) -> p a c", a=8), func=AF.Copy),
                                reads=[("tpu", kg)], writes=[("uT", u_, kg)])
                        else:
                            ph.op("dve", lambda e, u_=u_, kg=kg: e.tensor_copy(
                                out=uT[u_][:, 8 * kg:8 * kg + 8, :], in_=tpu[kg][:].rearrange("p (a c) -> p a c", a=8)),
                                reads=[("tpu", kg)], writes=[("uT", u_, kg)])

                def mid(r):
                    if F_MODE == 'l':
                        return
                    u_ = r % 4
                    s2 = r % 2
                    rg, q4 = divmod(r, 4)
                    gs_ = rg % 2
                    def mm(e, u_=u_):
                        last = None
                        for k in range(16):
                            for tg in range(HB // 512):
                                last = e.matmul(actp[:, tg * 512:(tg + 1) * 512], lhsT=uT[u_][:, k, :], rhs=h2T[:, k, tg * 512:(tg + 1) * 512],
                                                start=(k == 0), stop=(k == 15))
                        return last
                    ph.op("pe", mm, reads=[("uT", u_, 0), ("uT", u_, 1), "h2T"], writes=[("actp", tg) for tg in range(HB // 512)])
                    ph.op("act", lambda e, s2=s2: e.activation(out=gl[s2][:], in_=actp[:], func=AF.Gelu_apprx_tanh),
                          reads=[("actp", tg) for tg in range(HB // 512)], writes=[("gl", s2)])
                    ph.op("dve", lambda e, s2=s2, u_=u_, gs_=gs_, q4=q4: e.tensor_tensor(out=W[u_][:], in0=gl[s2][:], in1=Gg[gs_][:, :, q4], op=ALU.mult),
                          reads=[("gl", s2), ("Gg", gs_)], writes=[("W", u_)])

                def vphase(grp):
                    if F_MODE == 'l':
                        return
                    wbase = (grp % 2) * RGV
                    for t in range(HB // 128):
                        for dh in range(2):
                            yb = ypc[0] % 2
                            ypc[0] += 1

                            def vmm(e, t=t, dh=dh, yb=yb, wbase=wbase):
                                last = None
                                for qq in range(RGV):
                                    for dg in range(2):
                                        c0 = dh * 1024 + dg * 512
                                        last = e.matmul(yp[yb][:, dg * 512:(dg + 1) * 512], lhsT=W[wbase + qq][:, t * 128:(t + 1) * 128],
                                                        rhs=vb[wbase + qq][:, c0:c0 + 512], start=(qq == 0), stop=(qq == RGV - 1))
                                return last
                            ph.op("pe", vmm, reads=[("W", wbase + qq) for qq in range(RGV)] + [("vb", wbase + qq) for qq in range(RGV)],
                                  writes=[("yp", yb)])
                            if grp == 0:
                                ph.op("dve", lambda e, t=t, dh=dh, yb=yb: e.tensor_copy(out=acc[:, t, dh * 1024:(dh + 1) * 1024], in_=yp[yb][:]),
                                      reads=[("yp", yb)], writes=[("acc", t, dh)])
                            else:
                                ph.op("dve", lambda e, t=t, dh=dh, yb=yb: e.tensor_tensor(out=acc[:, t, dh * 1024:(dh + 1) * 1024], in0=yp[yb][:],
                                                                                        in1=acc[:, t, dh * 1024:(dh + 1) * 1024], op=ALU.add),
                                      reads=[("yp", yb), ("acc", t, dh)], writes=[("acc", t, dh)])

                for r in range(4):
                    loadu(r)
                front(0)
                front(1)
                for grp in range(ngroups):
                    r0, r1 = 2 * grp, 2 * grp + 1
                    mid(r0)
                    loadv(r0)
                    loadv(r1)
                    loadu(r0 + 4)
                    loadu(r1 + 4)
                    front(r0 + 2)
                    front(r1 + 2)
                    if grp > 0:
                        vphase(grp - 1)
                    mid(r1)
                vphase(ngroups - 1)
                for t in range(HB // 128 if F_MODE == '' else 0):
                    ph.op("sp", lambda e, t=t: e.dma_start(out=y2_d[hb * HB + t * 128:hb * HB + (t + 1) * 128, :], in_=acc[:, t, :]),
                          reads=[("acc", t, 0), ("acc", t, 1)], writes=[("y2_d", t)], dma=True, semkey=("st", "acc", t % 2))
                ph.emit()
        if stop_after == "F":
            _dbg_copy(nc, dbg, y2_d, rows=T)
            return nc

        with ExitStack() as ph_es:
            ph = Phase(nc, "G")
            def sbp(nm, shape, dt=F32):
                return ph_es.enter_context(nc.sbuf_tensor("gq_" + nm, list(shape), dt))
            gg = sbp("gg", [128, D])
            yt = [sbp(f"yt{i}", [128, D]) for i in range(2)]
            xt = [sbp(f"xt{i}", [128, D]) for i in range(2)]
            junk = sbp("junk", [128, D], BF16)
            ss = sbp("ss", [128, 4])
            rs = sbp("rs", [128, 4])
            ph.op("sp", lambda e: e.dma_start(out=gg[:], in_=modbc_d[:, 5 * D:6 * D]), writes=["gg"], dma=True)
            for t in range(T // 128):
                s = t % 2
                ph.op("sp", lambda e, s=s, t=t: e.dma_start(out=yt[s][:], in_=y2_d[t * 128:(t + 1) * 128, :]), writes=[("yt", s)], dma=True)
                ph.op("sp", lambda e, s=s, t=t: e.dma_start(out=xt[s][:], in_=x1_d[t * 128:(t + 1) * 128, :]), writes=[("xt", s)], dma=True)
                ph.op("act", lambda e, s=s: e.activation(out=junk[:], in_=yt[s][:], func=AF.Square, accum_out=ss[:, s:s + 1]),
                      reads=[("yt", s)], writes=["junk", ("ss", s)])
                ph.op("act", lambda e, s=s: e.activation(out=rs[:, s:s + 1], in_=ss[:, s:s + 1], func=AF.Sqrt, scale=1.0 / D, bias=EPS),
                      reads=[("ss", s)], writes=[("rs", s)])
                ph.op("dve", lambda e, s=s: e.reciprocal(out=rs[:, 2 + s:3 + s], in_=rs[:, s:s + 1]), reads=[("rs", s)], writes=[("rs2", s)])
                ph.op("dve", lambda e, s=s: e.scalar_tensor_tensor(out=yt[s][:], in0=yt[s][:], scalar=rs[:, 2 + s:3 + s], in1=gg[:],
                                                                  op0=ALU.mult, op1=ALU.mult),
                      reads=[("yt", s), ("rs2", s), "gg"], writes=[("yt", s)])
                ph.op("pool", lambda e, s=s: e.tensor_tensor(out=yt[s][:], in0=yt[s][:], in1=xt[s][:], op=ALU.add),
                      reads=[("yt", s), ("xt", s)], writes=[("yt", s)])
                ph.op("sp", lambda e, s=s, t=t: e.dma_start(out=out[t * 128:(t + 1) * 128, :], in_=yt[s][:]),
                      reads=[("yt", s)], writes=[("out", t)], dma=True, semkey=("st", "yt", s))
            ph.emit()
    return nc


def _dbg_copy3(nc, dbg, slots_d):
    with ExitStack() as es:
        buf = es.enter_context(nc.sbuf_tensor("dbgbuf3", [128, T], F32))
        s1 = es.enter_context(nc.semaphore("dbg3_s1"))
        with nc.Block() as block:
            @block.sync
            def _(e):
                n = 0
                for q in range(3):
                    e.dma_start(out=buf[:], in_=slots_d[q]).then_inc(s1, 16)
                    n += 16
                    e.wait_ge(s1, n)
                    e.dma_start(out=dbg[q * 128:(q + 1) * 128, :], in_=buf[:]).then_inc(s1, 16)
                    n += 16
                    e.wait_ge(s1, n)


def _dbg_copy(nc, dbg, src, rows):
    with ExitStack() as es:
        buf = es.enter_context(nc.sbuf_tensor("dbgbuf", [128, D], F32))
        s1 = es.enter_context(nc.semaphore("dbg_s1"))
        with nc.Block() as block:
            @block.sync
            def _(e):
                n = 0
                for t in range(rows // 128):
                    e.dma_start(out=buf[:], in_=src[t * 128:(t + 1) * 128, :]).then_inc(s1, 16)
                    n += 16
                    e.wait_ge(s1, n)
                    e.dma_start(out=dbg[t * 128:(t + 1) * 128, :], in_=buf[:]).then_inc(s1, 16)
                    n += 16
                    e.wait_ge(s1, n)


def _make_cols(inp, b, j):
    cols = np.zeros((128, NCOL), np.float32)

    def put(c0, vec):
        v = np.asarray(vec, np.float32).reshape(-1, 128)
        cols[:, c0:c0 + v.shape[0]] = v.T
    put(C_C, inp["c"][b])
    for tap in range(4):
        put(C_LW + tap * 8, inp["lru_conv_w"][0, tap])
    put(C_LB, inp["lru_conv_b"][0])
    put(C_BA, inp["lru_ba"][0])
    put(C_BX, inp["lru_bx"][0])
    put(C_LAM, inp["lru_lambda"][0])
    for tap in range(31):
        put(C_CW + tap * 8, inp["conf_dw_w"][0, tap])
    put(C_CB, inp["conf_dw_b"][0])
    put(C_LG, inp["conf_ln_g"][0])
    put(C_LNB, inp["conf_ln_b"][0])
    nvalid_blocks = (T * j) // TB
    for blk in range(NPRE // TB):
        cols[:, C_FL + blk] = 1.0 if blk >= (NPRE // TB - nvalid_blocks) else 0.0
    return cols


def make_in_maps(inp):
    x = np.ascontiguousarray(inp["x"], dtype=np.float32)
    shared = {
        "ident": np.eye(128, dtype=np.float32),
        "iota": np.tile(np.arange(128, dtype=np.float32)[None, :], (128, 1)),
        "w_mod": np.ascontiguousarray(inp["w_mod"][0]),
        "b_mod": np.ascontiguousarray(inp["b_mod"][0]),
        "g_pre_mix": np.ascontiguousarray(inp["g_pre_mix"][0]),
        "g_post_mix": np.ascontiguousarray(inp["g_post_mix"][0]),
        "g_pre_ffn": np.ascontiguousarray(inp["g_pre_ffn"][0]),
        "g_post_ffn": np.ascontiguousarray(inp["g_post_ffn"][0]),
        "w_in": np.ascontiguousarray(inp["w_in"][0]),
        "lru_wa": np.ascontiguousarray(inp["lru_wa"][0]),
        "lru_wx": np.ascontiguousarray(inp["lru_wx"][0]),
        "w_out": np.ascontiguousarray(inp["w_out"][0]),
        "peer_wq": np.ascontiguousarray(inp["peer_wq"][0]),
        "peer_sk": np.ascontiguousarray(inp["peer_subkeys"][0].reshape(16, 128, 128)),
        "peer_u": np.ascontiguousarray(inp["peer_u"][0]),
        "peer_v": np.ascontiguousarray(inp["peer_v"][0]),
    }
    maps = []
    for c in range(8):
        b, j = divmod(c, 4)
        m = dict(shared)
        m["x_main"] = np.ascontiguousarray(x[b, T * j:T * (j + 1)])
        pre = np.zeros((NPRE, D), np.float32)
        nv = T * j
        if nv:
            pre[NPRE - nv:] = x[b, 0:nv]
        m["x_pre"] = pre
        m["cols"] = _make_cols(inp, b, j)
        maps.append(m)
    return maps


def kernel(**inputs):
    nc = build_program()
    maps = make_in_maps(inputs)
    res = run_bass_kernel_spmd(nc, maps, core_ids=list(range(8)))
    out = np.empty((2, 8192, D), np.float32)
    for c in range(8):
        b, j = divmod(c, 4)
        out[b, T * j:T * (j + 1)] = res.results[c]["out"]
    return out
```

```python
import numpy as np
from contextlib import ExitStack
import concourse.bass as bass
import concourse.mybir as mybir
from concourse.bass_utils import run_bass_kernel_spmd

F32 = mybir.dt.float32
BF16 = mybir.dt.bfloat16
U32 = mybir.dt.uint32
AF = mybir.ActivationFunctionType
ALU = mybir.AluOpType
AX = mybir.AxisListType

D = 2048
KC = 16
T = 2048
NPRE = 6144
TB = 1024
EPS = 1e-6
NEXP_CH = 128
RG = 4
HB = 1024
F_GROUPS = 0
F_MODE = ''

C_C = 0
C_LW = 16
C_LB = 48
C_BA = 56
C_BX = 64
C_LAM = 72
C_CW = 80
C_CB = 328
C_LG = 336
C_LNB = 344
C_FL = 352
NCOL = 358


class _Op:
    __slots__ = ("eng", "fn", "deps", "need_inc", "dma", "semkey", "token")


class Phase:
    ENG = ("sp", "pool", "act", "dve", "pe")

    gpool = None

    def __init__(self, nc, name):
        self.nc = nc
        self.name = name
        self.ops = []
        self.lw = {}
        self.rd = {}

    def op(self, eng, fn, reads=(), writes=(), dma=False, semkey=None):
        o = _Op()
        o.eng = eng
        o.fn = fn
        o.dma = dma
        o.need_inc = dma
        o.token = None
        o.semkey = semkey if semkey is not None else (("dma", writes[0]) if dma else None)
        deps = []
        for k in reads:
            w = self.lw.get(k)
            if w is not None:
                deps.append(w)
        for k in writes:
            w = self.lw.get(k)
            if w is not None:
                deps.append(w)
            deps.extend(self.rd.get(k, ()))
        o.deps = []
        seen = set()
        for d in deps:
            if id(d) not in seen and d is not o:
                seen.add(id(d))
                o.deps.append(d)
                d.need_inc = True
        for k in writes:
            self.lw[k] = o
            self.rd[k] = []
        for k in reads:
            self.rd.setdefault(k, []).append(o)
        self.ops.append(o)
        return o

    def emit(self):
        nc = self.nc
        gp = self.gpool
        local = {}
        for o in self.ops:
            if o.dma:
                k = o.semkey
                local[k] = local.get(k, 0) + 16
                o.token = (k, local[k])
            elif o.need_inc:
                k = ("eng", o.eng)
                local[k] = local.get(k, 0) + 1
                o.token = (k, local[k])
        swkeys = set(o.semkey for o in self.ops if o.dma and o.eng == "pool")
        slot = {}
        nhw = nsw = 0
        for k in local:
            if k[0] == "eng":
                slot[k] = self.ENG.index(k[1])
                continue
            lst = gp["sw"] if k in swkeys else gp["hw"]
            n_ = nsw if k in swkeys else nhw
            if n_ >= len(lst):
                lst.append(None)
            if k in swkeys:
                nsw += 1
            else:
                nhw += 1
            slot[k] = ("sw" if k in swkeys else "hw", n_)
        def getslot(sl):
            if isinstance(sl, int):
                while len(gp["sems"]) < 5:
                    i = len(gp["sems"])
                    gp["sems"].append(gp["stack"].enter_context(nc.semaphore(f"gsem{i}")))
                    gp["counts"].append(0)
                return sl
            kind, n_ = sl
            lst = gp[kind]
            if lst[n_] is None:
                i = len(gp["sems"])
                gp["sems"].append(gp["stack"].enter_context(nc.semaphore(f"gsem{i}")))
                gp["counts"].append(0)
                lst[n_] = i
            return lst[n_]
        getslot(0)
        slot = {k: getslot(v) for k, v in slot.items()}
        base = {k: gp["counts"][slot[k]] for k in local}
        sems = {k: gp["sems"][slot[k]] for k in local}
        by_eng = {e: [o for o in self.ops if o.eng == e] for e in self.ENG}
        with nc.Block() as block:
            reg = {"sp": block.sync, "pool": block.gpsimd, "act": block.scalar,
                   "dve": block.vector, "pe": block.tensor}
            for ename in self.ENG:
                ops = by_eng[ename]

                def body(e, ops=ops):
                    waited = {}
                    for o in ops:
                        need = {}
                        for d in o.deps:
                            if d.eng == "pe" and o.eng == "pe" and not d.dma and not o.dma:
                                continue
                            s, v = d.token
                            if need.get(s, 0) < v:
                                need[s] = v
                        for s, v in need.items():
                            if waited.get(s, 0) < v:
                                e.wait_ge(sems[s], base[s] + v)
                                waited[s] = v
                        ins = o.fn(e)
                        if o.dma:
                            ins.then_inc(sems[o.semkey], 16)
                        elif o.need_inc:
                            ins.then_inc(sems[("eng", o.eng)], 1)
                    for s, v in local.items():
                        if waited.get(s, 0) < v:
                            e.wait_ge(sems[s], base[s] + v)
                reg[ename](body)
        for k, v in local.items():
            gp["counts"][slot[k]] += v


def _col(cols, c):
    return cols[:, c:c + 1]


def build_program(stop_after=None):
    nc = bass.Bass("TRN2", target_bir_lowering=False)

    def din(name, shape, dt=F32):
        return nc.dram_tensor(name, list(shape), dt, kind="ExternalInput").ap()

    x_main = din("x_main", [T, D])
    x_pre = din("x_pre", [NPRE, D])
    cols_d = din("cols", [128, NCOL])
    ident_d = din("ident", [128, 128])
    iota_d = din("iota", [128, 128])
    w_mod = din("w_mod", [D, 6 * D])
    b_mod = din("b_mod", [6 * D])
    g_pre_mix = din("g_pre_mix", [D])
    g_post_mix = din("g_post_mix", [D])
    g_pre_ffn = din("g_pre_ffn", [D])
    g_post_ffn = din("g_post_ffn", [D])
    w_in = din("w_in", [D, 2 * D])
    lru_wa = din("lru_wa", [8, 128, 128])
    lru_wx = din("lru_wx", [8, 128, 128])
    w_out = din("w_out", [D, D])
    peer_wq = din("peer_wq", [D, D])
    peer_sk = din("peer_sk", [16, 128, 128])
    peer_u = din("peer_u", [16384, D])
    peer_v = din("peer_v", [16384, D])
    out = nc.dram_tensor("out", [T, D], F32, kind="ExternalOutput").ap()

    modbc_d = nc.dram_tensor("modbc_d", [128, 6 * D], F32).ap()
    catT_d = nc.dram_tensor("catT_d", [16, 128, T], BF16).ap()
    x1_d = nc.dram_tensor("x1_d", [T, D], F32).ap()
    h2T_d = nc.dram_tensor("h2T_d", [16, 128, T], BF16).ap()
    slots_d = nc.dram_tensor("slots_d", [3, 128, T], F32).ap()
    G_ds = [nc.dram_tensor(f"G_d{i}", [128, 4, T, 4], BF16).ap() for i in range(8)]

    dbg = None
    if stop_after is not None:
        dbg = nc.dram_tensor("dbg", [T, D], F32, kind="ExternalOutput").ap()

    with ExitStack() as outer:
        Phase.gpool = {"sems": [], "counts": [], "stack": outer, "hw": [], "sw": []}

        def sb(name, shape, dt=F32):
            return outer.enter_context(nc.sbuf_tensor("g_" + name, list(shape), dt))
        cols = sb("cols", [128, NCOL])
        ident = sb("ident", [128, 128])
        iota = sb("iota", [128, 128])
        ones = sb("ones", [128, 128])
        ident_bf = sb("ident_bf", [128, 128], BF16)
        iota_bf = sb("iota_bf", [128, 128], BF16)
        carry = sb("carry", [128, 8, 3])
        zcarry = sb("zcarry", [128, 8, 30])
        state = sb("state", [128, 8])
        clc = sb("clc", [128, 16])

        with ExitStack() as ph_es:
            ph = Phase(nc, "A")
            def sbp(name, shape, dt=F32, es=ph_es):
                return es.enter_context(nc.sbuf_tensor("a_" + name, list(shape), dt))
            def psp(name, shape, es=ph_es):
                return es.enter_context(nc.psum_tensor("a_" + name, list(shape), F32))
            caT = sbp("caT", [128, 16])
            caTb = sbp("caTb", [128, 16, 128])
            wm = [sbp(f"wm{i}", [128, 16, 512]) for i in range(2)]
            bmb = [sbp(f"bmb{i}", [128, 512]) for i in range(2)]
            gb = [sbp(f"gb{i}", [128, 512]) for i in range(2)]
            mo = [sbp(f"mo{i}", [128, 512]) for i in range(2)]
            tmpA = sbp("tmpA", [128, 16])
            mps = [psp(f"mps{i}", [128, 512]) for i in range(2)]

            ph.op("sp", lambda e: e.dma_start(out=cols[:], in_=cols_d), writes=["cols"], dma=True)
            ph.op("sp", lambda e: e.dma_start(out=ident[:], in_=ident_d), writes=["ident"], dma=True)
            ph.op("sp", lambda e: e.dma_start(out=iota[:], in_=iota_d), writes=["iota"], dma=True)
            ph.op("pool", lambda e: e.memset(ones[:], 1.0), writes=["ones"])
            ph.op("dve", lambda e: e.tensor_copy(out=ident_bf[:], in_=ident[:]), reads=["ident"], writes=["ident_bf"])
            ph.op("dve", lambda e: e.tensor_copy(out=iota_bf[:], in_=iota[:]), reads=["iota"], writes=["iota_bf"])
            ph.op("pool", lambda e: e.memset(carry[:], 0.0), writes=["carry"])
            ph.op("pool", lambda e: e.memset(zcarry[:], 0.0), writes=["zcarry"])
            ph.op("pool", lambda e: e.memset(state[:], 0.0), writes=["state"])
            ph.op("act", lambda e: e.activation(out=caT[:], in_=cols[:, C_C:C_C + 16], func=AF.Silu),
                  reads=["cols"], writes=["caT"])
            ph.op("act", lambda e: e.activation(out=tmpA[:, 0:8], in_=cols[:, C_LAM:C_LAM + 8], func=AF.Exp, scale=-1.0),
                  reads=["cols"], writes=["tmpA"])
            ph.op("act", lambda e: e.activation(out=tmpA[:, 8:16], in_=tmpA[:, 0:8], func=AF.Ln, bias=1.0),
                  reads=["tmpA"], writes=["tmpA2"])
            ph.op("dve", lambda e: e.tensor_scalar(out=clc[:, 0:8], in0=tmpA[:, 8:16], scalar1=-8.0, scalar2=None, op0=ALU.mult),
                  reads=["tmpA2"], writes=["clc0"])
            ph.op("dve", lambda e: e.tensor_scalar(out=clc[:, 8:16], in0=tmpA[:, 8:16], scalar1=-16.0, scalar2=None, op0=ALU.mult),
                  reads=["tmpA2"], writes=["clc1"])
            for k in range(16):
                ph.op("dve", lambda e, k=k: e.tensor_copy(out=caTb[:, k, :], in_=caT[:, k:k + 1].to_broadcast([128, 128])),
                      reads=["caT"], writes=[("caTb", k)])
            gsrc = {1: g_pre_mix, 2: g_post_mix, 4: g_pre_ffn, 5: g_post_ffn}
            for n in range(24):
                s = n % 2
                sec = n // 4
                cb = (n % 4) * 512
                ph.op("sp", lambda e, n=n, s=s: e.dma_start(
                    out=wm[s][:], in_=w_mod.rearrange("(k p) c -> p k c", p=128)[:, :, n * 512:(n + 1) * 512]),
                    writes=[("wm", s)], dma=True)
                ph.op("sp", lambda e, n=n, s=s: e.dma_start(
                    out=bmb[s][:], in_=b_mod[n * 512:(n + 1) * 512].partition_broadcast(128)),
                    writes=[("bmb", s)], dma=True)
                if sec in gsrc:
                    ph.op("sp", lambda e, s=s, sec=sec, cb=cb: e.dma_start(
                        out=gb[s][:], in_=gsrc[sec][cb:cb + 512].partition_broadcast(128)),
                        writes=[("gb", s)], dma=True)

                def mm(e, s=s):
                    last = None
                    for k in range(16):
                        last = e.matmul(mps[s][:], lhsT=caTb[:, k, :], rhs=wm[s][:, k, :], start=(k == 0), stop=(k == 15))
                    return last
                ph.op("pe", mm, reads=[("wm", s)] + [("caTb", k) for k in range(16)], writes=[("mps", s)])
                if sec in (0, 3):
                    ph.op("dve", lambda e, s=s: e.tensor_tensor(out=mo[s][:], in0=mps[s][:], in1=bmb[s][:], op=ALU.add),
                          reads=[("mps", s), ("bmb", s)], writes=[("mo", s)])
                else:
                    ph.op("dve", lambda e, s=s: e.tensor_tensor(out=bmb[s][:], in0=mps[s][:], in1=bmb[s][:], op=ALU.add),
                          reads=[("mps", s), ("bmb", s)], writes=[("bmb", s)])
                    if sec in (1, 4):
                        ph.op("dve", lambda e, s=s: e.scalar_tensor_tensor(out=mo[s][:], in0=bmb[s][:], scalar=1.0, in1=gb[s][:],
                                                                          op0=ALU.add, op1=ALU.mult),
                              reads=[("bmb", s), ("gb", s)], writes=[("mo", s)])
                    else:
                        ph.op("dve", lambda e, s=s: e.tensor_tensor(out=mo[s][:], in0=bmb[s][:], in1=gb[s][:], op=ALU.mult),
                              reads=[("bmb", s), ("gb", s)], writes=[("mo", s)])
                ph.op("sp", lambda e, n=n, s=s: e.dma_start(out=modbc_d[:, n * 512:(n + 1) * 512], in_=mo[s][:]),
                      reads=[("mo", s)], writes=[("modbc_d", n)], dma=True, semkey=("st", "mo", s))
            ph.emit()

        if stop_after == "A":
            _dbg_copy(nc, dbg, modbc_d[:, 0:D], rows=128)
            return nc

        def mixer_phase(name, xsrc, nblocks, prefix):
            with ExitStack() as ph_es:
                ph = Phase(nc, name)
                def sbp(nm, shape, dt=F32):
                    return ph_es.enter_context(nc.sbuf_tensor(name + "_" + nm, list(shape), dt))
                def psp(nm, shape):
                    return ph_es.enter_context(nc.psum_tensor(name + "_" + nm, list(shape), F32))
                N = TB
                A1bc = sbp("A1bc", [128, D])
                sh1bc = sbp("sh1bc", [128, D])
                xt = [sbp(f"xt{i}", [128, D]) for i in range(2)]
                xn = [sbp(f"xn{i}", [128, D]) for i in range(2)]
                junk = sbp("junk", [128, D], BF16)
                ss = sbp("ss", [128, 4])
                rs = sbp("rs", [128, 4])
                hT = sbp("hT", [128, 16, N], BF16)
                wsl = [sbp(f"wsl{i}", [128, 16, 128], BF16) for i in range(4)]
                wga = sbp("wga", [128, 8, 128])
                wgx = sbp("wgx", [128, 8, 128])
                NBK = dict(xpad=2, xl=3, rr=1, ig=2, aa=2, a2=2, uu=1, hs=1) if prefix else dict(xpad=1, xl=1, rr=1, ig=1, aa=1, a2=1, uu=1, hs=1)
                xpadS = [sbp(f"xpad{j}", [128, N + 3]) for j in range(NBK["xpad"])]
                xpad = xpadS[0]
                cA = sbp("cA", [128, N])
                cB = sbp("cB", [128, N])
                xlS = [sbp(f"xl{j}", [128, N]) for j in range(NBK["xl"])]
                xl = xlS[0]
                rrS = [sbp(f"rr{j}", [128, N]) for j in range(NBK["rr"])]
                rr = rrS[0]
                igS = [sbp(f"ig{j}", [128, N]) for j in range(NBK["ig"])]
                ig = igS[0]
                aaS = [sbp(f"aa{j}", [128, N]) for j in range(NBK["aa"])]
                aa = aaS[0]
                a2S = [sbp(f"a2{j}", [128, N]) for j in range(NBK["a2"])]
                a2 = a2S[0]
                uuS = [sbp(f"uu{j}", [128, N]) for j in range(NBK["uu"])]
                uu = uuS[0]
                hsS = [sbp(f"hs{j}", [128, N]) for j in range(NBK["hs"])]
                hs = hsS[0]
                tp = [psp(f"tp{i}", [128, 512]) for i in range(2)]
                pp = psp("pp", [128, N])
                gpa = psp("gpa", [128, N])
                gpx = psp("gpx", [128, N])
                if not prefix:
                    zpad = sbp("zpad", [128, N + 30])
                    cz = sbp("cz", [128, 8, N])
                    gel = sbp("gel", [128, N])
                    catc = [sbp(f"catc{i}", [128, N], BF16) for i in range(2)]
                    mean = aa
                    rstd = a2
                    sqt = [rr, ig]
                zs = sbp("zs", [128, 32])
                zsg = sbp("zsg", [128, 32])

                ph.op("sp", lambda e: e.dma_start(out=A1bc[:], in_=modbc_d[:, D:2 * D]), writes=["A1bc"], dma=True)
                ph.op("sp", lambda e: e.dma_start(out=sh1bc[:], in_=modbc_d[:, 0:D]), writes=["sh1bc"], dma=True)
                ph.op("sp", lambda e: e.dma_start(out=wga[:], in_=lru_wa.rearrange("h i j -> i h j")), writes=["wga"], dma=True)
                ph.op("sp", lambda e: e.dma_start(out=wgx[:], in_=lru_wx.rearrange("h i j -> i h j")), writes=["wgx"], dma=True)

                plan = []
                for b_ in range(nblocks):
                    for i_ in range(8):
                        plan.append(i_ * 128)
                        if not prefix:
                            plan.append(1024 + i_ * 128)
                    if (prefix and b_ == nblocks - 1) or not prefix:
                        for i_ in range(8):
                            plan.append(2048 + i_ * 128)
                            plan.append(3072 + i_ * 128)
                wctr = [0, 0]

                def issue_loads():
                    while wctr[0] < len(plan) and wctr[0] < wctr[1] + 3:
                        n_ = wctr[0]
                        s_ = n_ % 4
                        c0 = plan[n_]
                        wctr[0] += 1
                        ph.op("pool", lambda e, s_=s_, c0=c0: e.dma_start(
                            out=wsl[s_][:], in_=w_in.rearrange("(k p) c -> p k c", p=128)[:, :, c0:c0 + 128]),
                            writes=[("wsl", s_)], dma=True)

                def load_slice(c0):
                    issue_loads()
                    n_ = wctr[1]
                    assert plan[n_] == c0, (n_, plan[n_], c0)
                    wctr[1] += 1
                    return n_ % 4

                def proj(dst, dname, s, t0, n):
                    for g0 in range(0, n, 512):
                        gn = min(512, n - g0)

                        def mm(e, g0=g0, gn=gn):
                            last = None
                            for k in range(16):
                                last = e.matmul(dst[:, g0:g0 + gn], lhsT=wsl[s][:, k, :], rhs=hT[:, k, t0 + g0:t0 + g0 + gn],
                                                start=(k == 0), stop=(k == 15))
                            return last
                        ph.op("pe", mm, reads=[("wsl", s), "hT"], writes=[(dname, g0)])
                    issue_loads()

                tctr = [0]
                for b in range(nblocks):
                    for t in range(N // 128):
                        s = tctr[0] % 2
                        tctr[0] += 1
                        r0 = b * N + t * 128
                        ph.op("sp", lambda e, s=s, r0=r0: e.dma_start(out=xt[s][:], in_=xsrc[r0:r0 + 128, :]),
                              writes=[("xt", s)], dma=True)
                        ph.op("act", lambda e, s=s: e.activation(out=junk[:], in_=xt[s][:], func=AF.Square, accum_out=ss[:, s:s + 1]),
                              reads=[("xt", s)], writes=["junk", ("ss", s)])
                        ph.op("act", lambda e, s=s: e.activation(out=rs[:, s:s + 1], in_=ss[:, s:s + 1], func=AF.Sqrt, scale=1.0 / D, bias=EPS),
                              reads=[("ss", s)], writes=[("rs", s)])
                        ph.op("dve", lambda e, s=s: e.reciprocal(out=rs[:, 2 + s:3 + s], in_=rs[:, s:s + 1]),
                              reads=[("rs", s)], writes=[("rs2", s)])
                        ph.op("dve", lambda e, s=s: e.scalar_tensor_tensor(out=xn[s][:], in0=xt[s][:], scalar=rs[:, 2 + s:3 + s], in1=A1bc[:],
                                                                          op0=ALU.mult, op1=ALU.mult),
                              reads=[("xt", s), ("rs2", s), "A1bc"], writes=[("xn", s)])
                        ph.op("pool", lambda e, s=s: e.tensor_tensor(out=xn[s][:], in0=xn[s][:], in1=sh1bc[:], op=ALU.add),
                              reads=[("xn", s), "sh1bc"], writes=[("xn", s)])
                        for kg in range(4):
                            bk = kg % 2

                            def trs(e, s=s, kg=kg, bk=bk):
                                last = None
                                for kk in range(4):
                                    k = 4 * kg + kk
                                    last = e.transpose(out=tp[bk][:, kk * 128:(kk + 1) * 128], in_=xn[s][:, k * 128:(k + 1) * 128],
                                                       identity=ident[:])
                                return last
                            ph.op("pe", trs, reads=[("xn", s), "ident"], writes=[("tp", bk)])
                            eng = "act" if kg % 2 == 0 else "dve"
                            if eng == "act":
                                ph.op("act", lambda e, kg=kg, bk=bk, t=t: e.activation(
                                    out=hT[:, 4 * kg:4 * kg + 4, t * 128:(t + 1) * 128],
                                    in_=tp[bk][:].rearrange("p (a c) -> p a c", a=4), func=AF.Copy),
                                    reads=[("tp", bk)], writes=["hT"])
                            else:
                                ph.op("dve", lambda e, kg=kg, bk=bk, t=t: e.tensor_copy(
                                    out=hT[:, 4 * kg:4 * kg + 4, t * 128:(t + 1) * 128],
                                    in_=tp[bk][:].rearrange("p (a c) -> p a c", a=4)),
                                    reads=[("tp", bk)], writes=["hT"])
                    fl = _col(cols, C_FL + b) if prefix else None

                    def st0(i, b=b, fl=fl):
                        j = i % NBK["xpad"]
                        sx = load_slice(i * 128)
                        proj(pp, "pp", sx, 0, N)
                        ph.op("act", lambda e, j=j: e.activation(out=xpadS[j][:, 3:3 + N], in_=pp[:], func=AF.Copy),
                              reads=[("pp", 0), ("pp", 512)], writes=[("xpad", j)])
                        ph.op("pool", lambda e, i=i, j=j: e.tensor_copy(out=xpadS[j][:, 0:3], in_=carry[:, i, :]),
                              reads=[("carry", i)], writes=[("xpadc", j)])

                    def st1(i, b=b, fl=fl):
                        jx = i % NBK["xpad"]
                        j = i % NBK["xl"]
                        xp = xpadS[jx]
                        rk = [("xpad", jx), ("xpadc", jx)]
                        ph.op("dve", lambda e, i=i, xp=xp: e.tensor_scalar(out=cA[:], in0=xp[:, 0:N], scalar1=_col(cols, C_LW + i),
                                                                          scalar2=_col(cols, C_LB + i), op0=ALU.mult, op1=ALU.add),
                              reads=rk, writes=["cA"])
                        ph.op("dve", lambda e, i=i, xp=xp: e.scalar_tensor_tensor(out=cB[:], in0=xp[:, 1:1 + N], scalar=_col(cols, C_LW + 8 + i),
                                                                                 in1=cA[:], op0=ALU.mult, op1=ALU.add),
                              reads=rk + ["cA"], writes=["cB"])
                        ph.op("dve", lambda e, i=i, xp=xp: e.scalar_tensor_tensor(out=cA[:], in0=xp[:, 2:2 + N], scalar=_col(cols, C_LW + 16 + i),
                                                                                 in1=cB[:], op0=ALU.mult, op1=ALU.add),
                              reads=rk + ["cB"], writes=["cA"])
                        ph.op("dve", lambda e, i=i, xp=xp, j=j: e.scalar_tensor_tensor(out=xlS[j][:], in0=xp[:, 3:3 + N], scalar=_col(cols, C_LW + 24 + i),
                                                                                      in1=cA[:], op0=ALU.mult, op1=ALU.add),
                              reads=rk + ["cA"], writes=[("xl", j)])
                        if prefix:
                            ph.op("pool", lambda e, i=i, fl=fl, xp=xp: e.tensor_scalar(out=carry[:, i, :], in0=xp[:, N:N + 3], scalar1=fl,
                                                                                      scalar2=None, op0=ALU.mult),
                                  reads=[("xpad", jx)], writes=[("carry", i)])
                        else:
                            ph.op("pool", lambda e, i=i, xp=xp: e.tensor_copy(out=carry[:, i, :], in_=xp[:, N:N + 3]),
                                  reads=[("xpad", jx)], writes=[("carry", i)])

                    def st2(i, b=b, fl=fl):
                        jl, jr, jg, ja, j2 = (i % NBK[k_] for k_ in ("xl", "rr", "ig", "aa", "a2"))
                        for (wg, gp, nm) in ((wga, gpa, "gpa"), (wgx, gpx, "gpx")):
                            for g0 in (0, 512):
                                ph.op("pe", lambda e, wg=wg, gp=gp, g0=g0, i=i, jl=jl: e.matmul(gp[:, g0:g0 + 512], lhsT=wg[:, i, :], rhs=xlS[jl][:, g0:g0 + 512],
                                                                                           start=True, stop=True),
                                      reads=[("xl", jl), "wga", "wgx"], writes=[(nm, g0)])
                        ph.op("act", lambda e, i=i, jr=jr: e.activation(out=rrS[jr][:], in_=gpa[:], func=AF.Sigmoid, bias=_col(cols, C_BA + i)),
                              reads=[("gpa", 0), ("gpa", 512)], writes=[("rr", jr)])
                        ph.op("act", lambda e, i=i, jg=jg: e.activation(out=igS[jg][:], in_=gpx[:], func=AF.Sigmoid, bias=_col(cols, C_BX + i)),
                              reads=[("gpx", 0), ("gpx", 512)], writes=[("ig", jg)])
                        ph.op("act", lambda e, i=i, jr=jr, ja=ja: e.activation(out=aaS[ja][:], in_=rrS[jr][:], func=AF.Exp, scale=clc[:, i:i + 1]),
                              reads=[("rr", jr)], writes=[("aa", ja)])
                        ph.op("act", lambda e, i=i, jr=jr, j2=j2: e.activation(out=a2S[j2][:], in_=rrS[jr][:], func=AF.Exp, scale=clc[:, 8 + i:9 + i]),
                              reads=[("rr", jr)], writes=[("a2", j2)])
                        ph.op("act", lambda e, j2=j2: e.activation(out=a2S[j2][:], in_=a2S[j2][:], func=AF.Sqrt, scale=-1.0, bias=1.0),
                              reads=[("a2", j2)], writes=[("a2", j2)])

                    def st3(i, b=b, fl=fl):
                        jl, jg, ja, j2, ju, jh = (i % NBK[k_] for k_ in ("xl", "ig", "aa", "a2", "uu", "hs"))
                        ph.op("pool", lambda e, jl=jl, jg=jg, ju=ju: e.tensor_tensor(out=uuS[ju][:], in0=igS[jg][:], in1=xlS[jl][:], op=ALU.mult),
                              reads=[("ig", jg), ("xl", jl)], writes=[("uu", ju)])
                        ph.op("pool", lambda e, j2=j2, ju=ju: e.tensor_tensor(out=uuS[ju][:], in0=uuS[ju][:], in1=a2S[j2][:], op=ALU.mult),
                              reads=[("uu", ju), ("a2", j2)], writes=[("uu", ju)])
                        ph.op("dve", lambda e, i=i, ja=ja, ju=ju, jh=jh: e.tensor_tensor_scan(out=hsS[jh][:], data0=aaS[ja][:], data1=uuS[ju][:],
                                                                                             initial=state[:, i:i + 1], op0=ALU.mult, op1=ALU.add),
                              reads=[("aa", ja), ("uu", ju), ("state", i)], writes=[("hs", jh)])
                        if prefix:
                            ph.op("pool", lambda e, i=i, fl=fl, jh=jh: e.tensor_scalar(out=state[:, i:i + 1], in0=hsS[jh][:, N - 1:N], scalar1=fl,
                                                                                      scalar2=None, op0=ALU.mult),
                                  reads=[("hs", jh)], writes=[("state", i)])
                        else:
                            ph.op("pool", lambda e, i=i, jh=jh: e.tensor_copy(out=state[:, i:i + 1], in_=hsS[jh][:, N - 1:N]),
                                  reads=[("hs", jh)], writes=[("state", i)])
                            sg_ = load_slice(1024 + i * 128)
                            proj(gpa, "gpa", sg_, 0, N)
                            ph.op("act", lambda e: e.activation(out=gel[:], in_=gpa[:], func=AF.Gelu_apprx_tanh),
                                  reads=[("gpa", 0), ("gpa", 512)], writes=["gel"])
                            cs_ = i % 2
                            ph.op("dve", lambda e, cs_=cs_, jh=jh: e.tensor_tensor(out=catc[cs_][:], in0=hsS[jh][:], in1=gel[:], op=ALU.mult),
                                  reads=[("hs", jh), "gel"], writes=[("catc", cs_)])
                            ph.op("sp", lambda e, cs_=cs_, i=i, b=b: e.dma_start(out=catT_d[i, :, b * N:(b + 1) * N], in_=catc[cs_][:]),
                                  reads=[("catc", cs_)], writes=[("catT_d", i, b)], dma=True, semkey=("st", "catc", cs_))

                    if prefix:
                        for s_ in range(8 + 3):
                            if s_ < 8:
                                st0(s_)
                            if 0 <= s_ - 1 < 8:
                                st1(s_ - 1)
                            if 0 <= s_ - 2 < 8:
                                st2(s_ - 2)
                            if 0 <= s_ - 3 < 8:
                                st3(s_ - 3)
                    else:
                        for i in range(8):
                            st0(i)
                            st1(i)
                            st2(i)
                            st3(i)
                    if prefix and b == nblocks - 1:
                        for i in range(8):
                            sv = load_slice(2048 + i * 128)
                            sg2 = load_slice(3072 + i * 128)
                            proj(pp, "pp", sv, N - 32, 32)
                            proj(gpa, "gpa", sg2, N - 32, 32)
                            ph.op("act", lambda e: e.activation(out=zsg[:], in_=gpa[:, 0:32], func=AF.Sigmoid),
                                  reads=[("gpa", 0)], writes=["zsg"])
                            ph.op("dve", lambda e: e.tensor_tensor(out=zs[:], in0=pp[:, 0:32], in1=zsg[:], op=ALU.mult),
                                  reads=[("pp", 0), "zsg"], writes=["zs"])
                            ph.op("pool", lambda e, i=i, fl=fl: e.tensor_scalar(out=zcarry[:, i, :], in0=zs[:, 2:32], scalar1=fl,
                                                                               scalar2=None, op0=ALU.mult),
                                  reads=["zs"], writes=[("zcarry", i)])
                    if not prefix:
                        for i in range(8):
                            sv = load_slice(2048 + i * 128)
                            sg2 = load_slice(3072 + i * 128)
                            proj(pp, "pp", sv, 0, N)
                            proj(gpx, "gpx", sg2, 0, N)
                            ph.op("act", lambda e: e.activation(out=gel[:], in_=gpx[:], func=AF.Sigmoid),
                                  reads=[("gpx", 0), ("gpx", 512)], writes=["gel"])
                            ph.op("dve", lambda e: e.tensor_tensor(out=zpad[:, 30:30 + N], in0=pp[:], in1=gel[:], op=ALU.mult),
                                  reads=[("pp", 0), ("pp", 512), "gel"], writes=["zpad"])
                            ph.op("pool", lambda e, i=i: e.tensor_copy(out=zpad[:, 0:30], in_=zcarry[:, i, :]),
                                  reads=[("zcarry", i), "zcarry"], writes=["zpadc"])
                            ph.op("dve", lambda e, i=i: e.tensor_scalar(out=cA[:], in0=zpad[:, 0:N], scalar1=_col(cols, C_CW + i),
                                                                       scalar2=_col(cols, C_CB + i), op0=ALU.mult, op1=ALU.add),
                                  reads=["zpad", "zpadc"], writes=["cA"])
                            bufs = [(cA, "cA"), (cB, "cB")]
                            for j in range(1, 31):
                                src, sn = bufs[(j - 1) % 2]
                                if j == 30:
                                    o_ap, dn = cz[:, i, :], ("cz", i)
                                else:
                                    o_ap, dn = bufs[j % 2][0][:], bufs[j % 2][1]
                                ph.op("dve", lambda e, i=i, j=j, src=src, o_ap=o_ap: e.scalar_tensor_tensor(
                                    out=o_ap, in0=zpad[:, j:j + N], scalar=_col(cols, C_CW + j * 8 + i), in1=src[:],
                                    op0=ALU.mult, op1=ALU.add),
                                    reads=["zpad", "zpadc", sn], writes=[dn])
                            ph.op("pool", lambda e, i=i: e.tensor_copy(out=zcarry[:, i, :], in_=zpad[:, N:N + 30]),
                                  reads=["zpad"], writes=[("zcarry", i)])
                            q_ = i % 2
                            ph.op("act", lambda e, i=i, q_=q_: e.activation(out=sqt[q_][:], in_=cz[:, i, :], func=AF.Square),
                                  reads=[("cz", i)], writes=[(("rr", 0), ("ig", 0))[q_]])
                            for g0 in (0, 512):
                                ph.op("pe", lambda e, i=i, g0=g0: e.matmul(gpa[:, g0:g0 + 512], lhsT=ones[:], rhs=cz[:, i, g0:g0 + 512],
                                                                          start=(i == 0), stop=(i == 7)),
                                      reads=[("cz", i), "ones"], writes=[("gpa", g0)])
                            for g0 in (0, 512):
                                ph.op("pe", lambda e, i=i, g0=g0, q_=q_: e.matmul(tp[g0 // 512][:], lhsT=ones[:], rhs=sqt[q_][:, g0:g0 + 512],
                                                                                 start=(i == 0), stop=(i == 7)),
                                      reads=[(("rr", 0), ("ig", 0))[q_], "ones"], writes=[("tp", g0 // 512)])
                        ph.op("act", lambda e: e.activation(out=mean[:], in_=gpa[:], func=AF.Copy, scale=1.0 / 1024.0),
                              reads=[("gpa", 0), ("gpa", 512)], writes=[("aa", 0)])
                        for g in range(2):
                            ph.op("act", lambda e, g=g: e.activation(out=rstd[:, g * 512:(g + 1) * 512], in_=tp[g][:], func=AF.Copy, scale=1.0 / 1024.0),
                                  reads=[("tp", g)], writes=[("a2", 0)])
                        ph.op("dve", lambda e: e.tensor_tensor(out=cA[:], in0=mean[:], in1=mean[:], op=ALU.mult),
                              reads=[("aa", 0)], writes=["cA"])
                        ph.op("dve", lambda e: e.tensor_tensor(out=rstd[:], in0=rstd[:], in1=cA[:], op=ALU.subtract),
                              reads=[("a2", 0), "cA"], writes=[("a2", 0)])
                        ph.op("act", lambda e: e.activation(out=rstd[:], in_=rstd[:], func=AF.Sqrt, bias=EPS),
                              reads=[("a2", 0)], writes=[("a2", 0)])
                        ph.op("dve", lambda e: e.reciprocal(out=rstd[:], in_=rstd[:]), reads=[("a2", 0)], writes=[("a2", 0)])
                        for i in range(8):
                            ph.op("pool", lambda e, i=i: e.tensor_tensor(out=cA[:], in0=cz[:, i, :], in1=mean[:], op=ALU.subtract),
                                  reads=[("cz", i), ("aa", 0)], writes=["cA"])
                            ph.op("dve", lambda e: e.tensor_tensor(out=cB[:], in0=cA[:], in1=rstd[:], op=ALU.mult),
                                  reads=["cA", ("a2", 0)], writes=["cB"])
                            cs_ = i % 2
                            ph.op("act", lambda e, i=i, cs_=cs_: e.activation(out=catc[cs_][:], in_=cB[:], func=AF.Silu,
                                                                             scale=_col(cols, C_LG + i), bias=_col(cols, C_LNB + i)),
                                  reads=["cB"], writes=[("catc", cs_)])
                            ph.op("sp", lambda e, cs_=cs_, i=i, b=b: e.dma_start(out=catT_d[8 + i, :, b * N:(b + 1) * N], in_=catc[cs_][:]),
                                  reads=[("catc", cs_)], writes=[("catT_d", 8 + i, b)], dma=True, semkey=("st", "catc", cs_))
                ph.emit()

        mixer_phase("P", x_pre, NPRE // TB, True)
        mixer_phase("M", x_main, T // TB, False)
        if stop_after == "M":
            return nc

        with ExitStack() as ph_es:
            ph = Phase(nc, "C")
            def sbp(nm, shape, dt=F32):
                return ph_es.enter_context(nc.sbuf_tensor("c_" + nm, list(shape), dt))
            def psp(nm, shape):
                return ph_es.enter_context(nc.psum_tensor("c_" + nm, list(shape), F32))
            wo = sbp("wo", [128, 16, D], BF16)
            gg = sbp("gg", [128, D])
            ct = [sbp(f"ct{i}", [128, 16, 128], BF16) for i in range(2)]
            xt = [sbp(f"cxt{i}", [128, D]) for i in range(2)]
            yo = [sbp(f"yo{i}", [128, D]) for i in range(2)]
            junk = sbp("cjunk", [128, D], BF16)
            ss = sbp("css", [128, 4])
            rs = sbp("crs", [128, 4])
            yp = [psp(f"yp{i}", [128, D]) for i in range(2)]
            for k in range(16):
                ph.op("pool", lambda e, k=k: e.dma_start(out=wo[:, k, :], in_=w_out[k * 128:(k + 1) * 128, :]),
                      writes=[("wo", k)], dma=True)
            ph.op("sp", lambda e: e.dma_start(out=gg[:], in_=modbc_d[:, 2 * D:3 * D]), writes=["gg"], dma=True)
            for t in range(T // 128):
                s = t % 2
                ph.op("sp", lambda e, s=s, t=t: e.dma_start(out=ct[s][:], in_=catT_d[:, :, t * 128:(t + 1) * 128].rearrange("k p n -> p k n")),
                      writes=[("ct", s)], dma=True)
                ph.op("sp", lambda e, s=s, t=t: e.dma_start(out=xt[s][:], in_=x_main[t * 128:(t + 1) * 128, :]),
                      writes=[("xt", s)], dma=True)

                def mm(e, s=s):
                    last = None
                    for g in range(4):
                        for k in range(16):
                            last = e.matmul(yp[s][:, g * 512:(g + 1) * 512], lhsT=ct[s][:, k, :], rhs=wo[:, k, g * 512:(g + 1) * 512],
                                            start=(k == 0), stop=(k == 15))
                    return last
                ph.op("pe", mm, reads=[("ct", s)] + [("wo", k) for k in range(16)], writes=[("yp", s)])
                ph.op("act", lambda e, s=s: e.activation(out=junk[:], in_=yp[s][:], func=AF.Square, accum_out=ss[:, s:s + 1]),
                      reads=[("yp", s)], writes=["junk", ("ss", s)])
                ph.op("act", lambda e, s=s: e.activation(out=rs[:, s:s + 1], in_=ss[:, s:s + 1], func=AF.Sqrt, scale=1.0 / D, bias=EPS),
                      reads=[("ss", s)], writes=[("rs", s)])
                ph.op("dve", lambda e, s=s: e.reciprocal(out=rs[:, 2 + s:3 + s], in_=rs[:, s:s + 1]),
                      reads=[("rs", s)], writes=[("rs2", s)])
                ph.op("dve", lambda e, s=s: e.scalar_tensor_tensor(out=yo[s][:], in0=yp[s][:], scalar=rs[:, 2 + s:3 + s], in1=gg[:],
                                                                  op0=ALU.mult, op1=ALU.mult),
                      reads=[("yp", s), ("rs2", s), "gg"], writes=[("yo", s)])
                ph.op("pool", lambda e, s=s: e.tensor_tensor(out=yo[s][:], in0=yo[s][:], in1=xt[s][:], op=ALU.add),
                      reads=[("yo", s), ("xt", s)], writes=[("yo", s)])
                ph.op("sp", lambda e, s=s, t=t: e.dma_start(out=x1_d[t * 128:(t + 1) * 128, :], in_=yo[s][:]),
                      reads=[("yo", s)], writes=[("x1_d", t)], dma=True, semkey=("st", "yo", s))
            ph.emit()
        if stop_after == "C":
            _dbg_copy(nc, dbg, x1_d, rows=T)
            return nc

        with ExitStack() as ph_es:
            ph = Phase(nc, "D")
            def sbp(nm, shape, dt=F32):
                return ph_es.enter_context(nc.sbuf_tensor("d_" + nm, list(shape), dt))
            def psp(nm, shape, dt=F32):
                return ph_es.enter_context(nc.psum_tensor("d_" + nm, list(shape), dt))
            A2bc = sbp("A2bc", [128, D])
            sh2bc = sbp("sh2bc", [128, D])
            xt = [sbp(f"xt{i}", [128, D]) for i in range(2)]
            xn = [sbp(f"xn{i}", [128, D]) for i in range(2)]
            junk = sbp("junk", [128, D], BF16)
            ss = sbp("ss", [128, 4])
            rs = sbp("rs", [128, 4])
            h2T = sbp("h2T", [128, 16, 512], BF16)
            wqs = [sbp(f"wqs{i}", [128, 16, 128], BF16) for i in range(4)]
            qT = sbp("qT", [128, 16, 512])
            skT = sbp("skT", [128, 16, 128])
            S = sbp("S", [128, 16, 128])
            S2 = sbp("S2", [128, 16, 128])
            vals = sbp("vals", [128, 16, 16])
            idx = sbp("idx", [128, 16, 16], U32)
            idxf = sbp("idxf", [128, 16, 16])
            cand = sbp("cand", [128, 8, 256])
            cand2 = sbp("cand2", [128, 8, 256])
            cval = sbp("cval", [128, 8, 16])
            cidx = sbp("cidx", [128, 8, 16], U32)
            cif = sbp("cif", [128, 8, 16])
            ex = sbp("ex", [128, 8, 16])
            esum = sbp("esum", [128, 8])
            e1 = sbp("e1", [128, 8, 16, 16])
            e2 = sbp("e2", [128, 8, 16, 16])
            i_f = sbp("i_f", [128, 8, 16])
            jjf = sbp("jjf", [128, 8, 16])
            slot = sbp("slot", [128, 3, 128])
            slT = [sbp(f"slT{i}", [128, 3, 128]) for i in range(2)]
            k16 = sbp("k16", [128, 3, 16])
            tp = [psp(f"tp{i}", [128, 512]) for i in range(2)]
            qp = [psp(f"qp{i}", [128, 512]) for i in range(2)]
            scp = psp("scp", [128, 2048])

            ph.op("sp", lambda e: e.dma_start(out=A2bc[:], in_=modbc_d[:, 4 * D:5 * D]), writes=["A2bc"], dma=True)
            ph.op("sp", lambda e: e.dma_start(out=sh2bc[:], in_=modbc_d[:, 3 * D:4 * D]), writes=["sh2bc"], dma=True)
            ph.op("sp", lambda e: e.dma_start(out=S[:], in_=peer_sk.rearrange("j n d -> n j d")), writes=["S"], dma=True)
            for jg in range(4):
                def trs(e, jg=jg):
                    last = None
                    for jj in range(4):
                        last = e.transpose(out=tp[jg % 2][:, jj * 128:(jj + 1) * 128], in_=S[:, 4 * jg + jj, :], identity=ident[:])
                    return last
                ph.op("pe", trs, reads=["S"], writes=[("tp", jg % 2)])
                ph.op("act", lambda e, jg=jg: e.activation(out=skT[:, 4 * jg:4 * jg + 4, :],
                                                          in_=tp[jg % 2][:].rearrange("p (a c) -> p a c", a=4), func=AF.Copy),
                      reads=[("tp", jg % 2)], writes=["skT"])
            ph.op("dve", lambda e: e.tensor_copy(out=k16[:, 0, :], in_=iota[:, 0:16]), writes=["k16a"])
            ph.op("dve", lambda e: e.tensor_scalar(out=k16[:, 1, :], in0=iota[:, 0:16], scalar1=16.0, scalar2=None, op0=ALU.mult),
                  writes=["k16b"])
            ph.op("dve", lambda e: e.tensor_scalar(out=k16[:, 2, :], in0=iota[:, 0:16], scalar1=16.0, scalar2=16.0, op0=ALU.mult, op1=ALU.add),
                  writes=["k16c"])

            wplan = [j for g in range(4) for j in range(16)]
            wctr = [0, 0]

            def issue_wq():
                while wctr[0] < len(wplan) and wctr[0] < wctr[1] + 3:
                    n_ = wctr[0]
                    wctr[0] += 1
                    ph.op("pool", lambda e, s_=n_ % 4, j=wplan[n_]: e.dma_start(
                        out=wqs[s_][:], in_=peer_wq.rearrange("(k p) c -> p k c", p=128)[:, :, j * 128:(j + 1) * 128]),
                        writes=[("wqs", n_ % 4)], dma=True)

            tctr = 0
            for g in range(4):
                for t in range(4):
                    s = tctr % 2
                    tctr += 1
                    r0 = g * 512 + t * 128
                    ph.op("sp", lambda e, s=s, r0=r0: e.dma_start(out=xt[s][:], in_=x1_d[r0:r0 + 128, :]), writes=[("xt", s)], dma=True)
                    ph.op("act", lambda e, s=s: e.activation(out=junk[:], in_=xt[s][:], func=AF.Square, accum_out=ss[:, s:s + 1]),
                          reads=[("xt", s)], writes=["junk", ("ss", s)])
                    ph.op("act", lambda e, s=s: e.activation(out=rs[:, s:s + 1], in_=ss[:, s:s + 1], func=AF.Sqrt, scale=1.0 / D, bias=EPS),
                          reads=[("ss", s)], writes=[("rs", s)])
                    ph.op("dve", lambda e, s=s: e.reciprocal(out=rs[:, 2 + s:3 + s], in_=rs[:, s:s + 1]), reads=[("rs", s)], writes=[("rs2", s)])
                    ph.op("dve", lambda e, s=s: e.scalar_tensor_tensor(out=xn[s][:], in0=xt[s][:], scalar=rs[:, 2 + s:3 + s], in1=A2bc[:],
                                                                      op0=ALU.mult, op1=ALU.mult),
                          reads=[("xt", s), ("rs2", s), "A2bc"], writes=[("xn", s)])
                    ph.op("pool", lambda e, s=s: e.tensor_tensor(out=xn[s][:], in0=xn[s][:], in1=sh2bc[:], op=ALU.add),
                          reads=[("xn", s), "sh2bc"], writes=[("xn", s)])
                    for kg in range(4):
                        bk = kg % 2

                        def trs(e, s=s, kg=kg, bk=bk):
                            last = None
                            for kk in range(4):
                                k = 4 * kg + kk
                                last = e.transpose(out=tp[bk][:, kk * 128:(kk + 1) * 128], in_=xn[s][:, k * 128:(k + 1) * 128], identity=ident[:])
                            return last
                        ph.op("pe", trs, reads=[("xn", s)], writes=[("tp", bk)])
                        if kg % 2 == 0:
                            ph.op("act", lambda e, kg=kg, bk=bk, t=t: e.activation(
                                out=h2T[:, 4 * kg:4 * kg + 4, t * 128:(t + 1) * 128],
                                in_=tp[bk][:].rearrange("p (a c) -> p a c", a=4), func=AF.Copy),
                                reads=[("tp", bk)], writes=["h2T"])
                        else:
                            ph.op("dve", lambda e, kg=kg, bk=bk, t=t: e.tensor_copy(
                                out=h2T[:, 4 * kg:4 * kg + 4, t * 128:(t + 1) * 128],
                                in_=tp[bk][:].rearrange("p (a c) -> p a c", a=4)),
                                reads=[("tp", bk)], writes=["h2T"])
                ph.op("sp", lambda e, g=g: e.dma_start(out=h2T_d[:, :, g * 512:(g + 1) * 512].rearrange("k p n -> p k n"), in_=h2T[:]),
                      reads=["h2T"], writes=[("h2T_d", g)], dma=True, semkey=("st", "h2T"))
                for j in range(16):
                    issue_wq()
                    sl = wctr[1] % 4
                    wctr[1] += 1

                    def mm(e, sl=sl, j=j):
                        last = None
                        for k in range(16):
                            last = e.matmul(qp[j % 2][:], lhsT=wqs[sl][:, k, :], rhs=h2T[:, k, :], start=(k == 0), stop=(k == 15))
                        return last
                    ph.op("pe", mm, reads=[("wqs", sl), "h2T"], writes=[("qp", j % 2)])
                    issue_wq()
                    if j % 2 == 0:
                        ph.op("act", lambda e, j=j: e.activation(out=qT[:, j, :], in_=qp[j % 2][:], func=AF.Copy),
                              reads=[("qp", j % 2)], writes=[("qT", j)])
                    else:
                        ph.op("dve", lambda e, j=j: e.tensor_copy(out=qT[:, j, :], in_=qp[j % 2][:]),
                              reads=[("qp", j % 2)], writes=[("qT", j)])
                for t in range(4):
                    tok0 = g * 512 + t * 128

                    def scm(e, t=t):
                        last = None
                        for j in range(16):
                            last = e.matmul(scp[:, j * 128:(j + 1) * 128], lhsT=qT[:, j, t * 128:(t + 1) * 128], rhs=skT[:, j, :],
                                            start=True, stop=True)
                        return last
                    ph.op("pe", scm, reads=[("qT", j) for j in range(16)] + ["skT"], writes=["scp"])
                    ph.op("act", lambda e: e.activation(out=S[:].rearrange("p a b -> p (a b)"), in_=scp[:], func=AF.Copy),
                          reads=["scp"], writes=["S"])
                    for j in range(16):
                        ph.op("dve", lambda e, j=j: e.max(out=vals[:, j, 0:8], in_=S[:, j, :]), reads=["S"], writes=[("v1", j)])
                    for j in range(16):
                        ph.op("dve", lambda e, j=j: e.match_replace(out=S2[:, j, :], in_to_replace=vals[:, j, 0:8], in_values=S[:, j, :], imm_value=-1e30),
                              reads=["S", ("v1", j)], writes=[("S2", j)])
                    for j in range(16):
                        ph.op("dve", lambda e, j=j: e.max(out=vals[:, j, 8:16], in_=S2[:, j, :]), reads=[("S2", j)], writes=[("v2", j)])
                    for j in range(16):
                        ph.op("dve", lambda e, j=j: e.max_index(out=idx[:, j, 0:8], in_max=vals[:, j, 0:8], in_values=S[:, j, :]),
                              reads=["S", ("v1", j)], writes=[("i1", j)])
                    for j in range(16):
                        ph.op("dve", lambda e, j=j: e.max_index(out=idx[:, j, 8:16], in_max=vals[:, j, 8:16], in_values=S2[:, j, :]),
                              reads=[("S2", j), ("v2", j)], writes=[("i2", j)])
                    allv = [("v1", j) for j in range(16)] + [("v2", j) for j in range(16)]
                    alli = [("i1", j) for j in range(16)] + [("i2", j) for j in range(16)]
                    ph.op("pool", lambda e: e.tensor_copy(out=idxf[:], in_=idx[:]), reads=alli, writes=["idxf"])
                    v4 = vals[:].rearrange("p (h q) k -> p h q k", q=2)
                    ph.op("pool", lambda e, v4=v4: e.tensor_tensor(
                        out=cand[:].rearrange("p h (a b) -> p h a b", a=16),
                        in0=v4[:, :, 0, :].unsqueeze(3).to_broadcast([128, 8, 16, 16]),
                        in1=v4[:, :, 1, :].unsqueeze(2).to_broadcast([128, 8, 16, 16]), op=ALU.add),
                        reads=allv, writes=["cand"])
                    for h in range(8):
                        ph.op("dve", lambda e, h=h: e.max(out=cval[:, h, 0:8], in_=cand[:, h, :]), reads=["cand"], writes=[("c1", h)])
                    for h in range(8):
                        ph.op("dve", lambda e, h=h: e.match_replace(out=cand2[:, h, :], in_to_replace=cval[:, h, 0:8], in_values=cand[:, h, :], imm_value=-1e30),
                              reads=["cand", ("c1", h)], writes=[("cand2", h)])
                    for h in range(8):
                        ph.op("dve", lambda e, h=h: e.max(out=cval[:, h, 8:16], in_=cand2[:, h, :]), reads=[("cand2", h)], writes=[("c2", h)])
                    for h in range(8):
                        ph.op("dve", lambda e, h=h: e.max_index(out=cidx[:, h, 0:8], in_max=cval[:, h, 0:8], in_values=cand[:, h, :]),
                              reads=["cand", ("c1", h)], writes=[("ci1", h)])
                    for h in range(8):
                        ph.op("dve", lambda e, h=h: e.max_index(out=cidx[:, h, 8:16], in_max=cval[:, h, 8:16], in_values=cand2[:, h, :]),
                              reads=[("cand2", h), ("c2", h)], writes=[("ci2", h)])
                    allc = [("c1", h) for h in range(8)] + [("c2", h) for h in range(8)]
                    allci = [("ci1", h) for h in range(8)] + [("ci2", h) for h in range(8)]
                    ph.op("pool", lambda e: e.tensor_tensor(out=ex[:], in0=cval[:], in1=cval[:, :, 0:1].to_broadcast([128, 8, 16]), op=ALU.subtract),
                          reads=allc, writes=["ex"])
                    ph.op("act", lambda e: e.activation(out=ex[:], in_=ex[:], func=AF.Exp), reads=["ex"], writes=["ex"])
                    ph.op("dve", lambda e: e.tensor_reduce(out=esum[:], in_=ex[:], axis=AX.X, op=ALU.add), reads=["ex"], writes=["esum"])
                    ph.op("dve", lambda e: e.reciprocal(out=esum[:], in_=esum[:]), reads=["esum"], writes=["esum"])
                    ph.op("pool", lambda e: e.tensor_tensor(out=slot[:, 2, :].rearrange("p (h k) -> p h k", h=8), in0=ex[:],
                                                            in1=esum[:].unsqueeze(2).to_broadcast([128, 8, 16]), op=ALU.mult),
                          reads=["ex", "esum"], writes=["slot_g"])
                    ph.op("pool", lambda e: e.tensor_copy(out=cif[:], in_=cidx[:]), reads=allci, writes=["cif"])
                    cb = cif[:].unsqueeze(3).to_broadcast([128, 8, 16, 16])
                    lo = k16[:, 1, :].unsqueeze(1).unsqueeze(1).to_broadcast([128, 8, 16, 16])
                    hi = k16[:, 2, :].unsqueeze(1).unsqueeze(1).to_broadcast([128, 8, 16, 16])
                    io = k16[:, 0, :].unsqueeze(1).unsqueeze(1).to_broadcast([128, 8, 16, 16])
                    i4 = idxf[:].rearrange("p (h q) k -> p h q k", q=2)
                    ph.op("dve", lambda e, cb=cb, lo=lo: e.tensor_tensor(out=e1[:], in0=cb, in1=lo, op=ALU.is_ge), reads=["cif", "k16b"], writes=["e1"])
                    ph.op("dve", lambda e, cb=cb, hi=hi: e.tensor_tensor(out=e2[:], in0=cb, in1=hi, op=ALU.is_ge), reads=["cif", "k16c"], writes=["e2"])
                    ph.op("dve", lambda e: e.tensor_tensor(out=e1[:], in0=e1[:], in1=e2[:], op=ALU.subtract), reads=["e1", "e2"], writes=["e1"])
                    ph.op("pool", lambda e, i4=i4: e.tensor_tensor(out=e2[:], in0=e1[:], in1=i4[:, :, 0, :].unsqueeze(2).to_broadcast([128, 8, 16, 16]), op=ALU.mult),
                          reads=["e1", "idxf"], writes=["e2"])
                    ph.op("dve", lambda e: e.tensor_reduce(out=slot[:, 0, :].rearrange("p (h k) -> p h k", h=8), in_=e2[:], axis=AX.X, op=ALU.add),
                          reads=["e2"], writes=["slot_r"])
                    ph.op("pool", lambda e, io=io: e.tensor_tensor(out=e2[:], in0=e1[:], in1=io, op=ALU.mult), reads=["e1", "k16a"], writes=["e2"])
                    ph.op("dve", lambda e: e.tensor_reduce(out=i_f[:], in_=e2[:], axis=AX.X, op=ALU.add), reads=["e2"], writes=["i_f"])
                    ph.op("dve", lambda e: e.scalar_tensor_tensor(out=jjf[:], in0=i_f[:], scalar=-16.0, in1=cif[:], op0=ALU.mult, op1=ALU.add),
                          reads=["i_f", "cif"], writes=["jjf"])
                    ph.op("dve", lambda e, io=io: e.tensor_tensor(out=e1[:], in0=jjf[:].unsqueeze(3).to_broadcast([128, 8, 16, 16]), in1=io, op=ALU.is_equal),
                          reads=["jjf", "k16a"], writes=["e1"])
                    ph.op("pool", lambda e, i4=i4: e.tensor_tensor(out=e2[:], in0=e1[:], in1=i4[:, :, 1, :].unsqueeze(2).to_broadcast([128, 8, 16, 16]), op=ALU.mult),
                          reads=["e1", "idxf"], writes=["e2"])
                    ph.op("dve", lambda e: e.tensor_reduce(out=slot[:, 1, :].rearrange("p (h k) -> p h k", h=8), in_=e2[:], axis=AX.X, op=ALU.add),
                          reads=["e2"], writes=["slot_c"])
                    st_ = t % 2

                    def trs(e):
                        last = None
                        for q in range(3):
                            last = e.transpose(out=tp[0][:, q * 128:(q + 1) * 128], in_=slot[:, q, :], identity=ident[:])
                        return last
                    ph.op("pe", trs, reads=["slot_r", "slot_c", "slot_g"], writes=[("tp", 0)])
                    ph.op("act", lambda e, st_=st_: e.activation(out=slT[st_][:], in_=tp[0][:, 0:384].rearrange("p (a c) -> p a c", a=3), func=AF.Copy),
                          reads=[("tp", 0)], writes=[("slT", st_)])
                    ph.op("sp", lambda e, st_=st_, tok0=tok0: e.dma_start(out=slots_d[:, :, tok0:tok0 + 128].rearrange("q p n -> p q n"), in_=slT[st_][:]),
                          reads=[("slT", st_)], writes=[("slots_d", tok0)], dma=True, semkey=("st", "slT", st_))
            ph.emit()
        if stop_after == "D":
            _dbg_copy3(nc, dbg, slots_d)
            return nc

        with ExitStack() as ph_es:
            ph = Phase(nc, "E")
            def sbp(nm, shape, dt=F32):
                return ph_es.enter_context(nc.sbuf_tensor("e_" + nm, list(shape), dt))
            def psp(nm, shape, dt=F32):
                return ph_es.enter_context(nc.psum_tensor("e_" + nm, list(shape), dt))
            slT = [sbp(f"slT{i}", [128, 3, 128]) for i in range(2)]
            Ab = [sbp(f"Ab{i}", [128, 128], BF16) for i in range(8)]
            Bb = [sbp(f"Bb{i}", [128, 128], BF16) for i in range(8)]
            Gs = [sbp(f"Gs{i}", [128, 32, 128, 4], BF16) for i in range(2)]
            Gp = [psp(f"Gp{i}", [128, 512]) for i in range(4)]
            for t in range(T // 128):
                s = t % 2
                ph.op("sp", lambda e, s=s, t=t: e.dma_start(out=slT[s][:], in_=slots_d[:, :, t * 128:(t + 1) * 128].rearrange("q p n -> p q n")),
                      writes=[("slT", s)], dma=True)
                for n4 in range(32):
                    bk = n4 % 4
                    for q in range(4):
                        n = n4 * 4 + q
                        a_ = n % 8
                        ph.op("dve", lambda e, s=s, n=n, a_=a_: e.tensor_scalar(out=Ab[a_][:], in0=iota_bf[:], scalar1=slT[s][:, 0, n:n + 1],
                                                                               scalar2=slT[s][:, 2, n:n + 1], op0=ALU.is_equal, op1=ALU.mult),
                              reads=[("slT", s)], writes=[("Ab", a_)])
                        ph.op("dve", lambda e, s=s, n=n, a_=a_: e.tensor_scalar(out=Bb[a_][:], in0=iota_bf[:], scalar1=slT[s][:, 1, n:n + 1],
                                                                               scalar2=None, op0=ALU.is_equal),
                              reads=[("slT", s)], writes=[("Bb", a_)])
                        ph.op("pe", lambda e, a_=a_, bk=bk, q=q: e.matmul(Gp[bk][:, q * 128:(q + 1) * 128], lhsT=Bb[a_][:], rhs=Ab[a_][:], start=True, stop=True),
                              reads=[("Ab", a_), ("Bb", a_)], writes=[("Gp", bk)])
                    ph.op("act", lambda e, s=s, n4=n4, bk=bk: e.activation(
                        out=Gs[s][:, :, n4 * 4:n4 * 4 + 4, :],
                        in_=Gp[bk][:].rearrange("p (q g r) -> p g q r", q=4, g=32, r=4), func=AF.Copy),
                        reads=[("Gp", bk)], writes=[("Gs", s)])
                for gq in range(4):
                    for g2 in range(2):
                        gi = gq * 2 + g2
                        ph.op("sp", lambda e, s=s, t=t, gi=gi: e.dma_start(out=G_ds[gi][:, :, t * 128:(t + 1) * 128, :],
                                                                          in_=Gs[s][:, gi * 4:(gi + 1) * 4, :, :]),
                              reads=[("Gs", s)], writes=[("G_d", t, gi)], dma=True, semkey=("st", "Gs", s, gq))
            ph.emit()

        if stop_after == "E":
            return nc
        y2_d = nc.dram_tensor("y2_d", [T, D], F32).ap()
        RGV = 4
        for hb in range(T // HB):
            with ExitStack() as ph_es:
                ph = Phase(nc, f"F{hb}")
                def sbp(nm, shape, dt=F32):
                    return ph_es.enter_context(nc.sbuf_tensor(f"f{hb}_" + nm, list(shape), dt))
                def psp(nm, shape, dt=F32):
                    return ph_es.enter_context(nc.psum_tensor(f"f{hb}_" + nm, list(shape), dt))
                h2T = sbp("h2T", [128, 16, HB], BF16)
                acc = sbp("acc", [128, HB // 128, D])
                ub = [sbp(f"ub{i}", [128, D], BF16) for i in range(3)]
                uT = [sbp(f"uT{i}", [128, 16, 128], BF16) for i in range(3)]
                vb = [sbp(f"vb{i}", [128, D], BF16) for i in range(8)]
                Gg = [sbp(f"Gg{i}", [128, HB, 4], BF16) for i in range(2)]
                gl = sbp("gl", [128, HB])
                W = [sbp(f"W{i}", [128, HB], BF16) for i in range(8)]
                tpu = [psp(f"tpu{i}", [128, 1024], BF16) for i in range(2)]
                actp = psp("actp", [128, HB])
                yp = [psp(f"yp{i}", [128, 1024]) for i in range(2)]
                ph.op("sp", lambda e: e.dma_start(out=h2T[:], in_=h2T_d[:, :, hb * HB:(hb + 1) * HB].rearrange("k p n -> p k n")),
                      writes=["h2T"], dma=True)
                nch = (F_GROUPS * RGV) if F_GROUPS else NEXP_CH
                ngroups = nch // RGV
                ypc = [0]

                def loadu(r):
                    if r >= nch:
                        return
                    rg, q4 = divmod(r, 4)
                    if q4 == 0:
                        ph.op("sp", lambda e, gs_=rg % 2, rg=rg: e.dma_start(out=Gg[gs_][:], in_=G_ds[rg // 4][:, rg % 4, hb * HB:(hb + 1) * HB, :]),
                              writes=[("Gg", rg % 2)], dma=True)
                    ph.op("pool", lambda e, r=r: e.dma_start(out=ub[r % 3][:], in_=peer_u[r * 128:(r + 1) * 128, :]), writes=[("ub", r % 3)], dma=True)

                def loadv(r):
                    if r >= nch:
                        return
                    ph.op("pool", lambda e, r=r: e.dma_start(out=vb[r % 8][:], in_=peer_v[r * 128:(r + 1) * 128, :]), writes=[("vb", r % 8)], dma=True)

                def front(r):
                    if r >= nch or F_MODE == 'l':
                        return
                    u_ = r % 3
                    for kg in range(2):
                        def trs(e, u_=u_, kg=kg):
                            last = None
                            for kk in range(8):
                                k = 8 * kg + kk
                                last = e.transpose(out=tpu[kg][:, kk * 128:(kk + 1) * 128], in_=ub[u_][:, k * 128:(k + 1) * 128], identity=ident_bf[:])
                            return last
                        ph.op("pe", trs, reads=[("ub", u_)], writes=[("tpu", kg)])
                        if kg == 0:
                            ph.op("act", lambda e, u_=u_, kg=kg: e.activation(
                                out=uT[u_][:, 8 * kg:8 * kg + 8, :], in_=tpu[kg][:].rearrange("p (a c) -> p a c", a=8), func=AF.Copy),
                                reads=[("tpu", kg)], writes=[("uT", u_, kg)])
                        else:
                            ph.op("dve", lambda e, u_=u_, kg=kg: e.tensor_copy(
                                out=uT[u_][:, 8 * kg:8 * kg + 8, :], in_=tpu[kg][:].rearrange("p (a c) -> p a c", a=8)),
                                reads=[("tpu", kg)], writes=[("uT", u_, kg)])

                def mid(r):
                    if F_MODE == 'l':
                        return
                    u_ = r % 3
                    w_ = r % 8
                    rg, q4 = divmod(r, 4)
                    gs_ = rg % 2

                    def mm(e, u_=u_):
                        last = None
                        for k in range(16):
                            for tg in range(HB // 512):
                                last = e.matmul(actp[:, tg * 512:(tg + 1) * 512], lhsT=uT[u_][:, k, :], rhs=h2T[:, k, tg * 512:(tg + 1) * 512],
                                                start=(k == 0), stop=(k == 15))
                        return last
                    ph.op("pe", mm, reads=[("uT", u_, 0), ("uT", u_, 1), "h2T"], writes=[("actp", tg) for tg in range(HB // 512)])
                    ph.op("act", lambda e: e.activation(out=gl[:], in_=actp[:], func=AF.Gelu_apprx_tanh),
                          reads=[("actp", tg) for tg in range(HB // 512)], writes=["gl"])
                    ph.op("dve", lambda e, w_=w_, gs_=gs_, q4=q4: e.tensor_tensor(out=W[w_][:], in0=gl[:], in1=Gg[gs_][:, :, q4], op=ALU.mult),
                          reads=["gl", ("Gg", gs_)], writes=[("W", w_)])

                def vphase(grp):
                    if F_MODE == 'l':
                        return
                    wbase = (grp % 2) * RGV
                    for t in range(HB // 128):
                        for dh in range(2):
                            yb = ypc[0] % 2
                            ypc[0] += 1

                            def vmm(e, t=t, dh=dh, yb=yb, wbase=wbase):
                                last = None
                                for qq in range(RGV):
                                    for dg in range(2):
                                        c0 = dh * 1024 + dg * 512
                                        last = e.matmul(yp[yb][:, dg * 512:(dg + 1) * 512], lhsT=W[wbase + qq][:, t * 128:(t + 1) * 128],
                                                        rhs=vb[wbase + qq][:, c0:c0 + 512], start=(qq == 0), stop=(qq == RGV - 1))
                                return last
                            ph.op("pe", vmm, reads=[("W", wbase + qq) for qq in range(RGV)] + [("vb", wbase + qq) for qq in range(RGV)],
                                  writes=[("yp", yb)])
                            if grp == 0:
                                ph.op("dve", lambda e, t=t, dh=dh, yb=yb: e.tensor_copy(out=acc[:, t, dh * 1024:(dh + 1) * 1024], in_=yp[yb][:]),
                                      reads=[("yp", yb)], writes=[("acc", t, dh)])
                            else:
                                ph.op("dve", lambda e, t=t, dh=dh, yb=yb: e.tensor_tensor(out=acc[:, t, dh * 1024:(dh + 1) * 1024], in0=yp[yb][:],
                                                                                        in1=acc[:, t, dh * 1024:(dh + 1) * 1024], op=ALU.add),
                                      reads=[("yp", yb), ("acc", t, dh)], writes=[("acc", t, dh)])

                for r in range(3):
                    loadu(r)
                front(0)
                front(1)
                for r in range(nch):
                    mid(r)
                    loadv(r)
                    loadu(r + 3)
                    front(r + 2)
                    if r % RGV == 0 and r >= RGV:
                        vphase(r // RGV - 1)
                vphase(ngroups - 1)
                for t in range(HB // 128 if F_MODE == '' else 0):
                    ph.op("sp", lambda e, t=t: e.dma_start(out=y2_d[hb * HB + t * 128:hb * HB + (t + 1) * 128, :], in_=acc[:, t, :]),
                          reads=[("acc", t, 0), ("acc", t, 1)], writes=[("y2_d", t)], dma=True, semkey=("st", "acc", t % 2))
                ph.emit()
        if stop_after == "F":
            _dbg_copy(nc, dbg, y2_d, rows=T)
            return nc

        with ExitStack() as ph_es:
            ph = Phase(nc, "G")
            def sbp(nm, shape, dt=F32):
                return ph_es.enter_context(nc.sbuf_tensor("gq_" + nm, list(shape), dt))
            gg = sbp("gg", [128, D])
            yt = [sbp(f"yt{i}", [128, D]) for i in range(2)]
            xt = [sbp(f"xt{i}", [128, D]) for i in range(2)]
            junk = sbp("junk", [128, D], BF16)
            ss = sbp("ss", [128, 4])
            rs = sbp("rs", [128, 4])
            ph.op("sp", lambda e: e.dma_start(out=gg[:], in_=modbc_d[:, 5 * D:6 * D]), writes=["gg"], dma=True)
            for t in range(T // 128):
                s = t % 2
                ph.op("sp", lambda e, s=s, t=t: e.dma_start(out=yt[s][:], in_=y2_d[t * 128:(t + 1) * 128, :]), writes=[("yt", s)], dma=True)
                ph.op("sp", lambda e, s=s, t=t: e.dma_start(out=xt[s][:], in_=x1_d[t * 128:(t + 1) * 128, :]), writes=[("xt", s)], dma=True)
                ph.op("act", lambda e, s=s: e.activation(out=junk[:], in_=yt[s][:], func=AF.Square, accum_out=ss[:, s:s + 1]),
                      reads=[("yt", s)], writes=["junk", ("ss", s)])
                ph.op("act", lambda e, s=s: e.activation(out=rs[:, s:s + 1], in_=ss[:, s:s + 1], func=AF.Sqrt, scale=1.0 / D, bias=EPS),
                      reads=[("ss", s)], writes=[("rs", s)])
                ph.op("dve", lambda e, s=s: e.reciprocal(out=rs[:, 2 + s:3 + s], in_=rs[:, s:s + 1]), reads=[("rs", s)], writes=[("rs2", s)])
                ph.op("dve", lambda e, s=s: e.scalar_tensor_tensor(out=yt[s][:], in0=yt[s][:], scalar=rs[:, 2 + s:3 + s], in1=gg[:],
                                                                  op0=ALU.mult, op1=ALU.mult),
                      reads=[("yt", s), ("rs2", s), "gg"], writes=[("yt", s)])
                ph.op("pool", lambda e, s=s: e.tensor_tensor(out=yt[s][:], in0=yt[s][:], in1=xt[s][:], op=ALU.add),
                      reads=[("yt", s), ("xt", s)], writes=[("yt", s)])
                ph.op("sp", lambda e, s=s, t=t: e.dma_start(out=out[t * 128:(t + 1) * 128, :], in_=yt[s][:]),
                      reads=[("yt", s)], writes=[("out", t)], dma=True, semkey=("st", "yt", s))
            ph.emit()
    return nc


def _dbg_copy3(nc, dbg, slots_d):
    with ExitStack() as es:
        buf = es.enter_context(nc.sbuf_tensor("dbgbuf3", [128, T], F32))
        s1 = es.enter_context(nc.semaphore("dbg3_s1"))
        with nc.Block() as block:
            @block.sync
            def _(e):
                n = 0
                for q in range(3):
                    e.dma_start(out=buf[:], in_=slots_d[q]).then_inc(s1, 16)
                    n += 16
                    e.wait_ge(s1, n)
                    e.dma_start(out=dbg[q * 128:(q + 1) * 128, :], in_=buf[:]).then_inc(s1, 16)
                    n += 16
                    e.wait_ge(s1, n)


def _dbg_copy(nc, dbg, src, rows):
    with ExitStack() as es:
        buf = es.enter_context(nc.sbuf_tensor("dbgbuf", [128, D], F32))
        s1 = es.enter_context(nc.semaphore("dbg_s1"))
        with nc.Block() as block:
            @block.sync
            def _(e):
                n = 0
                for t in range(rows // 128):
                    e.dma_start(out=buf[:], in_=src[t * 128:(t + 1) * 128, :]).then_inc(s1, 16)
                    n += 16
                    e.wait_ge(s1, n)
                    e.dma_start(out=dbg[t * 128:(t + 1) * 128, :], in_=buf[:]).then_inc(s1, 16)
                    n += 16
                    e.wait_ge(s1, n)


def _make_cols(inp, b, j):
    cols = np.zeros((128, NCOL), np.float32)

    def put(c0, vec):
        v = np.asarray(vec, np.float32).reshape(-1, 128)
        cols[:, c0:c0 + v.shape[0]] = v.T
    put(C_C, inp["c"][b])
    for tap in range(4):
        put(C_LW + tap * 8, inp["lru_conv_w"][0, tap])
    put(C_LB, inp["lru_conv_b"][0])
    put(C_BA, inp["lru_ba"][0])
    put(C_BX, inp["lru_bx"][0])
    put(C_LAM, inp["lru_lambda"][0])
    for tap in range(31):
        put(C_CW + tap * 8, inp["conf_dw_w"][0, tap])
    put(C_CB, inp["conf_dw_b"][0])
    put(C_LG, inp["conf_ln_g"][0])
    put(C_LNB, inp["conf_ln_b"][0])
    nvalid_blocks = (T * j) // TB
    for blk in range(NPRE // TB):
        cols[:, C_FL + blk] = 1.0 if blk >= (NPRE // TB - nvalid_blocks) else 0.0
    return cols


def make_in_maps(inp):
    x = np.ascontiguousarray(inp["x"], dtype=np.float32)
    shared = {
        "ident": np.eye(128, dtype=np.float32),
        "iota": np.tile(np.arange(128, dtype=np.float32)[None, :], (128, 1)),
        "w_mod": np.ascontiguousarray(inp["w_mod"][0]),
        "b_mod": np.ascontiguousarray(inp["b_mod"][0]),
        "g_pre_mix": np.ascontiguousarray(inp["g_pre_mix"][0]),
        "g_post_mix": np.ascontiguousarray(inp["g_post_mix"][0]),
        "g_pre_ffn": np.ascontiguousarray(inp["g_pre_ffn"][0]),
        "g_post_ffn": np.ascontiguousarray(inp["g_post_ffn"][0]),
        "w_in": np.ascontiguousarray(inp["w_in"][0]),
        "lru_wa": np.ascontiguousarray(inp["lru_wa"][0]),
        "lru_wx": np.ascontiguousarray(inp["lru_wx"][0]),
        "w_out": np.ascontiguousarray(inp["w_out"][0]),
        "peer_wq": np.ascontiguousarray(inp["peer_wq"][0]),
        "peer_sk": np.ascontiguousarray(inp["peer_subkeys"][0].reshape(16, 128, 128)),
        "peer_u": np.ascontiguousarray(inp["peer_u"][0]),
        "peer_v": np.ascontiguousarray(inp["peer_v"][0]),
    }
    maps = []
    for c in range(8):
        b, j = divmod(c, 4)
        m = dict(shared)
        m["x_main"] = np.ascontiguousarray(x[b, T * j:T * (j + 1)])
        pre = np.zeros((NPRE, D), np.float32)
        nv = T * j
        if nv:
            pre[NPRE - nv:] = x[b, 0:nv]
        m["x_pre"] = pre
        m["cols"] = _make_cols(inp, b, j)
        maps.append(m)
    return maps


def kernel(**inputs):
    nc = build_program()
    maps = make_in_maps(inputs)
    res = run_bass_kernel_spmd(nc, maps, core_ids=list(range(8)))
    out = np.empty((2, 8192, D), np.float32)
    for c in range(8):
        b, j = divmod(c, 4)
        out[b, T * j:T * (j + 1)] = res.results[c]["out"]
    return out
```

```python
import numpy as np
from contextlib import ExitStack
import concourse.bass as bass
import concourse.mybir as mybir
from concourse.bass_utils import run_bass_kernel_spmd

F32 = mybir.dt.float32
BF16 = mybir.dt.bfloat16
U32 = mybir.dt.uint32
AF = mybir.ActivationFunctionType
ALU = mybir.AluOpType
AX = mybir.AxisListType

D = 2048
KC = 16
T = 2048
NPRE = 6144
TB = 1024
EPS = 1e-6
NEXP_CH = 128
RG = 4
HB = 1024
F_GROUPS = 0
F_MODE = ''

C_C = 0
C_LW = 16
C_LB = 48
C_BA = 56
C_BX = 64
C_LAM = 72
C_CW = 80
C_CB = 328
C_LG = 336
C_LNB = 344
C_FL = 352
NCOL = 358


class _Op:
    __slots__ = ("eng", "fn", "deps", "need_inc", "dma", "semkey", "token")


class Phase:
    ENG = ("sp", "pool", "act", "dve", "pe")

    gpool = None

    def __init__(self, nc, name):
        self.nc = nc
        self.name = name
        self.ops = []
        self.lw = {}
        self.rd = {}

    def op(self, eng, fn, reads=(), writes=(), dma=False, semkey=None):
        o = _Op()
        o.eng = eng
        o.fn = fn
        o.dma = dma
        o.need_inc = dma
        o.token = None
        o.semkey = semkey if semkey is not None else (("dma", writes[0]) if dma else None)
        deps = []
        for k in reads:
            w = self.lw.get(k)
            if w is not None:
                deps.append(w)
        for k in writes:
            w = self.lw.get(k)
            if w is not None:
                deps.append(w)
            deps.extend(self.rd.get(k, ()))
        o.deps = []
        seen = set()
        for d in deps:
            if id(d) not in seen and d is not o:
                seen.add(id(d))
                o.deps.append(d)
                d.need_inc = True
        for k in writes:
            self.lw[k] = o
            self.rd[k] = []
        for k in reads:
            self.rd.setdefault(k, []).append(o)
        self.ops.append(o)
        return o

    def emit(self):
        nc = self.nc
        gp = self.gpool
        local = {}
        for o in self.ops:
            if o.dma:
                k = o.semkey
                local[k] = local.get(k, 0) + 16
                o.token = (k, local[k])
            elif o.need_inc:
                k = ("eng", o.eng)
                local[k] = local.get(k, 0) + 1
                o.token = (k, local[k])
        swkeys = set(o.semkey for o in self.ops if o.dma and o.eng == "pool")
        slot = {}
        nhw = nsw = 0
        for k in local:
            if k[0] == "eng":
                slot[k] = self.ENG.index(k[1])
                continue
            lst = gp["sw"] if k in swkeys else gp["hw"]
            n_ = nsw if k in swkeys else nhw
            if n_ >= len(lst):
                lst.append(None)
            if k in swkeys:
                nsw += 1
            else:
                nhw += 1
            slot[k] = ("sw" if k in swkeys else "hw", n_)
        def getslot(sl):
            if isinstance(sl, int):
                while len(gp["sems"]) < 5:
                    i = len(gp["sems"])
                    gp["sems"].append(gp["stack"].enter_context(nc.semaphore(f"gsem{i}")))
                    gp["counts"].append(0)
                return sl
            kind, n_ = sl
            lst = gp[kind]
            if lst[n_] is None:
                i = len(gp["sems"])
                gp["sems"].append(gp["stack"].enter_context(nc.semaphore(f"gsem{i}")))
                gp["counts"].append(0)
                lst[n_] = i
            return lst[n_]
        getslot(0)
        slot = {k: getslot(v) for k, v in slot.items()}
        base = {k: gp["counts"][slot[k]] for k in local}
        sems = {k: gp["sems"][slot[k]] for k in local}
        by_eng = {e: [o for o in self.ops if o.eng == e] for e in self.ENG}
        with nc.Block() as block:
            reg = {"sp": block.sync, "pool": block.gpsimd, "act": block.scalar,
                   "dve": block.vector, "pe": block.tensor}
            for ename in self.ENG:
                ops = by_eng[ename]

                def body(e, ops=ops):
                    waited = {}
                    for o in ops:
                        need = {}
                        for d in o.deps:
                            if d.eng == "pe" and o.eng == "pe" and not d.dma and not o.dma:
                                continue
                            s, v = d.token
                            if need.get(s, 0) < v:
                                need[s] = v
                        for s, v in need.items():
                            if waited.get(s, 0) < v:
                                e.wait_ge(sems[s], base[s] + v)
                                waited[s] = v
                        ins = o.fn(e)
                        if o.dma:
                            ins.then_inc(sems[o.semkey], 16)
                        elif o.need_inc:
                            ins.then_inc(sems[("eng", o.eng)], 1)
                    for s, v in local.items():
                        if waited.get(s, 0) < v:
                            e.wait_ge(sems[s], base[s] + v)
                reg[ename](body)
        for k, v in local.items():
            gp["counts"][slot[k]] += v


def _col(cols, c):
    return cols[:, c:c + 1]


def build_program(stop_after=None):
    nc = bass.Bass("TRN2", target_bir_lowering=False)

    def din(name, shape, dt=F32):
        return nc.dram_tensor(name, list(shape), dt, kind="ExternalInput").ap()

    x_main = din("x_main", [T, D])
    x_pre = din("x_pre", [NPRE, D])
    cols_d = din("cols", [128, NCOL])
    ident_d = din("ident", [128, 128])
    iota_d = din("iota", [128, 128])
    w_mod = din("w_mod", [D, 6 * D])
    b_mod = din("b_mod", [6 * D])
    g_pre_mix = din("g_pre_mix", [D])
    g_post_mix = din("g_post_mix", [D])
    g_pre_ffn = din("g_pre_ffn", [D])
    g_post_ffn = din("g_post_ffn", [D])
    w_in = din("w_in", [D, 2 * D])
    lru_wa = din("lru_wa", [8, 128, 128])
    lru_wx = din("lru_wx", [8, 128, 128])
    w_out = din("w_out", [D, D])
    peer_wq = din("peer_wq", [D, D])
    peer_sk = din("peer_sk", [16, 128, 128])
    peer_u = din("peer_u", [16384, D])
    peer_v = din("peer_v", [16384, D])
    out = nc.dram_tensor("out", [T, D], F32, kind="ExternalOutput").ap()

    modbc_d = nc.dram_tensor("modbc_d", [128, 6 * D], F32).ap()
    catT_d = nc.dram_tensor("catT_d", [16, 128, T], BF16).ap()
    x1_d = nc.dram_tensor("x1_d", [T, D], F32).ap()
    h2T_d = nc.dram_tensor("h2T_d", [16, 128, T], BF16).ap()
    slots_d = nc.dram_tensor("slots_d", [3, 128, T], F32).ap()
    G_ds = [nc.dram_tensor(f"G_d{i}", [128, 4, T, 4], BF16).ap() for i in range(8)]

    dbg = None
    if stop_after is not None:
        dbg = nc.dram_tensor("dbg", [T, D], F32, kind="ExternalOutput").ap()

    with ExitStack() as outer:
        Phase.gpool = {"sems": [], "counts": [], "stack": outer, "hw": [], "sw": []}

        def sb(name, shape, dt=F32):
            return outer.enter_context(nc.sbuf_tensor("g_" + name, list(shape), dt))
        cols = sb("cols", [128, NCOL])
        ident = sb("ident", [128, 128])
        iota = sb("iota", [128, 128])
        ones = sb("ones", [128, 128])
        ident_bf = sb("ident_bf", [128, 128], BF16)
        iota_bf = sb("iota_bf", [128, 128], BF16)
        carry = sb("carry", [128, 8, 3])
        zcarry = sb("zcarry", [128, 8, 30])
        state = sb("state", [128, 8])
        clc = sb("clc", [128, 16])

        with ExitStack() as ph_es:
            ph = Phase(nc, "A")
            def sbp(name, shape, dt=F32, es=ph_es):
                return es.enter_context(nc.sbuf_tensor("a_" + name, list(shape), dt))
            def psp(name, shape, es=ph_es):
                return es.enter_context(nc.psum_tensor("a_" + name, list(shape), F32))
            caT = sbp("caT", [128, 16])
            caTb = sbp("caTb", [128, 16, 128])
            wm = [sbp(f"wm{i}", [128, 16, 512]) for i in range(2)]
            bmb = [sbp(f"bmb{i}", [128, 512]) for i in range(2)]
            gb = [sbp(f"gb{i}", [128, 512]) for i in range(2)]
            mo = [sbp(f"mo{i}", [128, 512]) for i in range(2)]
            tmpA = sbp("tmpA", [128, 16])
            mps = [psp(f"mps{i}", [128, 512]) for i in range(2)]

            ph.op("sp", lambda e: e.dma_start(out=cols[:], in_=cols_d), writes=["cols"], dma=True)
            ph.op("sp", lambda e: e.dma_start(out=ident[:], in_=ident_d), writes=["ident"], dma=True)
            ph.op("sp", lambda e: e.dma_start(out=iota[:], in_=iota_d), writes=["iota"], dma=True)
            ph.op("pool", lambda e: e.memset(ones[:], 1.0), writes=["ones"])
            ph.op("dve", lambda e: e.tensor_copy(out=ident_bf[:], in_=ident[:]), reads=["ident"], writes=["ident_bf"])
            ph.op("dve", lambda e: e.tensor_copy(out=iota_bf[:], in_=iota[:]), reads=["iota"], writes=["iota_bf"])
            ph.op("pool", lambda e: e.memset(carry[:], 0.0), writes=["carry"])
            ph.op("pool", lambda e: e.memset(zcarry[:], 0.0), writes=["zcarry"])
            ph.op("pool", lambda e: e.memset(state[:], 0.0), writes=["state"])
            ph.op("act", lambda e: e.activation(out=caT[:], in_=cols[:, C_C:C_C + 16], func=AF.Silu),
                  reads=["cols"], writes=["caT"])
            ph.op("act", lambda e: e.activation(out=tmpA[:, 0:8], in_=cols[:, C_LAM:C_LAM + 8], func=AF.Exp, scale=-1.0),
                  reads=["cols"], writes=["tmpA"])
            ph.op("act", lambda e: e.activation(out=tmpA[:, 8:16], in_=tmpA[:, 0:8], func=AF.Ln, bias=1.0),
                  reads=["tmpA"], writes=["tmpA2"])
            ph.op("dve", lambda e: e.tensor_scalar(out=clc[:, 0:8], in0=tmpA[:, 8:16], scalar1=-8.0, scalar2=None, op0=ALU.mult),
                  reads=["tmpA2"], writes=["clc0"])
            ph.op("dve", lambda e: e.tensor_scalar(out=clc[:, 8:16], in0=tmpA[:, 8:16], scalar1=-16.0, scalar2=None, op0=ALU.mult),
                  reads=["tmpA2"], writes=["clc1"])
            for k in range(16):
                ph.op("dve", lambda e, k=k: e.tensor_copy(out=caTb[:, k, :], in_=caT[:, k:k + 1].to_broadcast([128, 128])),
                      reads=["caT"], writes=[("caTb", k)])
            gsrc = {1: g_pre_mix, 2: g_post_mix, 4: g_pre_ffn, 5: g_post_ffn}
            for n in range(24):
                s = n % 2
                sec = n // 4
                cb = (n % 4) * 512
                ph.op("sp", lambda e, n=n, s=s: e.dma_start(
                    out=wm[s][:], in_=w_mod.rearrange("(k p) c -> p k c", p=128)[:, :, n * 512:(n + 1) * 512]),
                    writes=[("wm", s)], dma=True)
                ph.op("sp", lambda e, n=n, s=s: e.dma_start(
                    out=bmb[s][:], in_=b_mod[n * 512:(n + 1) * 512].partition_broadcast(128)),
                    writes=[("bmb", s)], dma=True)
                if sec in gsrc:
                    ph.op("sp", lambda e, s=s, sec=sec, cb=cb: e.dma_start(
                        out=gb[s][:], in_=gsrc[sec][cb:cb + 512].partition_broadcast(128)),
                        writes=[("gb", s)], dma=True)

                def mm(e, s=s):
                    last = None
                    for k in range(16):
                        last = e.matmul(mps[s][:], lhsT=caTb[:, k, :], rhs=wm[s][:, k, :], start=(k == 0), stop=(k == 15))
                    return last
                ph.op("pe", mm, reads=[("wm", s)] + [("caTb", k) for k in range(16)], writes=[("mps", s)])
                if sec in (0, 3):
                    ph.op("dve", lambda e, s=s: e.tensor_tensor(out=mo[s][:], in0=mps[s][:], in1=bmb[s][:], op=ALU.add),
                          reads=[("mps", s), ("bmb", s)], writes=[("mo", s)])
                else:
                    ph.op("dve", lambda e, s=s: e.tensor_tensor(out=bmb[s][:], in0=mps[s][:], in1=bmb[s][:], op=ALU.add),
                          reads=[("mps", s), ("bmb", s)], writes=[("bmb", s)])
                    if sec in (1, 4):
                        ph.op("dve", lambda e, s=s: e.scalar_tensor_tensor(out=mo[s][:], in0=bmb[s][:], scalar=1.0, in1=gb[s][:],
                                                                          op0=ALU.add, op1=ALU.mult),
                              reads=[("bmb", s), ("gb", s)], writes=[("mo", s)])
                    else:
                        ph.op("dve", lambda e, s=s: e.tensor_tensor(out=mo[s][:], in0=bmb[s][:], in1=gb[s][:], op=ALU.mult),
                              reads=[("bmb", s), ("gb", s)], writes=[("mo", s)])
                ph.op("sp", lambda e, n=n, s=s: e.dma_start(out=modbc_d[:, n * 512:(n + 1) * 512], in_=mo[s][:]),
                      reads=[("mo", s)], writes=[("modbc_d", n)], dma=True, semkey=("st", "mo", s))
            ph.emit()

        if stop_after == "A":
            _dbg_copy(nc, dbg, modbc_d[:, 0:D], rows=128)
            return nc

        def mixer_phase(name, xsrc, nblocks, prefix):
            with ExitStack() as ph_es:
                ph = Phase(nc, name)
                def sbp(nm, shape, dt=F32):
                    return ph_es.enter_context(nc.sbuf_tensor(name + "_" + nm, list(shape), dt))
                def psp(nm, shape):
                    return ph_es.enter_context(nc.psum_tensor(name + "_" + nm, list(shape), F32))
                N = TB
                A1bc = sbp("A1bc", [128, D])
                sh1bc = sbp("sh1bc", [128, D])
                xt = [sbp(f"xt{i}", [128, D]) for i in range(2)]
                xn = [sbp(f"xn{i}", [128, D]) for i in range(2)]
                junk = sbp("junk", [128, D], BF16)
                ss = sbp("ss", [128, 4])
                rs = sbp("rs", [128, 4])
                hT = sbp("hT", [128, 16, N], BF16)
                wsl = [sbp(f"wsl{i}", [128, 16, 128], BF16) for i in range(4)]
                wga = sbp("wga", [128, 8, 128])
                wgx = sbp("wgx", [128, 8, 128])
                NBK = dict(xpad=2, xl=3, rr=1, ig=2, aa=2, a2=2, uu=1, hs=1) if prefix else dict(xpad=1, xl=1, rr=1, ig=1, aa=1, a2=1, uu=1, hs=1)
                xpadS = [sbp(f"xpad{j}", [128, N + 3]) for j in range(NBK["xpad"])]
                xpad = xpadS[0]
                cA = sbp("cA", [128, N])
                cB = sbp("cB", [128, N])
                xlS = [sbp(f"xl{j}", [128, N]) for j in range(NBK["xl"])]
                xl = xlS[0]
                rrS = [sbp(f"rr{j}", [128, N]) for j in range(NBK["rr"])]
                rr = rrS[0]
                igS = [sbp(f"ig{j}", [128, N]) for j in range(NBK["ig"])]
                ig = igS[0]
                aaS = [sbp(f"aa{j}", [128, N]) for j in range(NBK["aa"])]
                aa = aaS[0]
                a2S = [sbp(f"a2{j}", [128, N]) for j in range(NBK["a2"])]
                a2 = a2S[0]
                uuS = [sbp(f"uu{j}", [128, N]) for j in range(NBK["uu"])]
                uu = uuS[0]
                hsS = [sbp(f"hs{j}", [128, N]) for j in range(NBK["hs"])]
                hs = hsS[0]
                tp = [psp(f"tp{i}", [128, 512]) for i in range(2)]
                pp = psp("pp", [128, N])
                gpa = psp("gpa", [128, N])
                gpx = psp("gpx", [128, N])
                if not prefix:
                    zpad = sbp("zpad", [128, N + 30], BF16)
                    dgw = sbp("dgw", [128, 31, 128], BF16)
                    cz = sbp("cz", [128, 8, N])
                    gel = sbp("gel", [128, N])
                    catc = [sbp(f"catc{i}", [128, N], BF16) for i in range(2)]
                    mean = aa
                    rstd = a2
                    sqt = [rr, ig]
                zs = sbp("zs", [128, 32])
                zsg = sbp("zsg", [128, 32])

                ph.op("sp", lambda e: e.dma_start(out=A1bc[:], in_=modbc_d[:, D:2 * D]), writes=["A1bc"], dma=True)
                ph.op("sp", lambda e: e.dma_start(out=sh1bc[:], in_=modbc_d[:, 0:D]), writes=["sh1bc"], dma=True)
                ph.op("sp", lambda e: e.dma_start(out=wga[:], in_=lru_wa.rearrange("h i j -> i h j")), writes=["wga"], dma=True)
                ph.op("sp", lambda e: e.dma_start(out=wgx[:], in_=lru_wx.rearrange("h i j -> i h j")), writes=["wgx"], dma=True)

                plan = []
                for b_ in range(nblocks):
                    for i_ in range(8):
                        plan.append(i_ * 128)
                        if not prefix:
                            plan.append(1024 + i_ * 128)
                    if (prefix and b_ == nblocks - 1) or not prefix:
                        for i_ in range(8):
                            plan.append(2048 + i_ * 128)
                            plan.append(3072 + i_ * 128)
                wctr = [0, 0]

                def issue_loads():
                    while wctr[0] < len(plan) and wctr[0] < wctr[1] + 3:
                        n_ = wctr[0]
                        s_ = n_ % 4
                        c0 = plan[n_]
                        wctr[0] += 1
                        ph.op("pool", lambda e, s_=s_, c0=c0: e.dma_start(
                            out=wsl[s_][:], in_=w_in.rearrange("(k p) c -> p k c", p=128)[:, :, c0:c0 + 128]),
                            writes=[("wsl", s_)], dma=True)

                def load_slice(c0):
                    issue_loads()
                    n_ = wctr[1]
                    assert plan[n_] == c0, (n_, plan[n_], c0)
                    wctr[1] += 1
                    return n_ % 4

                def proj(dst, dname, s, t0, n):
                    for g0 in range(0, n, 512):
                        gn = min(512, n - g0)

                        def mm(e, g0=g0, gn=gn):
                            last = None
                            for k in range(16):
                                last = e.matmul(dst[:, g0:g0 + gn], lhsT=wsl[s][:, k, :], rhs=hT[:, k, t0 + g0:t0 + g0 + gn],
                                                start=(k == 0), stop=(k == 15))
                            return last
                        ph.op("pe", mm, reads=[("wsl", s), "hT"], writes=[(dname, g0)])
                    issue_loads()

                tctr = [0]
                for b in range(nblocks):
                    for t in range(N // 128):
                        s = tctr[0] % 2
                        tctr[0] += 1
                        r0 = b * N + t * 128
                        ph.op("sp", lambda e, s=s, r0=r0: e.dma_start(out=xt[s][:], in_=xsrc[r0:r0 + 128, :]),
                              writes=[("xt", s)], dma=True)
                        ph.op("act", lambda e, s=s: e.activation(out=junk[:], in_=xt[s][:], func=AF.Square, accum_out=ss[:, s:s + 1]),
                              reads=[("xt", s)], writes=["junk", ("ss", s)])
                        ph.op("act", lambda e, s=s: e.activation(out=rs[:, s:s + 1], in_=ss[:, s:s + 1], func=AF.Sqrt, scale=1.0 / D, bias=EPS),
                              reads=[("ss", s)], writes=[("rs", s)])
                        ph.op("dve", lambda e, s=s: e.reciprocal(out=rs[:, 2 + s:3 + s], in_=rs[:, s:s + 1]),
                              reads=[("rs", s)], writes=[("rs2", s)])
                        ph.op("dve", lambda e, s=s: e.scalar_tensor_tensor(out=xn[s][:], in0=xt[s][:], scalar=rs[:, 2 + s:3 + s], in1=A1bc[:],
                                                                          op0=ALU.mult, op1=ALU.mult),
                              reads=[("xt", s), ("rs2", s), "A1bc"], writes=[("xn", s)])
                        ph.op("pool", lambda e, s=s: e.tensor_tensor(out=xn[s][:], in0=xn[s][:], in1=sh1bc[:], op=ALU.add),
                              reads=[("xn", s), "sh1bc"], writes=[("xn", s)])
                        for kg in range(4):
                            bk = kg % 2

                            def trs(e, s=s, kg=kg, bk=bk):
                                last = None
                                for kk in range(4):
                                    k = 4 * kg + kk
                                    last = e.transpose(out=tp[bk][:, kk * 128:(kk + 1) * 128], in_=xn[s][:, k * 128:(k + 1) * 128],
                                                       identity=ident[:])
                                return last
                            ph.op("pe", trs, reads=[("xn", s), "ident"], writes=[("tp", bk)])
                            eng = "act" if kg % 2 == 0 else "dve"
                            if eng == "act":
                                ph.op("act", lambda e, kg=kg, bk=bk, t=t: e.activation(
                                    out=hT[:, 4 * kg:4 * kg + 4, t * 128:(t + 1) * 128],
                                    in_=tp[bk][:].rearrange("p (a c) -> p a c", a=4), func=AF.Copy),
                                    reads=[("tp", bk)], writes=["hT"])
                            else:
                                ph.op("dve", lambda e, kg=kg, bk=bk, t=t: e.tensor_copy(
                                    out=hT[:, 4 * kg:4 * kg + 4, t * 128:(t + 1) * 128],
                                    in_=tp[bk][:].rearrange("p (a c) -> p a c", a=4)),
                                    reads=[("tp", bk)], writes=["hT"])
                    fl = _col(cols, C_FL + b) if prefix else None

                    def st0(i, b=b, fl=fl):
                        j = i % NBK["xpad"]
                        sx = load_slice(i * 128)
                        proj(pp, "pp", sx, 0, N)
                        ph.op("act", lambda e, j=j: e.activation(out=xpadS[j][:, 3:3 + N], in_=pp[:], func=AF.Copy),
                              reads=[("pp", 0), ("pp", 512)], writes=[("xpad", j)])
                        ph.op("pool", lambda e, i=i, j=j: e.tensor_copy(out=xpadS[j][:, 0:3], in_=carry[:, i, :]),
                              reads=[("carry", i)], writes=[("xpadc", j)])

                    def st1(i, b=b, fl=fl):
                        jx = i % NBK["xpad"]
                        j = i % NBK["xl"]
                        xp = xpadS[jx]
                        rk = [("xpad", jx), ("xpadc", jx)]
                        ph.op("dve", lambda e, i=i, xp=xp: e.tensor_scalar(out=cA[:], in0=xp[:, 0:N], scalar1=_col(cols, C_LW + i),
                                                                          scalar2=_col(cols, C_LB + i), op0=ALU.mult, op1=ALU.add),
                              reads=rk, writes=["cA"])
                        ph.op("dve", lambda e, i=i, xp=xp: e.scalar_tensor_tensor(out=cB[:], in0=xp[:, 1:1 + N], scalar=_col(cols, C_LW + 8 + i),
                                                                                 in1=cA[:], op0=ALU.mult, op1=ALU.add),
                              reads=rk + ["cA"], writes=["cB"])
                        ph.op("dve", lambda e, i=i, xp=xp: e.scalar_tensor_tensor(out=cA[:], in0=xp[:, 2:2 + N], scalar=_col(cols, C_LW + 16 + i),
                                                                                 in1=cB[:], op0=ALU.mult, op1=ALU.add),
                              reads=rk + ["cB"], writes=["cA"])
                        ph.op("dve", lambda e, i=i, xp=xp, j=j: e.scalar_tensor_tensor(out=xlS[j][:], in0=xp[:, 3:3 + N], scalar=_col(cols, C_LW + 24 + i),
                                                                                      in1=cA[:], op0=ALU.mult, op1=ALU.add),
                              reads=rk + ["cA"], writes=[("xl", j)])
                        if prefix:
                            ph.op("pool", lambda e, i=i, fl=fl, xp=xp: e.tensor_scalar(out=carry[:, i, :], in0=xp[:, N:N + 3], scalar1=fl,
                                                                                      scalar2=None, op0=ALU.mult),
                                  reads=[("xpad", jx)], writes=[("carry", i)])
                        else:
                            ph.op("pool", lambda e, i=i, xp=xp: e.tensor_copy(out=carry[:, i, :], in_=xp[:, N:N + 3]),
                                  reads=[("xpad", jx)], writes=[("carry", i)])

                    def st2(i, b=b, fl=fl):
                        jl, jr, jg, ja, j2 = (i % NBK[k_] for k_ in ("xl", "rr", "ig", "aa", "a2"))
                        for (wg, gp, nm) in ((wga, gpa, "gpa"), (wgx, gpx, "gpx")):
                            for g0 in (0, 512):
                                ph.op("pe", lambda e, wg=wg, gp=gp, g0=g0, i=i, jl=jl: e.matmul(gp[:, g0:g0 + 512], lhsT=wg[:, i, :], rhs=xlS[jl][:, g0:g0 + 512],
                                                                                           start=True, stop=True),
                                      reads=[("xl", jl), "wga", "wgx"], writes=[(nm, g0)])
                        ph.op("act", lambda e, i=i, jr=jr: e.activation(out=rrS[jr][:], in_=gpa[:], func=AF.Sigmoid, bias=_col(cols, C_BA + i)),
                              reads=[("gpa", 0), ("gpa", 512)], writes=[("rr", jr)])
                        ph.op("act", lambda e, i=i, jg=jg: e.activation(out=igS[jg][:], in_=gpx[:], func=AF.Sigmoid, bias=_col(cols, C_BX + i)),
                              reads=[("gpx", 0), ("gpx", 512)], writes=[("ig", jg)])
                        ph.op("act", lambda e, i=i, jr=jr, ja=ja: e.activation(out=aaS[ja][:], in_=rrS[jr][:], func=AF.Exp, scale=clc[:, i:i + 1]),
                              reads=[("rr", jr)], writes=[("aa", ja)])
                        ph.op("act", lambda e, i=i, jr=jr, j2=j2: e.activation(out=a2S[j2][:], in_=rrS[jr][:], func=AF.Exp, scale=clc[:, 8 + i:9 + i]),
                              reads=[("rr", jr)], writes=[("a2", j2)])
                        ph.op("act", lambda e, j2=j2: e.activation(out=a2S[j2][:], in_=a2S[j2][:], func=AF.Sqrt, scale=-1.0, bias=1.0),
                              reads=[("a2", j2)], writes=[("a2", j2)])

                    def st3(i, b=b, fl=fl):
                        jl, jg, ja, j2, ju, jh = (i % NBK[k_] for k_ in ("xl", "ig", "aa", "a2", "uu", "hs"))
                        ph.op("pool", lambda e, jl=jl, jg=jg, ju=ju: e.tensor_tensor(out=uuS[ju][:], in0=igS[jg][:], in1=xlS[jl][:], op=ALU.mult),
                              reads=[("ig", jg), ("xl", jl)], writes=[("uu", ju)])
                        ph.op("pool", lambda e, j2=j2, ju=ju: e.tensor_tensor(out=uuS[ju][:], in0=uuS[ju][:], in1=a2S[j2][:], op=ALU.mult),
                              reads=[("uu", ju), ("a2", j2)], writes=[("uu", ju)])
                        ph.op("dve", lambda e, i=i, ja=ja, ju=ju, jh=jh: e.tensor_tensor_scan(out=hsS[jh][:], data0=aaS[ja][:], data1=uuS[ju][:],
                                                                                             initial=state[:, i:i + 1], op0=ALU.mult, op1=ALU.add),
                              reads=[("aa", ja), ("uu", ju), ("state", i)], writes=[("hs", jh)])
                        if prefix:
                            ph.op("pool", lambda e, i=i, fl=fl, jh=jh: e.tensor_scalar(out=state[:, i:i + 1], in0=hsS[jh][:, N - 1:N], scalar1=fl,
                                                                                      scalar2=None, op0=ALU.mult),
                                  reads=[("hs", jh)], writes=[("state", i)])
                        else:
                            ph.op("pool", lambda e, i=i, jh=jh: e.tensor_copy(out=state[:, i:i + 1], in_=hsS[jh][:, N - 1:N]),
                                  reads=[("hs", jh)], writes=[("state", i)])
                            sg_ = load_slice(1024 + i * 128)
                            proj(gpa, "gpa", sg_, 0, N)
                            ph.op("act", lambda e: e.activation(out=gel[:], in_=gpa[:], func=AF.Gelu_apprx_tanh),
                                  reads=[("gpa", 0), ("gpa", 512)], writes=["gel"])
                            cs_ = i % 2
                            ph.op("dve", lambda e, cs_=cs_, jh=jh: e.tensor_tensor(out=catc[cs_][:], in0=hsS[jh][:], in1=gel[:], op=ALU.mult),
                                  reads=[("hs", jh), "gel"], writes=[("catc", cs_)])
                            ph.op("sp", lambda e, cs_=cs_, i=i, b=b: e.dma_start(out=catT_d[i, :, b * N:(b + 1) * N], in_=catc[cs_][:]),
                                  reads=[("catc", cs_)], writes=[("catT_d", i, b)], dma=True, semkey=("st", "catc", cs_))

                    if prefix:
                        for s_ in range(8 + 3):
                            if s_ < 8:
                                st0(s_)
                            if 0 <= s_ - 1 < 8:
                                st1(s_ - 1)
                            if 0 <= s_ - 2 < 8:
                                st2(s_ - 2)
                            if 0 <= s_ - 3 < 8:
                                st3(s_ - 3)
                    else:
                        for i in range(8):
                            st0(i)
                            st1(i)
                            st2(i)
                            st3(i)
                    if prefix and b == nblocks - 1:
                        for i in range(8):
                            sv = load_slice(2048 + i * 128)
                            sg2 = load_slice(3072 + i * 128)
                            proj(pp, "pp", sv, N - 32, 32)
                            proj(gpa, "gpa", sg2, N - 32, 32)
                            ph.op("act", lambda e: e.activation(out=zsg[:], in_=gpa[:, 0:32], func=AF.Sigmoid),
                                  reads=[("gpa", 0)], writes=["zsg"])
                            ph.op("dve", lambda e: e.tensor_tensor(out=zs[:], in0=pp[:, 0:32], in1=zsg[:], op=ALU.mult),
                                  reads=[("pp", 0), "zsg"], writes=["zs"])
                            ph.op("pool", lambda e, i=i, fl=fl: e.tensor_scalar(out=zcarry[:, i, :], in0=zs[:, 2:32], scalar1=fl,
                                                                               scalar2=None, op0=ALU.mult),
                                  reads=["zs"], writes=[("zcarry", i)])
                    if not prefix:
                        for i in range(8):
                            sv = load_slice(2048 + i * 128)
                            sg2 = load_slice(3072 + i * 128)
                            proj(pp, "pp", sv, 0, N)
                            proj(gpx, "gpx", sg2, 0, N)
                            ph.op("act", lambda e: e.activation(out=gel[:], in_=gpx[:], func=AF.Sigmoid),
                                  reads=[("gpx", 0), ("gpx", 512)], writes=["gel"])
                            ph.op("dve", lambda e: e.tensor_tensor(out=zpad[:, 30:30 + N], in0=pp[:], in1=gel[:], op=ALU.mult),
                                  reads=[("pp", 0), ("pp", 512), "gel"], writes=["zpad"])
                            ph.op("pool", lambda e, i=i: e.tensor_copy(out=zpad[:, 0:30], in_=zcarry[:, i, :]),
                                  reads=[("zcarry", i), "zcarry"], writes=["zpadc"])
                            ph.op("pool", lambda e, i=i: e.tensor_tensor(
                                out=dgw[:], in0=ident_bf[:].unsqueeze(1).to_broadcast([128, 31, 128]),
                                in1=cols[:, C_CW + i:C_CW + i + 241:8].unsqueeze(2).to_broadcast([128, 31, 128]), op=ALU.mult),
                                writes=["dgw"])
                            for g0 in (0, 512):
                                def cmm(e, g0=g0):
                                    last = None
                                    for j in range(31):
                                        last = e.matmul(pp[:, g0:g0 + 512], lhsT=dgw[:, j, :], rhs=zpad[:, g0 + j:g0 + j + 512],
                                                        start=(j == 0), stop=(j == 30))
                                    return last
                                ph.op("pe", cmm, reads=["zpad", "zpadc", "dgw"], writes=[("pp", g0)])
                            ph.op("act", lambda e, i=i: e.activation(out=cz[:, i, :], in_=pp[:], func=AF.Identity, bias=_col(cols, C_CB + i)),
                                  reads=[("pp", 0), ("pp", 512)], writes=[("cz", i)])
                            ph.op("pool", lambda e, i=i: e.tensor_copy(out=zcarry[:, i, :], in_=zpad[:, N:N + 30]),
                                  reads=["zpad"], writes=[("zcarry", i)])
                            q_ = i % 2
                            ph.op("act", lambda e, i=i, q_=q_: e.activation(out=sqt[q_][:], in_=cz[:, i, :], func=AF.Square),
                                  reads=[("cz", i)], writes=[(("rr", 0), ("ig", 0))[q_]])
                            for g0 in (0, 512):
                                ph.op("pe", lambda e, i=i, g0=g0: e.matmul(gpa[:, g0:g0 + 512], lhsT=ones[:], rhs=cz[:, i, g0:g0 + 512],
                                                                          start=(i == 0), stop=(i == 7)),
                                      reads=[("cz", i), "ones"], writes=[("gpa", g0)])
                            for g0 in (0, 512):
                                ph.op("pe", lambda e, i=i, g0=g0, q_=q_: e.matmul(tp[g0 // 512][:], lhsT=ones[:], rhs=sqt[q_][:, g0:g0 + 512],
                                                                                 start=(i == 0), stop=(i == 7)),
                                      reads=[(("rr", 0), ("ig", 0))[q_], "ones"], writes=[("tp", g0 // 512)])
                        ph.op("act", lambda e: e.activation(out=mean[:], in_=gpa[:], func=AF.Copy, scale=1.0 / 1024.0),
                              reads=[("gpa", 0), ("gpa", 512)], writes=[("aa", 0)])
                        for g in range(2):
                            ph.op("act", lambda e, g=g: e.activation(out=rstd[:, g * 512:(g + 1) * 512], in_=tp[g][:], func=AF.Copy, scale=1.0 / 1024.0),
                                  reads=[("tp", g)], writes=[("a2", 0)])
                        ph.op("dve", lambda e: e.tensor_tensor(out=cA[:], in0=mean[:], in1=mean[:], op=ALU.mult),
                              reads=[("aa", 0)], writes=["cA"])
                        ph.op("dve", lambda e: e.tensor_tensor(out=rstd[:], in0=rstd[:], in1=cA[:], op=ALU.subtract),
                              reads=[("a2", 0), "cA"], writes=[("a2", 0)])
                        ph.op("act", lambda e: e.activation(out=rstd[:], in_=rstd[:], func=AF.Sqrt, bias=EPS),
                              reads=[("a2", 0)], writes=[("a2", 0)])
                        ph.op("dve", lambda e: e.reciprocal(out=rstd[:], in_=rstd[:]), reads=[("a2", 0)], writes=[("a2", 0)])
                        for i in range(8):
                            ph.op("pool", lambda e, i=i: e.tensor_tensor(out=cA[:], in0=cz[:, i, :], in1=mean[:], op=ALU.subtract),
                                  reads=[("cz", i), ("aa", 0)], writes=["cA"])
                            ph.op("dve", lambda e: e.tensor_tensor(out=cB[:], in0=cA[:], in1=rstd[:], op=ALU.mult),
                                  reads=["cA", ("a2", 0)], writes=["cB"])
                            cs_ = i % 2
                            ph.op("act", lambda e, i=i, cs_=cs_: e.activation(out=catc[cs_][:], in_=cB[:], func=AF.Silu,
                                                                             scale=_col(cols, C_LG + i), bias=_col(cols, C_LNB + i)),
                                  reads=["cB"], writes=[("catc", cs_)])
                            ph.op("sp", lambda e, cs_=cs_, i=i, b=b: e.dma_start(out=catT_d[8 + i, :, b * N:(b + 1) * N], in_=catc[cs_][:]),
                                  reads=[("catc", cs_)], writes=[("catT_d", 8 + i, b)], dma=True, semkey=("st", "catc", cs_))
                ph.emit()

        mixer_phase("P", x_pre, NPRE // TB, True)
        mixer_phase("M", x_main, T // TB, False)
        if stop_after == "M":
            return nc

        with ExitStack() as ph_es:
            ph = Phase(nc, "C")
            def sbp(nm, shape, dt=F32):
                return ph_es.enter_context(nc.sbuf_tensor("c_" + nm, list(shape), dt))
            def psp(nm, shape):
                return ph_es.enter_context(nc.psum_tensor("c_" + nm, list(shape), F32))
            wo = sbp("wo", [128, 16, D], BF16)
            gg = sbp("gg", [128, D])
            ct = [sbp(f"ct{i}", [128, 16, 128], BF16) for i in range(2)]
            xt = [sbp(f"cxt{i}", [128, D]) for i in range(2)]
            yo = [sbp(f"yo{i}", [128, D]) for i in range(2)]
            junk = sbp("cjunk", [128, D], BF16)
            ss = sbp("css", [128, 4])
            rs = sbp("crs", [128, 4])
            yp = [psp(f"yp{i}", [128, D]) for i in range(2)]
            for k in range(16):
                ph.op("pool", lambda e, k=k: e.dma_start(out=wo[:, k, :], in_=w_out[k * 128:(k + 1) * 128, :]),
                      writes=[("wo", k)], dma=True)
            ph.op("sp", lambda e: e.dma_start(out=gg[:], in_=modbc_d[:, 2 * D:3 * D]), writes=["gg"], dma=True)
            for t in range(T // 128):
                s = t % 2
                ph.op("sp", lambda e, s=s, t=t: e.dma_start(out=ct[s][:], in_=catT_d[:, :, t * 128:(t + 1) * 128].rearrange("k p n -> p k n")),
                      writes=[("ct", s)], dma=True)
                ph.op("sp", lambda e, s=s, t=t: e.dma_start(out=xt[s][:], in_=x_main[t * 128:(t + 1) * 128, :]),
                      writes=[("xt", s)], dma=True)

                def mm(e, s=s):
                    last = None
                    for g in range(4):
                        for k in range(16):
                            last = e.matmul(yp[s][:, g * 512:(g + 1) * 512], lhsT=ct[s][:, k, :], rhs=wo[:, k, g * 512:(g + 1) * 512],
                                            start=(k == 0), stop=(k == 15))
                    return last
                ph.op("pe", mm, reads=[("ct", s)] + [("wo", k) for k in range(16)], writes=[("yp", s)])
                ph.op("act", lambda e, s=s: e.activation(out=junk[:], in_=yp[s][:], func=AF.Square, accum_out=ss[:, s:s + 1]),
                      reads=[("yp", s)], writes=["junk", ("ss", s)])
                ph.op("act", lambda e, s=s: e.activation(out=rs[:, s:s + 1], in_=ss[:, s:s + 1], func=AF.Sqrt, scale=1.0 / D, bias=EPS),
                      reads=[("ss", s)], writes=[("rs", s)])
                ph.op("dve", lambda e, s=s: e.reciprocal(out=rs[:, 2 + s:3 + s], in_=rs[:, s:s + 1]),
                      reads=[("rs", s)], writes=[("rs2", s)])
                ph.op("dve", lambda e, s=s: e.scalar_tensor_tensor(out=yo[s][:], in0=yp[s][:], scalar=rs[:, 2 + s:3 + s], in1=gg[:],
                                                                  op0=ALU.mult, op1=ALU.mult),
                      reads=[("yp", s), ("rs2", s), "gg"], writes=[("yo", s)])
                ph.op("pool", lambda e, s=s: e.tensor_tensor(out=yo[s][:], in0=yo[s][:], in1=xt[s][:], op=ALU.add),
                      reads=[("yo", s), ("xt", s)], writes=[("yo", s)])
                ph.op("sp", lambda e, s=s, t=t: e.dma_start(out=x1_d[t * 128:(t + 1) * 128, :], in_=yo[s][:]),
                      reads=[("yo", s)], writes=[("x1_d", t)], dma=True, semkey=("st", "yo", s))
            ph.emit()
        if stop_after == "C":
            _dbg_copy(nc, dbg, x1_d, rows=T)
            return nc

        with ExitStack() as ph_es:
            ph = Phase(nc, "D")
            def sbp(nm, shape, dt=F32):
                return ph_es.enter_context(nc.sbuf_tensor("d_" + nm, list(shape), dt))
            def psp(nm, shape, dt=F32):
                return ph_es.enter_context(nc.psum_tensor("d_" + nm, list(shape), dt))
            A2bc = sbp("A2bc", [128, D])
            sh2bc = sbp("sh2bc", [128, D])
            xt = [sbp(f"xt{i}", [128, D]) for i in range(2)]
            xn = [sbp(f"xn{i}", [128, D]) for i in range(2)]
            junk = sbp("junk", [128, D], BF16)
            ss = sbp("ss", [128, 4])
            rs = sbp("rs", [128, 4])
            h2T = sbp("h2T", [128, 16, 512], BF16)
            wqs = [sbp(f"wqs{i}", [128, 16, 128], BF16) for i in range(4)]
            qT = sbp("qT", [128, 16, 512])
            skT = sbp("skT", [128, 16, 128])
            S = sbp("S", [128, 16, 128])
            S2 = sbp("S2", [128, 16, 128])
            vals = sbp("vals", [128, 16, 16])
            idx = sbp("idx", [128, 16, 16], U32)
            idxf = sbp("idxf", [128, 16, 16])
            cand = sbp("cand", [128, 8, 256])
            cand2 = sbp("cand2", [128, 8, 256])
            cval = sbp("cval", [128, 8, 16])
            cidx = sbp("cidx", [128, 8, 16], U32)
            cif = sbp("cif", [128, 8, 16])
            ex = sbp("ex", [128, 8, 16])
            esum = sbp("esum", [128, 8])
            e1 = sbp("e1", [128, 8, 16, 16])
            e2 = sbp("e2", [128, 8, 16, 16])
            i_f = sbp("i_f", [128, 8, 16])
            jjf = sbp("jjf", [128, 8, 16])
            slot = sbp("slot", [128, 3, 128])
            slT = [sbp(f"slT{i}", [128, 3, 128]) for i in range(2)]
            k16 = sbp("k16", [128, 3, 16])
            tp = [psp(f"tp{i}", [128, 512]) for i in range(2)]
            qp = [psp(f"qp{i}", [128, 512]) for i in range(2)]
            scp = psp("scp", [128, 2048])

            ph.op("sp", lambda e: e.dma_start(out=A2bc[:], in_=modbc_d[:, 4 * D:5 * D]), writes=["A2bc"], dma=True)
            ph.op("sp", lambda e: e.dma_start(out=sh2bc[:], in_=modbc_d[:, 3 * D:4 * D]), writes=["sh2bc"], dma=True)
            ph.op("sp", lambda e: e.dma_start(out=S[:], in_=peer_sk.rearrange("j n d -> n j d")), writes=["S"], dma=True)
            for jg in range(4):
                def trs(e, jg=jg):
                    last = None
                    for jj in range(4):
                        last = e.transpose(out=tp[jg % 2][:, jj * 128:(jj + 1) * 128], in_=S[:, 4 * jg + jj, :], identity=ident[:])
                    return last
                ph.op("pe", trs, reads=["S"], writes=[("tp", jg % 2)])
                ph.op("act", lambda e, jg=jg: e.activation(out=skT[:, 4 * jg:4 * jg + 4, :],
                                                          in_=tp[jg % 2][:].rearrange("p (a c) -> p a c", a=4), func=AF.Copy),
                      reads=[("tp", jg % 2)], writes=["skT"])
            ph.op("dve", lambda e: e.tensor_copy(out=k16[:, 0, :], in_=iota[:, 0:16]), writes=["k16a"])
            ph.op("dve", lambda e: e.tensor_scalar(out=k16[:, 1, :], in0=iota[:, 0:16], scalar1=16.0, scalar2=None, op0=ALU.mult),
                  writes=["k16b"])
            ph.op("dve", lambda e: e.tensor_scalar(out=k16[:, 2, :], in0=iota[:, 0:16], scalar1=16.0, scalar2=16.0, op0=ALU.mult, op1=ALU.add),
                  writes=["k16c"])

            wplan = [j for g in range(4) for j in range(16)]
            wctr = [0, 0]
            idxfS = [idxf, sbp("idxf1", [128, 16, 16])]
            cvalS = [cval, sbp("cval1", [128, 8, 16])]
            cidxS = [cidx, sbp("cidx1", [128, 8, 16], U32)]
            pending = []
            tn = [0]

            def pump():
                if pending:
                    pending.pop(0)()

            def flush():
                while pending:
                    pending.pop(0)()

            def issue_wq():
                while wctr[0] < len(wplan) and wctr[0] < wctr[1] + 3:
                    n_ = wctr[0]
                    wctr[0] += 1
                    ph.op("pool", lambda e, s_=n_ % 4, j=wplan[n_]: e.dma_start(
                        out=wqs[s_][:], in_=peer_wq.rearrange("(k p) c -> p k c", p=128)[:, :, j * 128:(j + 1) * 128]),
                        writes=[("wqs", n_ % 4)], dma=True)

            tctr = 0
            for g in range(4):
                for t in range(4):
                    s = tctr % 2
                    tctr += 1
                    r0 = g * 512 + t * 128
                    ph.op("sp", lambda e, s=s, r0=r0: e.dma_start(out=xt[s][:], in_=x1_d[r0:r0 + 128, :]), writes=[("xt", s)], dma=True)
                    ph.op("act", lambda e, s=s: e.activation(out=junk[:], in_=xt[s][:], func=AF.Square, accum_out=ss[:, s:s + 1]),
                          reads=[("xt", s)], writes=["junk", ("ss", s)])
                    ph.op("act", lambda e, s=s: e.activation(out=rs[:, s:s + 1], in_=ss[:, s:s + 1], func=AF.Sqrt, scale=1.0 / D, bias=EPS),
                          reads=[("ss", s)], writes=[("rs", s)])
                    ph.op("dve", lambda e, s=s: e.reciprocal(out=rs[:, 2 + s:3 + s], in_=rs[:, s:s + 1]), reads=[("rs", s)], writes=[("rs2", s)])
                    ph.op("dve", lambda e, s=s: e.scalar_tensor_tensor(out=xn[s][:], in0=xt[s][:], scalar=rs[:, 2 + s:3 + s], in1=A2bc[:],
                                                                      op0=ALU.mult, op1=ALU.mult),
                          reads=[("xt", s), ("rs2", s), "A2bc"], writes=[("xn", s)])
                    ph.op("pool", lambda e, s=s: e.tensor_tensor(out=xn[s][:], in0=xn[s][:], in1=sh2bc[:], op=ALU.add),
                          reads=[("xn", s), "sh2bc"], writes=[("xn", s)])
                    for kg in range(4):
                        bk = kg % 2

                        def trs(e, s=s, kg=kg, bk=bk):
                            last = None
                            for kk in range(4):
                                k = 4 * kg + kk
                                last = e.transpose(out=tp[bk][:, kk * 128:(kk + 1) * 128], in_=xn[s][:, k * 128:(k + 1) * 128], identity=ident[:])
                            return last
                        ph.op("pe", trs, reads=[("xn", s)], writes=[("tp", bk)])
                        if kg % 2 == 0:
                            ph.op("act", lambda e, kg=kg, bk=bk, t=t: e.activation(
                                out=h2T[:, 4 * kg:4 * kg + 4, t * 128:(t + 1) * 128],
                                in_=tp[bk][:].rearrange("p (a c) -> p a c", a=4), func=AF.Copy),
                                reads=[("tp", bk)], writes=["h2T"])
                        else:
                            ph.op("dve", lambda e, kg=kg, bk=bk, t=t: e.tensor_copy(
                                out=h2T[:, 4 * kg:4 * kg + 4, t * 128:(t + 1) * 128],
                                in_=tp[bk][:].rearrange("p (a c) -> p a c", a=4)),
                                reads=[("tp", bk)], writes=["h2T"])
                ph.op("sp", lambda e, g=g: e.dma_start(out=h2T_d[:, :, g * 512:(g + 1) * 512].rearrange("k p n -> p k n"), in_=h2T[:]),
                      reads=["h2T"], writes=[("h2T_d", g)], dma=True, semkey=("st", "h2T"))
                for j in range(16):
                    issue_wq()
                    sl = wctr[1] % 4
                    wctr[1] += 1

                    def mm(e, sl=sl, j=j):
                        last = None
                        for k in range(16):
                            last = e.matmul(qp[j % 2][:], lhsT=wqs[sl][:, k, :], rhs=h2T[:, k, :], start=(k == 0), stop=(k == 15))
                        return last
                    ph.op("pe", mm, reads=[("wqs", sl), "h2T"], writes=[("qp", j % 2)])
                    issue_wq()
                    if j % 2 == 0:
                        ph.op("act", lambda e, j=j: e.activation(out=qT[:, j, :], in_=qp[j % 2][:], func=AF.Copy),
                              reads=[("qp", j % 2)], writes=[("qT", j)])
                    else:
                        ph.op("dve", lambda e, j=j: e.tensor_copy(out=qT[:, j, :], in_=qp[j % 2][:]),
                              reads=[("qp", j % 2)], writes=[("qT", j)])
                for t in range(4):
                    tok0 = g * 512 + t * 128
                    bs = tn[0] % 2
                    tn[0] += 1

                    def scm(e, t=t):
                        last = None
                        for j in range(16):
                            last = e.matmul(scp[:, j * 128:(j + 1) * 128], lhsT=qT[:, j, t * 128:(t + 1) * 128], rhs=skT[:, j, :],
                                            start=True, stop=True)
                        return last
                    ph.op("pe", scm, reads=[("qT", j) for j in range(16)] + ["skT"], writes=["scp"])
                    ph.op("act", lambda e: e.activation(out=S[:].rearrange("p a b -> p (a b)"), in_=scp[:], func=AF.Copy),
                          reads=["scp"], writes=["S"])
                    for j in range(16):
                        ph.op("dve", lambda e, j=j: e.max(out=vals[:, j, 0:8], in_=S[:, j, :]), reads=["S"], writes=[("v1", j)])
                    pump()
                    for j in range(16):
                        ph.op("dve", lambda e, j=j: e.match_replace(out=S2[:, j, :], in_to_replace=vals[:, j, 0:8], in_values=S[:, j, :], imm_value=-1e30),
                              reads=["S", ("v1", j)], writes=[("S2", j)])
                    pump()
                    for j in range(16):
                        ph.op("dve", lambda e, j=j: e.max(out=vals[:, j, 8:16], in_=S2[:, j, :]), reads=[("S2", j)], writes=[("v2", j)])
                    pump()
                    for j in range(16):
                        ph.op("dve", lambda e, j=j: e.max_index(out=idx[:, j, 0:8], in_max=vals[:, j, 0:8], in_values=S[:, j, :]),
                              reads=["S", ("v1", j)], writes=[("i1", j)])
                    pump()
                    for j in range(16):
                        ph.op("dve", lambda e, j=j: e.max_index(out=idx[:, j, 8:16], in_max=vals[:, j, 8:16], in_values=S2[:, j, :]),
                              reads=[("S2", j), ("v2", j)], writes=[("i2", j)])
                    pump()
                    allv = [("v1", j) for j in range(16)] + [("v2", j) for j in range(16)]
                    alli = [("i1", j) for j in range(16)] + [("i2", j) for j in range(16)]
                    ph.op("pool", lambda e, bs=bs: e.tensor_copy(out=idxfS[bs][:], in_=idx[:]), reads=alli, writes=[("idxf", bs)])
                    v4 = vals[:].rearrange("p (h q) k -> p h q k", q=2)
                    ph.op("pool", lambda e, v4=v4: e.tensor_tensor(
                        out=cand[:].rearrange("p h (a b) -> p h a b", a=16),
                        in0=v4[:, :, 0, :].unsqueeze(3).to_broadcast([128, 8, 16, 16]),
                        in1=v4[:, :, 1, :].unsqueeze(2).to_broadcast([128, 8, 16, 16]), op=ALU.add),
                        reads=allv, writes=["cand"])
                    pump()
                    cvl, cix = cvalS[bs], cidxS[bs]
                    for h in range(8):
                        ph.op("dve", lambda e, h=h, cvl=cvl: e.max(out=cvl[:, h, 0:8], in_=cand[:, h, :]), reads=["cand"], writes=[("c1", bs, h)])
                    pump()
                    for h in range(8):
                        ph.op("dve", lambda e, h=h, cvl=cvl: e.match_replace(out=cand2[:, h, :], in_to_replace=cvl[:, h, 0:8], in_values=cand[:, h, :], imm_value=-1e30),
                              reads=["cand", ("c1", bs, h)], writes=[("cand2", h)])
                    pump()
                    for h in range(8):
                        ph.op("dve", lambda e, h=h, cvl=cvl: e.max(out=cvl[:, h, 8:16], in_=cand2[:, h, :]), reads=[("cand2", h)], writes=[("c2", bs, h)])
                    pump()
                    for h in range(8):
                        ph.op("dve", lambda e, h=h, cvl=cvl, cix=cix: e.max_index(out=cix[:, h, 0:8], in_max=cvl[:, h, 0:8], in_values=cand[:, h, :]),
                              reads=["cand", ("c1", bs, h)], writes=[("ci1", bs, h)])
                    pump()
                    for h in range(8):
                        ph.op("dve", lambda e, h=h, cvl=cvl, cix=cix: e.max_index(out=cix[:, h, 8:16], in_max=cvl[:, h, 8:16], in_values=cand2[:, h, :]),
                              reads=[("cand2", h), ("c2", bs, h)], writes=[("ci2", bs, h)])
                    flush()

                    def make_b(bs=bs, t=t, tok0=tok0):
                        cvl, cix, ixf = cvalS[bs], cidxS[bs], idxfS[bs]
                        allc = [("c1", bs, h) for h in range(8)] + [("c2", bs, h) for h in range(8)]
                        allci = [("ci1", bs, h) for h in range(8)] + [("ci2", bs, h) for h in range(8)]
                        cb = cif[:].unsqueeze(3).to_broadcast([128, 8, 16, 16])
                        lo = k16[:, 1, :].unsqueeze(1).unsqueeze(1).to_broadcast([128, 8, 16, 16])
                        hi = k16[:, 2, :].unsqueeze(1).unsqueeze(1).to_broadcast([128, 8, 16, 16])
                        io = k16[:, 0, :].unsqueeze(1).unsqueeze(1).to_broadcast([128, 8, 16, 16])
                        i4 = ixf[:].rearrange("p (h q) k -> p h q k", q=2)
                        st_ = t % 2
                        steps = []

                        def s1():
                            ph.op("pool", lambda e: e.tensor_tensor(out=ex[:], in0=cvl[:], in1=cvl[:, :, 0:1].to_broadcast([128, 8, 16]), op=ALU.subtract),
                                  reads=allc, writes=["ex"])
                            ph.op("act", lambda e: e.activation(out=ex[:], in_=ex[:], func=AF.Exp), reads=["ex"], writes=["ex"])
                            ph.op("pool", lambda e: e.tensor_copy(out=cif[:], in_=cix[:]), reads=allci, writes=["cif"])

                        def s2():
                            ph.op("dve", lambda e: e.tensor_reduce(out=esum[:], in_=ex[:], axis=AX.X, op=ALU.add), reads=["ex"], writes=["esum"])
                            ph.op("dve", lambda e: e.reciprocal(out=esum[:], in_=esum[:]), reads=["esum"], writes=["esum"])
                            ph.op("pool", lambda e: e.tensor_tensor(out=slot[:, 2, :].rearrange("p (h k) -> p h k", h=8), in0=ex[:],
                                                                    in1=esum[:].unsqueeze(2).to_broadcast([128, 8, 16]), op=ALU.mult),
                                  reads=["ex", "esum"], writes=["slot_g"])
                            ph.op("dve", lambda e: e.tensor_tensor(out=e1[:], in0=cb, in1=lo, op=ALU.is_ge), reads=["cif", "k16b"], writes=["e1"])
                            ph.op("dve", lambda e: e.tensor_tensor(out=e2[:], in0=cb, in1=hi, op=ALU.is_ge), reads=["cif", "k16c"], writes=["e2"])
                            ph.op("dve", lambda e: e.tensor_tensor(out=e1[:], in0=e1[:], in1=e2[:], op=ALU.subtract), reads=["e1", "e2"], writes=["e1"])
                            ph.op("pool", lambda e: e.tensor_tensor(out=e2[:], in0=e1[:], in1=i4[:, :, 0, :].unsqueeze(2).to_broadcast([128, 8, 16, 16]), op=ALU.mult),
                                  reads=["e1", ("idxf", bs)], writes=["e2"])

                        def s3():
                            ph.op("dve", lambda e: e.tensor_reduce(out=slot[:, 0, :].rearrange("p (h k) -> p h k", h=8), in_=e2[:], axis=AX.X, op=ALU.add),
                                  reads=["e2"], writes=["slot_r"])
                            ph.op("pool", lambda e: e.tensor_tensor(out=e2[:], in0=e1[:], in1=io, op=ALU.mult), reads=["e1", "k16a"], writes=["e2"])

                        def s4():
                            ph.op("dve", lambda e: e.tensor_reduce(out=i_f[:], in_=e2[:], axis=AX.X, op=ALU.add), reads=["e2"], writes=["i_f"])
                            ph.op("dve", lambda e: e.scalar_tensor_tensor(out=jjf[:], in0=i_f[:], scalar=-16.0, in1=cif[:], op0=ALU.mult, op1=ALU.add),
                                  reads=["i_f", "cif"], writes=["jjf"])
                            ph.op("dve", lambda e: e.tensor_tensor(out=e1[:], in0=jjf[:].unsqueeze(3).to_broadcast([128, 8, 16, 16]), in1=io, op=ALU.is_equal),
                                  reads=["jjf", "k16a"], writes=["e1"])
                            ph.op("pool", lambda e: e.tensor_tensor(out=e2[:], in0=e1[:], in1=i4[:, :, 1, :].unsqueeze(2).to_broadcast([128, 8, 16, 16]), op=ALU.mult),
                                  reads=["e1", ("idxf", bs)], writes=["e2"])

                        def s5():
                            ph.op("dve", lambda e: e.tensor_reduce(out=slot[:, 1, :].rearrange("p (h k) -> p h k", h=8), in_=e2[:], axis=AX.X, op=ALU.add),
                                  reads=["e2"], writes=["slot_c"])

                            def trs(e):
                                last = None
                                for q in range(3):
                                    last = e.transpose(out=tp[0][:, q * 128:(q + 1) * 128], in_=slot[:, q, :], identity=ident[:])
                                return last
                            ph.op("pe", trs, reads=["slot_r", "slot_c", "slot_g"], writes=[("tp", 0)])
                            ph.op("act", lambda e: e.activation(out=slT[st_][:], in_=tp[0][:, 0:384].rearrange("p (a c) -> p a c", a=3), func=AF.Copy),
                                  reads=[("tp", 0)], writes=[("slT", st_)])
                            ph.op("sp", lambda e: e.dma_start(out=slots_d[:, :, tok0:tok0 + 128].rearrange("q p n -> p q n"), in_=slT[st_][:]),
                                  reads=[("slT", st_)], writes=[("slots_d", tok0)], dma=True, semkey=("st", "slT", st_))
                        return [s1, s2, s3, s4, s5]
                    pending.extend(make_b())
                    if t == 3:
                        flush()
            ph.emit()
        if stop_after == "D":
            _dbg_copy3(nc, dbg, slots_d)
            return nc

        with ExitStack() as ph_es:
            ph = Phase(nc, "E")
            def sbp(nm, shape, dt=F32):
                return ph_es.enter_context(nc.sbuf_tensor("e_" + nm, list(shape), dt))
            def psp(nm, shape, dt=F32):
                return ph_es.enter_context(nc.psum_tensor("e_" + nm, list(shape), dt))
            slT = [sbp(f"slT{i}", [128, 3, 128]) for i in range(2)]
            Ab = [sbp(f"Ab{i}", [128, 128], BF16) for i in range(8)]
            Bb = [sbp(f"Bb{i}", [128, 128], BF16) for i in range(8)]
            Gs = [sbp(f"Gs{i}", [128, 32, 128, 4], BF16) for i in range(2)]
            Gp = [psp(f"Gp{i}", [128, 512]) for i in range(4)]
            for t in range(T // 128):
                s = t % 2
                ph.op("sp", lambda e, s=s, t=t: e.dma_start(out=slT[s][:], in_=slots_d[:, :, t * 128:(t + 1) * 128].rearrange("q p n -> p q n")),
                      writes=[("slT", s)], dma=True)
                for n4 in range(32):
                    bk = n4 % 4
                    for q in range(4):
                        n = n4 * 4 + q
                        a_ = n % 8
                        ph.op("dve", lambda e, s=s, n=n, a_=a_: e.tensor_scalar(out=Ab[a_][:], in0=iota_bf[:], scalar1=slT[s][:, 0, n:n + 1],
                                                                               scalar2=slT[s][:, 2, n:n + 1], op0=ALU.is_equal, op1=ALU.mult),
                              reads=[("slT", s)], writes=[("Ab", a_)])
                        ph.op("dve", lambda e, s=s, n=n, a_=a_: e.tensor_scalar(out=Bb[a_][:], in0=iota_bf[:], scalar1=slT[s][:, 1, n:n + 1],
                                                                               scalar2=None, op0=ALU.is_equal),
                              reads=[("slT", s)], writes=[("Bb", a_)])
                        ph.op("pe", lambda e, a_=a_, bk=bk, q=q: e.matmul(Gp[bk][:, q * 128:(q + 1) * 128], lhsT=Bb[a_][:], rhs=Ab[a_][:], start=True, stop=True),
                              reads=[("Ab", a_), ("Bb", a_)], writes=[("Gp", bk)])
                    ph.op("act", lambda e, s=s, n4=n4, bk=bk: e.activation(
                        out=Gs[s][:, :, n4 * 4:n4 * 4 + 4, :],
                        in_=Gp[bk][:].rearrange("p (q g r) -> p g q r", q=4, g=32, r=4), func=AF.Copy),
                        reads=[("Gp", bk)], writes=[("Gs", s)])
                for gq in range(4):
                    for g2 in range(2):
                        gi = gq * 2 + g2
                        ph.op("sp", lambda e, s=s, t=t, gi=gi: e.dma_start(out=G_ds[gi][:, :, t * 128:(t + 1) * 128, :],
                                                                          in_=Gs[s][:, gi * 4:(gi + 1) * 4, :, :]),
                              reads=[("Gs", s)], writes=[("G_d", t, gi)], dma=True, semkey=("st", "Gs", s, gq))
            ph.emit()

        if stop_after == "E":
            return nc
        y2_d = nc.dram_tensor("y2_d", [T, D], F32).ap()
        RGV = 4
        for hb in range(T // HB):
            with ExitStack() as ph_es:
                ph = Phase(nc, f"F{hb}")
                def sbp(nm, shape, dt=F32):
                    return ph_es.enter_context(nc.sbuf_tensor(f"f{hb}_" + nm, list(shape), dt))
                def psp(nm, shape, dt=F32):
                    return ph_es.enter_context(nc.psum_tensor(f"f{hb}_" + nm, list(shape), dt))
                h2T = sbp("h2T", [128, 16, HB], BF16)
                acc = sbp("acc", [128, HB // 128, D])
                ub = [sbp(f"ub{i}", [128, D], BF16) for i in range(3)]
                uT = [sbp(f"uT{i}", [128, 16, 128], BF16) for i in range(3)]
                vb = [sbp(f"vb{i}", [128, D], BF16) for i in range(8)]
                Gg = [sbp(f"Gg{i}", [128, HB, 4], BF16) for i in range(2)]
                gl = sbp("gl", [128, HB])
                W = [sbp(f"W{i}", [128, HB], BF16) for i in range(8)]
                tpu = [psp(f"tpu{i}", [128, 1024], BF16) for i in range(2)]
                actp = psp("actp", [128, HB])
                yp = [psp(f"yp{i}", [128, 1024]) for i in range(2)]
                ph.op("sp", lambda e: e.dma_start(out=h2T[:], in_=h2T_d[:, :, hb * HB:(hb + 1) * HB].rearrange("k p n -> p k n")),
                      writes=["h2T"], dma=True)
                nch = (F_GROUPS * RGV) if F_GROUPS else NEXP_CH
                ngroups = nch // RGV
                ypc = [0]

                def loadu(r):
                    if r >= nch:
                        return
                    rg, q4 = divmod(r, 4)
                    if q4 == 0:
                        ph.op("sp", lambda e, gs_=rg % 2, rg=rg: e.dma_start(out=Gg[gs_][:], in_=G_ds[rg // 4][:, rg % 4, hb * HB:(hb + 1) * HB, :]),
                              writes=[("Gg", rg % 2)], dma=True)
                    ph.op("pool", lambda e, r=r: e.dma_start(out=ub[r % 3][:], in_=peer_u[r * 128:(r + 1) * 128, :]), writes=[("ub", r % 3)], dma=True)

                def loadv(r):
                    if r >= nch:
                        return
                    ph.op("pool", lambda e, r=r: e.dma_start(out=vb[r % 8][:], in_=peer_v[r * 128:(r + 1) * 128, :]), writes=[("vb", r % 8)], dma=True)

                def front(r):
                    if r >= nch or F_MODE == 'l':
                        return
                    u_ = r % 3
                    for kg in range(2):
                        def trs(e, u_=u_, kg=kg):
                            last = None
                            for kk in range(8):
                                k = 8 * kg + kk
                                last = e.transpose(out=tpu[kg][:, kk * 128:(kk + 1) * 128], in_=ub[u_][:, k * 128:(k + 1) * 128], identity=ident_bf[:])
                            return last
                        ph.op("pe", trs, reads=[("ub", u_)], writes=[("tpu", kg)])
                        if kg == 0:
                            ph.op("act", lambda e, u_=u_, kg=kg: e.activation(
                                out=uT[u_][:, 8 * kg:8 * kg + 8, :], in_=tpu[kg][:].rearrange("p (a c) -> p a c", a=8), func=AF.Copy),
                                reads=[("tpu", kg)], writes=[("uT", u_, kg)])
                        else:
                            ph.op("dve", lambda e, u_=u_, kg=kg: e.tensor_copy(
                                out=uT[u_][:, 8 * kg:8 * kg + 8, :], in_=tpu[kg][:].rearrange("p (a c) -> p a c", a=8)),
                                reads=[("tpu", kg)], writes=[("uT", u_, kg)])

                def mid(r):
                    if F_MODE == 'l':
                        return
                    u_ = r % 3
                    w_ = r % 8
                    rg, q4 = divmod(r, 4)
                    gs_ = rg % 2

                    def mm(e, u_=u_):
                        last = None
                        for k in range(16):
                            for tg in range(HB // 512):
                                last = e.matmul(actp[:, tg * 512:(tg + 1) * 512], lhsT=uT[u_][:, k, :], rhs=h2T[:, k, tg * 512:(tg + 1) * 512],
                                                start=(k == 0), stop=(k == 15))
                        return last
                    ph.op("pe", mm, reads=[("uT", u_, 0), ("uT", u_, 1), "h2T"], writes=[("actp", tg) for tg in range(HB // 512)])
                    ph.op("act", lambda e: e.activation(out=gl[:], in_=actp[:], func=AF.Gelu_apprx_tanh),
                          reads=[("actp", tg) for tg in range(HB // 512)], writes=["gl"])
                    ph.op("dve", lambda e, w_=w_, gs_=gs_, q4=q4: e.tensor_tensor(out=W[w_][:], in0=gl[:], in1=Gg[gs_][:, :, q4], op=ALU.mult),
                          reads=["gl", ("Gg", gs_)], writes=[("W", w_)])

                def vphase(grp):
                    if F_MODE == 'l':
                        return
                    wbase = (grp % 2) * RGV
                    for t in range(HB // 128):
                        for dh in range(2):
                            yb = ypc[0] % 2
                            ypc[0] += 1

                            def vmm(e, t=t, dh=dh, yb=yb, wbase=wbase):
                                last = None
                                for qq in range(RGV):
                                    for dg in range(2):
                                        c0 = dh * 1024 + dg * 512
                                        last = e.matmul(yp[yb][:, dg * 512:(dg + 1) * 512], lhsT=W[wbase + qq][:, t * 128:(t + 1) * 128],
                                                        rhs=vb[wbase + qq][:, c0:c0 + 512], start=(qq == 0), stop=(qq == RGV - 1))
                                return last
                            ph.op("pe", vmm, reads=[("W", wbase + qq) for qq in range(RGV)] + [("vb", wbase + qq) for qq in range(RGV)],
                                  writes=[("yp", yb)])
                            if grp == 0:
                                ph.op("dve", lambda e, t=t, dh=dh, yb=yb: e.tensor_copy(out=acc[:, t, dh * 1024:(dh + 1) * 1024], in_=yp[yb][:]),
                                      reads=[("yp", yb)], writes=[("acc", t, dh)])
                            else:
                                ph.op("dve", lambda e, t=t, dh=dh, yb=yb: e.tensor_tensor(out=acc[:, t, dh * 1024:(dh + 1) * 1024], in0=yp[yb][:],
                                                                                        in1=acc[:, t, dh * 1024:(dh + 1) * 1024], op=ALU.add),
                                      reads=[("yp", yb), ("acc", t, dh)], writes=[("acc", t, dh)])

                for r in range(3):
                    loadu(r)
                front(0)
                front(1)
                for r in range(nch):
                    mid(r)
                    loadv(r)
                    loadu(r + 3)
                    front(r + 2)
                    if r % RGV == 0 and r >= RGV:
                        vphase(r // RGV - 1)
                vphase(ngroups - 1)
                for t in range(HB // 128 if F_MODE == '' else 0):
                    ph.op("sp", lambda e, t=t: e.dma_start(out=y2_d[hb * HB + t * 128:hb * HB + (t + 1) * 128, :], in_=acc[:, t, :]),
                          reads=[("acc", t, 0), ("acc", t, 1)], writes=[("y2_d", t)], dma=True, semkey=("st", "acc", t % 2))
                ph.emit()
        if stop_after == "F":
            _dbg_copy(nc, dbg, y2_d, rows=T)
            return nc

        with ExitStack() as ph_es:
            ph = Phase(nc, "G")
            def sbp(nm, shape, dt=F32):
                return ph_es.enter_context(nc.sbuf_tensor("gq_" + nm, list(shape), dt))
            gg = sbp("gg", [128, D])
            yt = [sbp(f"yt{i}", [128, D]) for i in range(2)]
            xt = [sbp(f"xt{i}", [128, D]) for i in range(2)]
            junk = sbp("junk", [128, D], BF16)
            ss = sbp("ss", [128, 4])
            rs = sbp("rs", [128, 4])
            ph.op("sp", lambda e: e.dma_start(out=gg[:], in_=modbc_d[:, 5 * D:6 * D]), writes=["gg"], dma=True)
            for t in range(T // 128):
                s = t % 2
                ph.op("sp", lambda e, s=s, t=t: e.dma_start(out=yt[s][:], in_=y2_d[t * 128:(t + 1) * 128, :]), writes=[("yt", s)], dma=True)
                ph.op("sp", lambda e, s=s, t=t: e.dma_start(out=xt[s][:], in_=x1_d[t * 128:(t + 1) * 128, :]), writes=[("xt", s)], dma=True)
                ph.op("act", lambda e, s=s: e.activation(out=junk[:], in_=yt[s][:], func=AF.Square, accum_out=ss[:, s:s + 1]),
                      reads=[("yt", s)], writes=["junk", ("ss", s)])
                ph.op("act", lambda e, s=s: e.activation(out=rs[:, s:s + 1], in_=ss[:, s:s + 1], func=AF.Sqrt, scale=1.0 / D, bias=EPS),
                      reads=[("ss", s)], writes=[("rs", s)])
                ph.op("dve", lambda e, s=s: e.reciprocal(out=rs[:, 2 + s:3 + s], in_=rs[:, s:s + 1]), reads=[("rs", s)], writes=[("rs2", s)])
                ph.op("dve", lambda e, s=s: e.scalar_tensor_tensor(out=yt[s][:], in0=yt[s][:], scalar=rs[:, 2 + s:3 + s], in1=gg[:],
                                                                  op0=ALU.mult, op1=ALU.mult),
                      reads=[("yt", s), ("rs2", s), "gg"], writes=[("yt", s)])
                ph.op("pool", lambda e, s=s: e.tensor_tensor(out=yt[s][:], in0=yt[s][:], in1=xt[s][:], op=ALU.add),
                      reads=[("yt", s), ("xt", s)], writes=[("yt", s)])
                ph.op("sp", lambda e, s=s, t=t: e.dma_start(out=out[t * 128:(t + 1) * 128, :], in_=yt[s][:]),
                      reads=[("yt", s)], writes=[("out", t)], dma=True, semkey=("st", "yt", s))
            ph.emit()
    return nc


def _dbg_copy3(nc, dbg, slots_d):
    with ExitStack() as es:
        buf = es.enter_context(nc.sbuf_tensor("dbgbuf3", [128, T], F32))
        s1 = es.enter_context(nc.semaphore("dbg3_s1"))
        with nc.Block() as block:
            @block.sync
            def _(e):
                n = 0
                for q in range(3):
                    e.dma_start(out=buf[:], in_=slots_d[q]).then_inc(s1, 16)
                    n += 16
                    e.wait_ge(s1, n)
                    e.dma_start(out=dbg[q * 128:(q + 1) * 128, :], in_=buf[:]).then_inc(s1, 16)
                    n += 16
                    e.wait_ge(s1, n)


def _dbg_copy(nc, dbg, src, rows):
    with ExitStack() as es:
        buf = es.enter_context(nc.sbuf_tensor("dbgbuf", [128, D], F32))
        s1 = es.enter_context(nc.semaphore("dbg_s1"))
        with nc.Block() as block:
            @block.sync
            def _(e):
                n = 0
                for t in range(rows // 128):
                    e.dma_start(out=buf[:], in_=src[t * 128:(t + 1) * 128, :]).then_inc(s1, 16)
                    n += 16
                    e.wait_ge(s1, n)
                    e.dma_start(out=dbg[t * 128:(t + 1) * 128, :], in_=buf[:]).then_inc(s1, 16)
                    n += 16
                    e.wait_ge(s1, n)


def _make_cols(inp, b, j):
    cols = np.zeros((128, NCOL), np.float32)

    def put(c0, vec):
        v = np.asarray(vec, np.float32).reshape(-1, 128)
        cols[:, c0:c0 + v.shape[0]] = v.T
    put(C_C, inp["c"][b])
    for tap in range(4):
        put(C_LW + tap * 8, inp["lru_conv_w"][0, tap])
    put(C_LB, inp["lru_conv_b"][0])
    put(C_BA, inp["lru_ba"][0])
    put(C_BX, inp["lru_bx"][0])
    put(C_LAM, inp["lru_lambda"][0])
    for tap in range(31):
        put(C_CW + tap * 8, inp["conf_dw_w"][0, tap])
    put(C_CB, inp["conf_dw_b"][0])
    put(C_LG, inp["conf_ln_g"][0])
    put(C_LNB, inp["conf_ln_b"][0])
    nvalid_blocks = (T * j) // TB
    for blk in range(NPRE // TB):
        cols[:, C_FL + blk] = 1.0 if blk >= (NPRE // TB - nvalid_blocks) else 0.0
    return cols


def make_in_maps(inp):
    x = np.ascontiguousarray(inp["x"], dtype=np.float32)
    shared = {
        "ident": np.eye(128, dtype=np.float32),
        "iota": np.tile(np.arange(128, dtype=np.float32)[None, :], (128, 1)),
        "w_mod": np.ascontiguousarray(inp["w_mod"][0]),
        "b_mod": np.ascontiguousarray(inp["b_mod"][0]),
        "g_pre_mix": np.ascontiguousarray(inp["g_pre_mix"][0]),
        "g_post_mix": np.ascontiguousarray(inp["g_post_mix"][0]),
        "g_pre_ffn": np.ascontiguousarray(inp["g_pre_ffn"][0]),
        "g_post_ffn": np.ascontiguousarray(inp["g_post_ffn"][0]),
        "w_in": np.ascontiguousarray(inp["w_in"][0]),
        "lru_wa": np.ascontiguousarray(inp["lru_wa"][0]),
        "lru_wx": np.ascontiguousarray(inp["lru_wx"][0]),
        "w_out": np.ascontiguousarray(inp["w_out"][0]),
        "peer_wq": np.ascontiguousarray(inp["peer_wq"][0]),
        "peer_sk": np.ascontiguousarray(inp["peer_subkeys"][0].reshape(16, 128, 128)),
        "peer_u": np.ascontiguousarray(inp["peer_u"][0]),
        "peer_v": np.ascontiguousarray(inp["peer_v"][0]),
    }
    maps = []
    for c in range(8):
        b, j = divmod(c, 4)
        m = dict(shared)
        m["x_main"] = np.ascontiguousarray(x[b, T * j:T * (j + 1)])
        pre = np.zeros((NPRE, D), np.float32)
        nv = T * j
        if nv:
            pre[NPRE - nv:] = x[b, 0:nv]
        m["x_pre"] = pre
        m["cols"] = _make_cols(inp, b, j)
        maps.append(m)
    return maps


def kernel(**inputs):
    nc = build_program()
    maps = make_in_maps(inputs)
    res = run_bass_kernel_spmd(nc, maps, core_ids=list(range(8)))
    out = np.empty((2, 8192, D), np.float32)
    for c in range(8):
        b, j = divmod(c, 4)
        out[b, T * j:T * (j + 1)] = res.results[c]["out"]
    return out
```

```python
import numpy as np
from contextlib import ExitStack
import concourse.bass as bass
import concourse.mybir as mybir
from concourse.bass_utils import run_bass_kernel_spmd

F32 = mybir.dt.float32
BF16 = mybir.dt.bfloat16
U32 = mybir.dt.uint32
AF = mybir.ActivationFunctionType
ALU = mybir.AluOpType
AX = mybir.AxisListType

D = 2048
KC = 16
T = 2048
NPRE = 6144
TB = 1024
EPS = 1e-6
NEXP_CH = 128
RG = 4
HB = 1024
F_GROUPS = 0
F_MODE = ''

C_C = 0
C_LW = 16
C_LB = 48
C_BA = 56
C_BX = 64
C_LAM = 72
C_CW = 80
C_CB = 328
C_LG = 336
C_LNB = 344
C_FL = 352
NCOL = 358


class _Op:
    __slots__ = ("eng", "fn", "deps", "need_inc", "dma", "semkey", "token")


class Phase:
    ENG = ("sp", "pool", "act", "dve", "pe")

    gpool = None

    def __init__(self, nc, name):
        self.nc = nc
        self.name = name
        self.ops = []
        self.lw = {}
        self.rd = {}

    def op(self, eng, fn, reads=(), writes=(), dma=False, semkey=None):
        o = _Op()
        o.eng = eng
        o.fn = fn
        o.dma = dma
        o.need_inc = dma
        o.token = None
        o.semkey = semkey if semkey is not None else (("dma", writes[0]) if dma else None)
        deps = []
        for k in reads:
            w = self.lw.get(k)
            if w is not None:
                deps.append(w)
        for k in writes:
            w = self.lw.get(k)
            if w is not None:
                deps.append(w)
            deps.extend(self.rd.get(k, ()))
        o.deps = []
        seen = set()
        for d in deps:
            if id(d) not in seen and d is not o:
                seen.add(id(d))
                o.deps.append(d)
                d.need_inc = True
        for k in writes:
            self.lw[k] = o
            self.rd[k] = []
        for k in reads:
            self.rd.setdefault(k, []).append(o)
        self.ops.append(o)
        return o

    def emit(self):
        nc = self.nc
        gp = self.gpool
        local = {}
        for o in self.ops:
            if o.dma:
                k = o.semkey
                local[k] = local.get(k, 0) + 16
                o.token = (k, local[k])
            elif o.need_inc:
                k = ("eng", o.eng)
                local[k] = local.get(k, 0) + 1
                o.token = (k, local[k])
        swkeys = set(o.semkey for o in self.ops if o.dma and o.eng == "pool")
        slot = {}
        nhw = nsw = 0
        for k in local:
            if k[0] == "eng":
                slot[k] = self.ENG.index(k[1])
                continue
            lst = gp["sw"] if k in swkeys else gp["hw"]
            n_ = nsw if k in swkeys else nhw
            if n_ >= len(lst):
                lst.append(None)
            if k in swkeys:
                nsw += 1
            else:
                nhw += 1
            slot[k] = ("sw" if k in swkeys else "hw", n_)
        def getslot(sl):
            if isinstance(sl, int):
                while len(gp["sems"]) < 5:
                    i = len(gp["sems"])
                    gp["sems"].append(gp["stack"].enter_context(nc.semaphore(f"gsem{i}")))
                    gp["counts"].append(0)
                return sl
            kind, n_ = sl
            lst = gp[kind]
            if lst[n_] is None:
                i = len(gp["sems"])
                gp["sems"].append(gp["stack"].enter_context(nc.semaphore(f"gsem{i}")))
                gp["counts"].append(0)
                lst[n_] = i
            return lst[n_]
        getslot(0)
        slot = {k: getslot(v) for k, v in slot.items()}
        base = {k: gp["counts"][slot[k]] for k in local}
        sems = {k: gp["sems"][slot[k]] for k in local}
        by_eng = {e: [o for o in self.ops if o.eng == e] for e in self.ENG}
        with nc.Block() as block:
            reg = {"sp": block.sync, "pool": block.gpsimd, "act": block.scalar,
                   "dve": block.vector, "pe": block.tensor}
            for ename in self.ENG:
                ops = by_eng[ename]

                def body(e, ops=ops):
                    waited = {}
                    for o in ops:
                        need = {}
                        for d in o.deps:
                            if d.eng == "pe" and o.eng == "pe" and not d.dma and not o.dma:
                                continue
                            s, v = d.token
                            if need.get(s, 0) < v:
                                need[s] = v
                        for s, v in need.items():
                            if waited.get(s, 0) < v:
                                e.wait_ge(sems[s], base[s] + v)
                                waited[s] = v
                        ins = o.fn(e)
                        if o.dma:
                            ins.then_inc(sems[o.semkey], 16)
                        elif o.need_inc:
                            ins.then_inc(sems[("eng", o.eng)], 1)
                    for s, v in local.items():
                        if waited.get(s, 0) < v:
                            e.wait_ge(sems[s], base[s] + v)
                reg[ename](body)
        for k, v in local.items():
            gp["counts"][slot[k]] += v


def _col(cols, c):
    return cols[:, c:c + 1]


def build_program(stop_after=None):
    nc = bass.Bass("TRN2", target_bir_lowering=False)

    def din(name, shape, dt=F32):
        return nc.dram_tensor(name, list(shape), dt, kind="ExternalInput").ap()

    x_main = din("x_main", [T, D])
    x_pre = din("x_pre", [NPRE, D])
    cols_d = din("cols", [128, NCOL])
    ident_d = din("ident", [128, 128])
    iota_d = din("iota", [128, 128])
    w_mod = din("w_mod", [D, 6 * D])
    b_mod = din("b_mod", [6 * D])
    g_pre_mix = din("g_pre_mix", [D])
    g_post_mix = din("g_post_mix", [D])
    g_pre_ffn = din("g_pre_ffn", [D])
    g_post_ffn = din("g_post_ffn", [D])
    w_in = din("w_in", [D, 2 * D])
    lru_wa = din("lru_wa", [8, 128, 128])
    lru_wx = din("lru_wx", [8, 128, 128])
    w_out = din("w_out", [D, D])
    peer_wq = din("peer_wq", [D, D])
    peer_sk = din("peer_sk", [16, 128, 128])
    peer_u = din("peer_u", [16384, D])
    peer_v = din("peer_v", [16384, D])
    out = nc.dram_tensor("out", [T, D], F32, kind="ExternalOutput").ap()

    modbc_d = nc.dram_tensor("modbc_d", [128, 6 * D], F32).ap()
    catT_d = nc.dram_tensor("catT_d", [16, 128, T], BF16).ap()
    x1_d = nc.dram_tensor("x1_d", [T, D], F32).ap()
    h2T_d = nc.dram_tensor("h2T_d", [16, 128, T], BF16).ap()
    slots_d = nc.dram_tensor("slots_d", [3, 128, T], F32).ap()
    G_ds = [nc.dram_tensor(f"G_d{i}", [128, 4, T, 4], BF16).ap() for i in range(8)]

    dbg = None
    if stop_after is not None:
        dbg = nc.dram_tensor("dbg", [T, D], F32, kind="ExternalOutput").ap()

    with ExitStack() as outer:
        Phase.gpool = {"sems": [], "counts": [], "stack": outer, "hw": [], "sw": []}

        def sb(name, shape, dt=F32):
            return outer.enter_context(nc.sbuf_tensor("g_" + name, list(shape), dt))
        cols = sb("cols", [128, NCOL])
        ident = sb("ident", [128, 128])
        iota = sb("iota", [128, 128])
        ones = sb("ones", [128, 128])
        ident_bf = sb("ident_bf", [128, 128], BF16)
        iota_bf = sb("iota_bf", [128, 128], BF16)
        carry = sb("carry", [128, 8, 3])
        zcarry = sb("zcarry", [128, 8, 30])
        state = sb("state", [128, 8])
        clc = sb("clc", [128, 16])

        with ExitStack() as ph_es:
            ph = Phase(nc, "A")
            def sbp(name, shape, dt=F32, es=ph_es):
                return es.enter_context(nc.sbuf_tensor("a_" + name, list(shape), dt))
            def psp(name, shape, es=ph_es):
                return es.enter_context(nc.psum_tensor("a_" + name, list(shape), F32))
            caT = sbp("caT", [128, 16])
            caTb = sbp("caTb", [128, 16, 128])
            wm = [sbp(f"wm{i}", [128, 16, 512]) for i in range(2)]
            bmb = [sbp(f"bmb{i}", [128, 512]) for i in range(2)]
            gb = [sbp(f"gb{i}", [128, 512]) for i in range(2)]
            mo = [sbp(f"mo{i}", [128, 512]) for i in range(2)]
            tmpA = sbp("tmpA", [128, 16])
            mps = [psp(f"mps{i}", [128, 512]) for i in range(2)]

            ph.op("sp", lambda e: e.dma_start(out=cols[:], in_=cols_d), writes=["cols"], dma=True)
            ph.op("sp", lambda e: e.dma_start(out=ident[:], in_=ident_d), writes=["ident"], dma=True)
            ph.op("sp", lambda e: e.dma_start(out=iota[:], in_=iota_d), writes=["iota"], dma=True)
            ph.op("pool", lambda e: e.memset(ones[:], 1.0), writes=["ones"])
            ph.op("dve", lambda e: e.tensor_copy(out=ident_bf[:], in_=ident[:]), reads=["ident"], writes=["ident_bf"])
            ph.op("dve", lambda e: e.tensor_copy(out=iota_bf[:], in_=iota[:]), reads=["iota"], writes=["iota_bf"])
            ph.op("pool", lambda e: e.memset(carry[:], 0.0), writes=["carry"])
            ph.op("pool", lambda e: e.memset(zcarry[:], 0.0), writes=["zcarry"])
            ph.op("pool", lambda e: e.memset(state[:], 0.0), writes=["state"])
            ph.op("act", lambda e: e.activation(out=caT[:], in_=cols[:, C_C:C_C + 16], func=AF.Silu),
                  reads=["cols"], writes=["caT"])
            ph.op("act", lambda e: e.activation(out=tmpA[:, 0:8], in_=cols[:, C_LAM:C_LAM + 8], func=AF.Exp, scale=-1.0),
                  reads=["cols"], writes=["tmpA"])
            ph.op("act", lambda e: e.activation(out=tmpA[:, 8:16], in_=tmpA[:, 0:8], func=AF.Ln, bias=1.0),
                  reads=["tmpA"], writes=["tmpA2"])
            ph.op("dve", lambda e: e.tensor_scalar(out=clc[:, 0:8], in0=tmpA[:, 8:16], scalar1=-8.0, scalar2=None, op0=ALU.mult),
                  reads=["tmpA2"], writes=["clc0"])
            ph.op("dve", lambda e: e.tensor_scalar(out=clc[:, 8:16], in0=tmpA[:, 8:16], scalar1=-16.0, scalar2=None, op0=ALU.mult),
                  reads=["tmpA2"], writes=["clc1"])
            for k in range(16):
                ph.op("dve", lambda e, k=k: e.tensor_copy(out=caTb[:, k, :], in_=caT[:, k:k + 1].to_broadcast([128, 128])),
                      reads=["caT"], writes=[("caTb", k)])
            gsrc = {1: g_pre_mix, 2: g_post_mix, 4: g_pre_ffn, 5: g_post_ffn}
            for n in range(24):
                s = n % 2
                sec = n // 4
                cb = (n % 4) * 512
                ph.op("sp", lambda e, n=n, s=s: e.dma_start(
                    out=wm[s][:], in_=w_mod.rearrange("(k p) c -> p k c", p=128)[:, :, n * 512:(n + 1) * 512]),
                    writes=[("wm", s)], dma=True)
                ph.op("sp", lambda e, n=n, s=s: e.dma_start(
                    out=bmb[s][:], in_=b_mod[n * 512:(n + 1) * 512].partition_broadcast(128)),
                    writes=[("bmb", s)], dma=True)
                if sec in gsrc:
                    ph.op("sp", lambda e, s=s, sec=sec, cb=cb: e.dma_start(
                        out=gb[s][:], in_=gsrc[sec][cb:cb + 512].partition_broadcast(128)),
                        writes=[("gb", s)], dma=True)

                def mm(e, s=s):
                    last = None
                    for k in range(16):
                        last = e.matmul(mps[s][:], lhsT=caTb[:, k, :], rhs=wm[s][:, k, :], start=(k == 0), stop=(k == 15))
                    return last
                ph.op("pe", mm, reads=[("wm", s)] + [("caTb", k) for k in range(16)], writes=[("mps", s)])
                if sec in (0, 3):
                    ph.op("dve", lambda e, s=s: e.tensor_tensor(out=mo[s][:], in0=mps[s][:], in1=bmb[s][:], op=ALU.add),
                          reads=[("mps", s), ("bmb", s)], writes=[("mo", s)])
                else:
                    ph.op("dve", lambda e, s=s: e.tensor_tensor(out=bmb[s][:], in0=mps[s][:], in1=bmb[s][:], op=ALU.add),
                          reads=[("mps", s), ("bmb", s)], writes=[("bmb", s)])
                    if sec in (1, 4):
                        ph.op("dve", lambda e, s=s: e.scalar_tensor_tensor(out=mo[s][:], in0=bmb[s][:], scalar=1.0, in1=gb[s][:],
                                                                          op0=ALU.add, op1=ALU.mult),
                              reads=[("bmb", s), ("gb", s)], writes=[("mo", s)])
                    else:
                        ph.op("dve", lambda e, s=s: e.tensor_tensor(out=mo[s][:], in0=bmb[s][:], in1=gb[s][:], op=ALU.mult),
                              reads=[("bmb", s), ("gb", s)], writes=[("mo", s)])
                ph.op("sp", lambda e, n=n, s=s: e.dma_start(out=modbc_d[:, n * 512:(n + 1) * 512], in_=mo[s][:]),
                      reads=[("mo", s)], writes=[("modbc_d", n)], dma=True, semkey=("st", "mo", s))
            ph.emit()

        if stop_after == "A":
            _dbg_copy(nc, dbg, modbc_d[:, 0:D], rows=128)
            return nc

        def mixer_phase(name, xsrc, nblocks, prefix):
            with ExitStack() as ph_es:
                ph = Phase(nc, name)
                def sbp(nm, shape, dt=F32):
                    return ph_es.enter_context(nc.sbuf_tensor(name + "_" + nm, list(shape), dt))
                def psp(nm, shape):
                    return ph_es.enter_context(nc.psum_tensor(name + "_" + nm, list(shape), F32))
                N = TB
                A1bc = sbp("A1bc", [128, D])
                sh1bc = sbp("sh1bc", [128, D])
                xt = [sbp(f"xt{i}", [128, D]) for i in range(2)]
                xn = [sbp(f"xn{i}", [128, D]) for i in range(2)]
                junk = sbp("junk", [128, D], BF16)
                ss = sbp("ss", [128, 4])
                rs = sbp("rs", [128, 4])
                hT = sbp("hT", [128, 16, N], BF16)
                wsl = [sbp(f"wsl{i}", [128, 16, 128], BF16) for i in range(4)]
                wga = sbp("wga", [128, 8, 128])
                wgx = sbp("wgx", [128, 8, 128])
                NBK = dict(xpad=2, xl=3, rr=1, ig=2, aa=2, a2=2, uu=1, hs=1) if prefix else dict(xpad=1, xl=1, rr=1, ig=1, aa=1, a2=1, uu=1, hs=1)
                xpadS = [sbp(f"xpad{j}", [128, N + 3]) for j in range(NBK["xpad"])]
                xpad = xpadS[0]
                cA = sbp("cA", [128, N])
                cB = sbp("cB", [128, N])
                xlS = [sbp(f"xl{j}", [128, N]) for j in range(NBK["xl"])]
                xl = xlS[0]
                rrS = [sbp(f"rr{j}", [128, N]) for j in range(NBK["rr"])]
                rr = rrS[0]
                igS = [sbp(f"ig{j}", [128, N]) for j in range(NBK["ig"])]
                ig = igS[0]
                aaS = [sbp(f"aa{j}", [128, N]) for j in range(NBK["aa"])]
                aa = aaS[0]
                a2S = [sbp(f"a2{j}", [128, N]) for j in range(NBK["a2"])]
                a2 = a2S[0]
                uuS = [sbp(f"uu{j}", [128, N]) for j in range(NBK["uu"])]
                uu = uuS[0]
                hsS = [sbp(f"hs{j}", [128, N]) for j in range(NBK["hs"])]
                hs = hsS[0]
                tp = [psp(f"tp{i}", [128, 512]) for i in range(2)]
                pp = psp("pp", [128, N])
                gpa = psp("gpa", [128, N])
                gpx = psp("gpx", [128, N])
                if not prefix:
                    zpad = sbp("zpad", [128, N + 30], BF16)
                    dgw = sbp("dgw", [128, 31, 128], BF16)
                    cz = sbp("cz", [128, 8, N])
                    gel = sbp("gel", [128, N])
                    catc = [sbp(f"catc{i}", [128, N], BF16) for i in range(2)]
                    mean = aa
                    rstd = a2
                    sqt = [rr, ig]
                zs = sbp("zs", [128, 32])
                zsg = sbp("zsg", [128, 32])

                ph.op("sp", lambda e: e.dma_start(out=A1bc[:], in_=modbc_d[:, D:2 * D]), writes=["A1bc"], dma=True)
                ph.op("sp", lambda e: e.dma_start(out=sh1bc[:], in_=modbc_d[:, 0:D]), writes=["sh1bc"], dma=True)
                ph.op("sp", lambda e: e.dma_start(out=wga[:], in_=lru_wa.rearrange("h i j -> i h j")), writes=["wga"], dma=True)
                ph.op("sp", lambda e: e.dma_start(out=wgx[:], in_=lru_wx.rearrange("h i j -> i h j")), writes=["wgx"], dma=True)

                plan = []
                for b_ in range(nblocks):
                    for i_ in range(8):
                        plan.append(i_ * 128)
                        if not prefix:
                            plan.append(1024 + i_ * 128)
                    if (prefix and b_ == nblocks - 1) or not prefix:
                        for i_ in range(8):
                            plan.append(2048 + i_ * 128)
                            plan.append(3072 + i_ * 128)
                wctr = [0, 0]

                def issue_loads():
                    while wctr[0] < len(plan) and wctr[0] < wctr[1] + 3:
                        n_ = wctr[0]
                        s_ = n_ % 4
                        c0 = plan[n_]
                        wctr[0] += 1
                        ph.op("pool", lambda e, s_=s_, c0=c0: e.dma_start(
                            out=wsl[s_][:], in_=w_in.rearrange("(k p) c -> p k c", p=128)[:, :, c0:c0 + 128]),
                            writes=[("wsl", s_)], dma=True)

                def load_slice(c0):
                    issue_loads()
                    n_ = wctr[1]
                    assert plan[n_] == c0, (n_, plan[n_], c0)
                    wctr[1] += 1
                    return n_ % 4

                def proj(dst, dname, s, t0, n):
                    for g0 in range(0, n, 512):
                        gn = min(512, n - g0)

                        def mm(e, g0=g0, gn=gn):
                            last = None
                            for k in range(16):
                                last = e.matmul(dst[:, g0:g0 + gn], lhsT=wsl[s][:, k, :], rhs=hT[:, k, t0 + g0:t0 + g0 + gn],
                                                start=(k == 0), stop=(k == 15))
                            return last
                        ph.op("pe", mm, reads=[("wsl", s), "hT"], writes=[(dname, g0)])
                    issue_loads()

                tctr = [0]
                for b in range(nblocks):
                    for t in range(N // 128):
                        s = tctr[0] % 2
                        tctr[0] += 1
                        r0 = b * N + t * 128
                        ph.op("sp", lambda e, s=s, r0=r0: e.dma_start(out=xt[s][:], in_=xsrc[r0:r0 + 128, :]),
                              writes=[("xt", s)], dma=True)
                        ph.op("act", lambda e, s=s: e.activation(out=junk[:], in_=xt[s][:], func=AF.Square, accum_out=ss[:, s:s + 1]),
                              reads=[("xt", s)], writes=["junk", ("ss", s)])
                        ph.op("act", lambda e, s=s: e.activation(out=rs[:, s:s + 1], in_=ss[:, s:s + 1], func=AF.Sqrt, scale=1.0 / D, bias=EPS),
                              reads=[("ss", s)], writes=[("rs", s)])
                        ph.op("dve", lambda e, s=s: e.reciprocal(out=rs[:, 2 + s:3 + s], in_=rs[:, s:s + 1]),
                              reads=[("rs", s)], writes=[("rs2", s)])
                        ph.op("dve", lambda e, s=s: e.scalar_tensor_tensor(out=xn[s][:], in0=xt[s][:], scalar=rs[:, 2 + s:3 + s], in1=A1bc[:],
                                                                          op0=ALU.mult, op1=ALU.mult),
                              reads=[("xt", s), ("rs2", s), "A1bc"], writes=[("xn", s)])
                        ph.op("pool", lambda e, s=s: e.tensor_tensor(out=xn[s][:], in0=xn[s][:], in1=sh1bc[:], op=ALU.add),
                              reads=[("xn", s), "sh1bc"], writes=[("xn", s)])
                        for kg in range(4):
                            bk = kg % 2

                            def trs(e, s=s, kg=kg, bk=bk):
                                last = None
                                for kk in range(4):
                                    k = 4 * kg + kk
                                    last = e.transpose(out=tp[bk][:, kk * 128:(kk + 1) * 128], in_=xn[s][:, k * 128:(k + 1) * 128],
                                                       identity=ident[:])
                                return last
                            ph.op("pe", trs, reads=[("xn", s), "ident"], writes=[("tp", bk)])
                            eng = "act" if kg % 2 == 0 else "dve"
                            if eng == "act":
                                ph.op("act", lambda e, kg=kg, bk=bk, t=t: e.activation(
                                    out=hT[:, 4 * kg:4 * kg + 4, t * 128:(t + 1) * 128],
                                    in_=tp[bk][:].rearrange("p (a c) -> p a c", a=4), func=AF.Copy),
                                    reads=[("tp", bk)], writes=["hT"])
                            else:
                                ph.op("dve", lambda e, kg=kg, bk=bk, t=t: e.tensor_copy(
                                    out=hT[:, 4 * kg:4 * kg + 4, t * 128:(t + 1) * 128],
                                    in_=tp[bk][:].rearrange("p (a c) -> p a c", a=4)),
                                    reads=[("tp", bk)], writes=["hT"])
                    fl = _col(cols, C_FL + b) if prefix else None

                    def st0(i, b=b, fl=fl):
                        j = i % NBK["xpad"]
                        sx = load_slice(i * 128)
                        proj(pp, "pp", sx, 0, N)
                        ph.op("act", lambda e, j=j: e.activation(out=xpadS[j][:, 3:3 + N], in_=pp[:], func=AF.Copy),
                              reads=[("pp", 0), ("pp", 512)], writes=[("xpad", j)])
                        ph.op("pool", lambda e, i=i, j=j: e.tensor_copy(out=xpadS[j][:, 0:3], in_=carry[:, i, :]),
                              reads=[("carry", i)], writes=[("xpadc", j)])

                    def st1(i, b=b, fl=fl):
                        jx = i % NBK["xpad"]
                        j = i % NBK["xl"]
                        xp = xpadS[jx]
                        rk = [("xpad", jx), ("xpadc", jx)]
                        ph.op("dve", lambda e, i=i, xp=xp: e.tensor_scalar(out=cA[:], in0=xp[:, 0:N], scalar1=_col(cols, C_LW + i),
                                                                          scalar2=_col(cols, C_LB + i), op0=ALU.mult, op1=ALU.add),
                              reads=rk, writes=["cA"])
                        ph.op("dve", lambda e, i=i, xp=xp: e.scalar_tensor_tensor(out=cB[:], in0=xp[:, 1:1 + N], scalar=_col(cols, C_LW + 8 + i),
                                                                                 in1=cA[:], op0=ALU.mult, op1=ALU.add),
                              reads=rk + ["cA"], writes=["cB"])
                        ph.op("dve", lambda e, i=i, xp=xp: e.scalar_tensor_tensor(out=cA[:], in0=xp[:, 2:2 + N], scalar=_col(cols, C_LW + 16 + i),
                                                                                 in1=cB[:], op0=ALU.mult, op1=ALU.add),
                              reads=rk + ["cB"], writes=["cA"])
                        ph.op("dve", lambda e, i=i, xp=xp, j=j: e.scalar_tensor_tensor(out=xlS[j][:], in0=xp[:, 3:3 + N], scalar=_col(cols, C_LW + 24 + i),
                                                                                      in1=cA[:], op0=ALU.mult, op1=ALU.add),
                              reads=rk + ["cA"], writes=[("xl", j)])
                        if prefix:
                            ph.op("pool", lambda e, i=i, fl=fl, xp=xp: e.tensor_scalar(out=carry[:, i, :], in0=xp[:, N:N + 3], scalar1=fl,
                                                                                      scalar2=None, op0=ALU.mult),
                                  reads=[("xpad", jx)], writes=[("carry", i)])
                        else:
                            ph.op("pool", lambda e, i=i, xp=xp: e.tensor_copy(out=carry[:, i, :], in_=xp[:, N:N + 3]),
                                  reads=[("xpad", jx)], writes=[("carry", i)])

                    def st2(i, b=b, fl=fl):
                        jl, jr, jg, ja, j2 = (i % NBK[k_] for k_ in ("xl", "rr", "ig", "aa", "a2"))
                        for (wg, gp, nm) in ((wga, gpa, "gpa"), (wgx, gpx, "gpx")):
                            for g0 in (0, 512):
                                ph.op("pe", lambda e, wg=wg, gp=gp, g0=g0, i=i, jl=jl: e.matmul(gp[:, g0:g0 + 512], lhsT=wg[:, i, :], rhs=xlS[jl][:, g0:g0 + 512],
                                                                                           start=True, stop=True),
                                      reads=[("xl", jl), "wga", "wgx"], writes=[(nm, g0)])
                        ph.op("act", lambda e, i=i, jr=jr: e.activation(out=rrS[jr][:], in_=gpa[:], func=AF.Sigmoid, bias=_col(cols, C_BA + i)),
                              reads=[("gpa", 0), ("gpa", 512)], writes=[("rr", jr)])
                        ph.op("act", lambda e, i=i, jg=jg: e.activation(out=igS[jg][:], in_=gpx[:], func=AF.Sigmoid, bias=_col(cols, C_BX + i)),
                              reads=[("gpx", 0), ("gpx", 512)], writes=[("ig", jg)])
                        ph.op("act", lambda e, i=i, jr=jr, ja=ja: e.activation(out=aaS[ja][:], in_=rrS[jr][:], func=AF.Exp, scale=clc[:, i:i + 1]),
                              reads=[("rr", jr)], writes=[("aa", ja)])
                        ph.op("act", lambda e, i=i, jr=jr, j2=j2: e.activation(out=a2S[j2][:], in_=rrS[jr][:], func=AF.Exp, scale=clc[:, 8 + i:9 + i]),
                              reads=[("rr", jr)], writes=[("a2", j2)])
                        ph.op("act", lambda e, j2=j2: e.activation(out=a2S[j2][:], in_=a2S[j2][:], func=AF.Sqrt, scale=-1.0, bias=1.0),
                              reads=[("a2", j2)], writes=[("a2", j2)])

                    def st3(i, b=b, fl=fl):
                        jl, jg, ja, j2, ju, jh = (i % NBK[k_] for k_ in ("xl", "ig", "aa", "a2", "uu", "hs"))
                        ph.op("pool", lambda e, jl=jl, jg=jg, ju=ju: e.tensor_tensor(out=uuS[ju][:], in0=igS[jg][:], in1=xlS[jl][:], op=ALU.mult),
                              reads=[("ig", jg), ("xl", jl)], writes=[("uu", ju)])
                        ph.op("pool", lambda e, j2=j2, ju=ju: e.tensor_tensor(out=uuS[ju][:], in0=uuS[ju][:], in1=a2S[j2][:], op=ALU.mult),
                              reads=[("uu", ju), ("a2", j2)], writes=[("uu", ju)])
                        ph.op("dve", lambda e, i=i, ja=ja, ju=ju, jh=jh: e.tensor_tensor_scan(out=hsS[jh][:], data0=aaS[ja][:], data1=uuS[ju][:],
                                                                                             initial=state[:, i:i + 1], op0=ALU.mult, op1=ALU.add),
                              reads=[("aa", ja), ("uu", ju), ("state", i)], writes=[("hs", jh)])
                        if prefix:
                            ph.op("pool", lambda e, i=i, fl=fl, jh=jh: e.tensor_scalar(out=state[:, i:i + 1], in0=hsS[jh][:, N - 1:N], scalar1=fl,
                                                                                      scalar2=None, op0=ALU.mult),
                                  reads=[("hs", jh)], writes=[("state", i)])
                        else:
                            ph.op("pool", lambda e, i=i, jh=jh: e.tensor_copy(out=state[:, i:i + 1], in_=hsS[jh][:, N - 1:N]),
                                  reads=[("hs", jh)], writes=[("state", i)])
                            sg_ = load_slice(1024 + i * 128)
                            proj(gpa, "gpa", sg_, 0, N)
                            ph.op("act", lambda e: e.activation(out=gel[:], in_=gpa[:], func=AF.Gelu_apprx_tanh),
                                  reads=[("gpa", 0), ("gpa", 512)], writes=["gel"])
                            cs_ = i % 2
                            ph.op("dve", lambda e, cs_=cs_, jh=jh: e.tensor_tensor(out=catc[cs_][:], in0=hsS[jh][:], in1=gel[:], op=ALU.mult),
                                  reads=[("hs", jh), "gel"], writes=[("catc", cs_)])
                            ph.op("sp", lambda e, cs_=cs_, i=i, b=b: e.dma_start(out=catT_d[i, :, b * N:(b + 1) * N], in_=catc[cs_][:]),
                                  reads=[("catc", cs_)], writes=[("catT_d", i, b)], dma=True, semkey=("st", "catc", cs_))

                    if prefix:
                        for s_ in range(8 + 3):
                            if s_ < 8:
                                st0(s_)
                            if 0 <= s_ - 1 < 8:
                                st1(s_ - 1)
                            if 0 <= s_ - 2 < 8:
                                st2(s_ - 2)
                            if 0 <= s_ - 3 < 8:
                                st3(s_ - 3)
                    else:
                        for i in range(8):
                            st0(i)
                            st1(i)
                            st2(i)
                            st3(i)
                    if prefix and b == nblocks - 1:
                        for i in range(8):
                            sv = load_slice(2048 + i * 128)
                            sg2 = load_slice(3072 + i * 128)
                            proj(pp, "pp", sv, N - 32, 32)
                            proj(gpa, "gpa", sg2, N - 32, 32)
                            ph.op("act", lambda e: e.activation(out=zsg[:], in_=gpa[:, 0:32], func=AF.Sigmoid),
                                  reads=[("gpa", 0)], writes=["zsg"])
                            ph.op("dve", lambda e: e.tensor_tensor(out=zs[:], in0=pp[:, 0:32], in1=zsg[:], op=ALU.mult),
                                  reads=[("pp", 0), "zsg"], writes=["zs"])
                            ph.op("pool", lambda e, i=i, fl=fl: e.tensor_scalar(out=zcarry[:, i, :], in0=zs[:, 2:32], scalar1=fl,
                                                                               scalar2=None, op0=ALU.mult),
                                  reads=["zs"], writes=[("zcarry", i)])
                    if not prefix:
                        for i in range(8):
                            sv = load_slice(2048 + i * 128)
                            sg2 = load_slice(3072 + i * 128)
                            proj(pp, "pp", sv, 0, N)
                            proj(gpx, "gpx", sg2, 0, N)
                            ph.op("act", lambda e: e.activation(out=gel[:], in_=gpx[:], func=AF.Sigmoid),
                                  reads=[("gpx", 0), ("gpx", 512)], writes=["gel"])
                            ph.op("dve", lambda e: e.tensor_tensor(out=zpad[:, 30:30 + N], in0=pp[:], in1=gel[:], op=ALU.mult),
                                  reads=[("pp", 0), ("pp", 512), "gel"], writes=["zpad"])
                            ph.op("pool", lambda e, i=i: e.tensor_copy(out=zpad[:, 0:30], in_=zcarry[:, i, :]),
                                  reads=[("zcarry", i), "zcarry"], writes=["zpadc"])
                            ph.op("pool", lambda e, i=i: e.tensor_tensor(
                                out=dgw[:], in0=ident_bf[:].unsqueeze(1).to_broadcast([128, 31, 128]),
                                in1=cols[:, C_CW + i:C_CW + i + 241:8].unsqueeze(2).to_broadcast([128, 31, 128]), op=ALU.mult),
                                writes=["dgw"])
                            for g0 in (0, 512):
                                def cmm(e, g0=g0):
                                    last = None
                                    for j in range(31):
                                        last = e.matmul(pp[:, g0:g0 + 512], lhsT=dgw[:, j, :], rhs=zpad[:, g0 + j:g0 + j + 512],
                                                        start=(j == 0), stop=(j == 30))
                                    return last
                                ph.op("pe", cmm, reads=["zpad", "zpadc", "dgw"], writes=[("pp", g0)])
                            ph.op("act", lambda e, i=i: e.activation(out=cz[:, i, :], in_=pp[:], func=AF.Identity, bias=_col(cols, C_CB + i)),
                                  reads=[("pp", 0), ("pp", 512)], writes=[("cz", i)])
                            ph.op("pool", lambda e, i=i: e.tensor_copy(out=zcarry[:, i, :], in_=zpad[:, N:N + 30]),
                                  reads=["zpad"], writes=[("zcarry", i)])
                            q_ = i % 2
                            ph.op("act", lambda e, i=i, q_=q_: e.activation(out=sqt[q_][:], in_=cz[:, i, :], func=AF.Square),
                                  reads=[("cz", i)], writes=[(("rr", 0), ("ig", 0))[q_]])
                            for g0 in (0, 512):
                                ph.op("pe", lambda e, i=i, g0=g0: e.matmul(gpa[:, g0:g0 + 512], lhsT=ones[:], rhs=cz[:, i, g0:g0 + 512],
                                                                          start=(i == 0), stop=(i == 7)),
                                      reads=[("cz", i), "ones"], writes=[("gpa", g0)])
                            for g0 in (0, 512):
                                ph.op("pe", lambda e, i=i, g0=g0, q_=q_: e.matmul(tp[g0 // 512][:], lhsT=ones[:], rhs=sqt[q_][:, g0:g0 + 512],
                                                                                 start=(i == 0), stop=(i == 7)),
                                      reads=[(("rr", 0), ("ig", 0))[q_], "ones"], writes=[("tp", g0 // 512)])
                        ph.op("act", lambda e: e.activation(out=mean[:], in_=gpa[:], func=AF.Copy, scale=1.0 / 1024.0),
                              reads=[("gpa", 0), ("gpa", 512)], writes=[("aa", 0)])
                        for g in range(2):
                            ph.op("act", lambda e, g=g: e.activation(out=rstd[:, g * 512:(g + 1) * 512], in_=tp[g][:], func=AF.Copy, scale=1.0 / 1024.0),
                                  reads=[("tp", g)], writes=[("a2", 0)])
                        ph.op("dve", lambda e: e.tensor_tensor(out=cA[:], in0=mean[:], in1=mean[:], op=ALU.mult),
                              reads=[("aa", 0)], writes=["cA"])
                        ph.op("dve", lambda e: e.tensor_tensor(out=rstd[:], in0=rstd[:], in1=cA[:], op=ALU.subtract),
                              reads=[("a2", 0), "cA"], writes=[("a2", 0)])
                        ph.op("act", lambda e: e.activation(out=rstd[:], in_=rstd[:], func=AF.Sqrt, bias=EPS),
                              reads=[("a2", 0)], writes=[("a2", 0)])
                        ph.op("dve", lambda e: e.reciprocal(out=rstd[:], in_=rstd[:]), reads=[("a2", 0)], writes=[("a2", 0)])
                        for i in range(8):
                            ph.op("pool", lambda e, i=i: e.tensor_tensor(out=cA[:], in0=cz[:, i, :], in1=mean[:], op=ALU.subtract),
                                  reads=[("cz", i), ("aa", 0)], writes=["cA"])
                            ph.op("dve", lambda e: e.tensor_tensor(out=cB[:], in0=cA[:], in1=rstd[:], op=ALU.mult),
                                  reads=["cA", ("a2", 0)], writes=["cB"])
                            cs_ = i % 2
                            ph.op("act", lambda e, i=i, cs_=cs_: e.activation(out=catc[cs_][:], in_=cB[:], func=AF.Silu,
                                                                             scale=_col(cols, C_LG + i), bias=_col(cols, C_LNB + i)),
                                  reads=["cB"], writes=[("catc", cs_)])
                            ph.op("sp", lambda e, cs_=cs_, i=i, b=b: e.dma_start(out=catT_d[8 + i, :, b * N:(b + 1) * N], in_=catc[cs_][:]),
                                  reads=[("catc", cs_)], writes=[("catT_d", 8 + i, b)], dma=True, semkey=("st", "catc", cs_))
                ph.emit()

        mixer_phase("P", x_pre, NPRE // TB, True)
        mixer_phase("M", x_main, T // TB, False)
        if stop_after == "M":
            return nc

        with ExitStack() as ph_es:
            ph = Phase(nc, "C")
            def sbp(nm, shape, dt=F32):
                return ph_es.enter_context(nc.sbuf_tensor("c_" + nm, list(shape), dt))
            def psp(nm, shape):
                return ph_es.enter_context(nc.psum_tensor("c_" + nm, list(shape), F32))
            wo = sbp("wo", [128, 16, D], BF16)
            gg = sbp("gg", [128, D])
            ct = [sbp(f"ct{i}", [128, 16, 128], BF16) for i in range(2)]
            xt = [sbp(f"cxt{i}", [128, D]) for i in range(2)]
            yo = [sbp(f"yo{i}", [128, D]) for i in range(2)]
            junk = sbp("cjunk", [128, D], BF16)
            ss = sbp("css", [128, 4])
            rs = sbp("crs", [128, 4])
            yp = [psp(f"yp{i}", [128, D]) for i in range(2)]
            for k in range(16):
                ph.op("pool", lambda e, k=k: e.dma_start(out=wo[:, k, :], in_=w_out[k * 128:(k + 1) * 128, :]),
                      writes=[("wo", k)], dma=True)
            ph.op("sp", lambda e: e.dma_start(out=gg[:], in_=modbc_d[:, 2 * D:3 * D]), writes=["gg"], dma=True)
            for t in range(T // 128):
                s = t % 2
                ph.op("sp", lambda e, s=s, t=t: e.dma_start(out=ct[s][:], in_=catT_d[:, :, t * 128:(t + 1) * 128].rearrange("k p n -> p k n")),
                      writes=[("ct", s)], dma=True)
                ph.op("sp", lambda e, s=s, t=t: e.dma_start(out=xt[s][:], in_=x_main[t * 128:(t + 1) * 128, :]),
                      writes=[("xt", s)], dma=True)

                def mm(e, s=s):
                    last = None
                    for g in range(4):
                        for k in range(16):
                            last = e.matmul(yp[s][:, g * 512:(g + 1) * 512], lhsT=ct[s][:, k, :], rhs=wo[:, k, g * 512:(g + 1) * 512],
                                            start=(k == 0), stop=(k == 15))
                    return last
                ph.op("pe", mm, reads=[("ct", s)] + [("wo", k) for k in range(16)], writes=[("yp", s)])
                ph.op("act", lambda e, s=s: e.activation(out=junk[:], in_=yp[s][:], func=AF.Square, accum_out=ss[:, s:s + 1]),
                      reads=[("yp", s)], writes=["junk", ("ss", s)])
                ph.op("act", lambda e, s=s: e.activation(out=rs[:, s:s + 1], in_=ss[:, s:s + 1], func=AF.Sqrt, scale=1.0 / D, bias=EPS),
                      reads=[("ss", s)], writes=[("rs", s)])
                ph.op("dve", lambda e, s=s: e.reciprocal(out=rs[:, 2 + s:3 + s], in_=rs[:, s:s + 1]),
                      reads=[("rs", s)], writes=[("rs2", s)])
                ph.op("dve", lambda e, s=s: e.scalar_tensor_tensor(out=yo[s][:], in0=yp[s][:], scalar=rs[:, 2 + s:3 + s], in1=gg[:],
                                                                  op0=ALU.mult, op1=ALU.mult),
                      reads=[("yp", s), ("rs2", s), "gg"], writes=[("yo", s)])
                ph.op("pool", lambda e, s=s: e.tensor_tensor(out=yo[s][:], in0=yo[s][:], in1=xt[s][:], op=ALU.add),
                      reads=[("yo", s), ("xt", s)], writes=[("yo", s)])
                ph.op("sp", lambda e, s=s, t=t: e.dma_start(out=x1_d[t * 128:(t + 1) * 128, :], in_=yo[s][:]),
                      reads=[("yo", s)], writes=[("x1_d", t)], dma=True, semkey=("st", "yo", s))
            ph.emit()
        if stop_after == "C":
            _dbg_copy(nc, dbg, x1_d, rows=T)
            return nc

        with ExitStack() as ph_es:
            ph = Phase(nc, "D")
            def sbp(nm, shape, dt=F32):
                return ph_es.enter_context(nc.sbuf_tensor("d_" + nm, list(shape), dt))
            def psp(nm, shape, dt=F32):
                return ph_es.enter_context(nc.psum_tensor("d_" + nm, list(shape), dt))
            A2bc = sbp("A2bc", [128, D])
            sh2bc = sbp("sh2bc", [128, D])
            xt = [sbp(f"xt{i}", [128, D]) for i in range(2)]
            xn = [sbp(f"xn{i}", [128, D]) for i in range(2)]
            junk = sbp("junk", [128, D], BF16)
            ss = sbp("ss", [128, 4])
            rs = sbp("rs", [128, 4])
            h2T = sbp("h2T", [128, 16, 512], BF16)
            wqs = [sbp(f"wqs{i}", [128, 16, 128], BF16) for i in range(4)]
            qT = sbp("qT", [128, 16, 512])
            skT = sbp("skT", [128, 16, 128])
            S = sbp("S", [128, 16, 128])
            S2 = sbp("S2", [128, 16, 128])
            vals = sbp("vals", [128, 16, 16])
            idx = sbp("idx", [128, 16, 16], U32)
            idxf = sbp("idxf", [128, 16, 16])
            cand = sbp("cand", [128, 8, 256])
            cand2 = sbp("cand2", [128, 8, 256])
            cval = sbp("cval", [128, 8, 16])
            cidx = sbp("cidx", [128, 8, 16], U32)
            cif = sbp("cif", [128, 8, 16])
            ex = sbp("ex", [128, 8, 16])
            esum = sbp("esum", [128, 8])
            e1 = sbp("e1", [128, 8, 16, 16])
            e2 = sbp("e2", [128, 8, 16, 16])
            i_f = sbp("i_f", [128, 8, 16])
            jjf = sbp("jjf", [128, 8, 16])
            slot = sbp("slot", [128, 3, 128])
            slT = [sbp(f"slT{i}", [128, 3, 128]) for i in range(2)]
            k16 = sbp("k16", [128, 3, 16])
            tp = [psp(f"tp{i}", [128, 512]) for i in range(2)]
            qp = [psp(f"qp{i}", [128, 512]) for i in range(2)]
            scp = psp("scp", [128, 2048])

            ph.op("sp", lambda e: e.dma_start(out=A2bc[:], in_=modbc_d[:, 4 * D:5 * D]), writes=["A2bc"], dma=True)
            ph.op("sp", lambda e: e.dma_start(out=sh2bc[:], in_=modbc_d[:, 3 * D:4 * D]), writes=["sh2bc"], dma=True)
            ph.op("sp", lambda e: e.dma_start(out=S[:], in_=peer_sk.rearrange("j n d -> n j d")), writes=["S"], dma=True)
            for jg in range(4):
                def trs(e, jg=jg):
                    last = None
                    for jj in range(4):
                        last = e.transpose(out=tp[jg % 2][:, jj * 128:(jj + 1) * 128], in_=S[:, 4 * jg + jj, :], identity=ident[:])
                    return last
                ph.op("pe", trs, reads=["S"], writes=[("tp", jg % 2)])
                ph.op("act", lambda e, jg=jg: e.activation(out=skT[:, 4 * jg:4 * jg + 4, :],
                                                          in_=tp[jg % 2][:].rearrange("p (a c) -> p a c", a=4), func=AF.Copy),
                      reads=[("tp", jg % 2)], writes=["skT"])
            ph.op("dve", lambda e: e.tensor_copy(out=k16[:, 0, :], in_=iota[:, 0:16]), writes=["k16a"])
            ph.op("dve", lambda e: e.tensor_scalar(out=k16[:, 1, :], in0=iota[:, 0:16], scalar1=16.0, scalar2=None, op0=ALU.mult),
                  writes=["k16b"])
            ph.op("dve", lambda e: e.tensor_scalar(out=k16[:, 2, :], in0=iota[:, 0:16], scalar1=16.0, scalar2=16.0, op0=ALU.mult, op1=ALU.add),
                  writes=["k16c"])

            wplan = [j for g in range(4) for j in range(16)]
            wctr = [0, 0]
            idxfS = [idxf, sbp("idxf1", [128, 16, 16])]
            cvalS = [cval, sbp("cval1", [128, 8, 16])]
            cidxS = [cidx, sbp("cidx1", [128, 8, 16], U32)]
            pending = []
            tn = [0]

            def pump():
                if pending:
                    pending.pop(0)()

            def flush():
                while pending:
                    pending.pop(0)()

            def issue_wq():
                while wctr[0] < len(wplan) and wctr[0] < wctr[1] + 3:
                    n_ = wctr[0]
                    wctr[0] += 1
                    ph.op("pool", lambda e, s_=n_ % 4, j=wplan[n_]: e.dma_start(
                        out=wqs[s_][:], in_=peer_wq.rearrange("(k p) c -> p k c", p=128)[:, :, j * 128:(j + 1) * 128]),
                        writes=[("wqs", n_ % 4)], dma=True)

            tctr = 0
            for g in range(4):
                for t in range(4):
                    s = tctr % 2
                    tctr += 1
                    r0 = g * 512 + t * 128
                    ph.op("sp", lambda e, s=s, r0=r0: e.dma_start(out=xt[s][:], in_=x1_d[r0:r0 + 128, :]), writes=[("xt", s)], dma=True)
                    ph.op("act", lambda e, s=s: e.activation(out=junk[:], in_=xt[s][:], func=AF.Square, accum_out=ss[:, s:s + 1]),
                          reads=[("xt", s)], writes=["junk", ("ss", s)])
                    ph.op("act", lambda e, s=s: e.activation(out=rs[:, s:s + 1], in_=ss[:, s:s + 1], func=AF.Sqrt, scale=1.0 / D, bias=EPS),
                          reads=[("ss", s)], writes=[("rs", s)])
                    ph.op("dve", lambda e, s=s: e.reciprocal(out=rs[:, 2 + s:3 + s], in_=rs[:, s:s + 1]), reads=[("rs", s)], writes=[("rs2", s)])
                    ph.op("dve", lambda e, s=s: e.scalar_tensor_tensor(out=xn[s][:], in0=xt[s][:], scalar=rs[:, 2 + s:3 + s], in1=A2bc[:],
                                                                      op0=ALU.mult, op1=ALU.mult),
                          reads=[("xt", s), ("rs2", s), "A2bc"], writes=[("xn", s)])
                    ph.op("pool", lambda e, s=s: e.tensor_tensor(out=xn[s][:], in0=xn[s][:], in1=sh2bc[:], op=ALU.add),
                          reads=[("xn", s), "sh2bc"], writes=[("xn", s)])
                    for kg in range(4):
                        bk = kg % 2

                        def trs(e, s=s, kg=kg, bk=bk):
                            last = None
                            for kk in range(4):
                                k = 4 * kg + kk
                                last = e.transpose(out=tp[bk][:, kk * 128:(kk + 1) * 128], in_=xn[s][:, k * 128:(k + 1) * 128], identity=ident[:])
                            return last
                        ph.op("pe", trs, reads=[("xn", s)], writes=[("tp", bk)])
                        if kg % 2 == 0:
                            ph.op("act", lambda e, kg=kg, bk=bk, t=t: e.activation(
                                out=h2T[:, 4 * kg:4 * kg + 4, t * 128:(t + 1) * 128],
                                in_=tp[bk][:].rearrange("p (a c) -> p a c", a=4), func=AF.Copy),
                                reads=[("tp", bk)], writes=["h2T"])
                        else:
                            ph.op("dve", lambda e, kg=kg, bk=bk, t=t: e.tensor_copy(
                                out=h2T[:, 4 * kg:4 * kg + 4, t * 128:(t + 1) * 128],
                                in_=tp[bk][:].rearrange("p (a c) -> p a c", a=4)),
                                reads=[("tp", bk)], writes=["h2T"])
                ph.op("sp", lambda e, g=g: e.dma_start(out=h2T_d[:, :, g * 512:(g + 1) * 512].rearrange("k p n -> p k n"), in_=h2T[:]),
                      reads=["h2T"], writes=[("h2T_d", g)], dma=True, semkey=("st", "h2T"))
                for j in range(16):
                    issue_wq()
                    sl = wctr[1] % 4
                    wctr[1] += 1

                    def mm(e, sl=sl, j=j):
                        last = None
                        for k in range(16):
                            last = e.matmul(qp[j % 2][:], lhsT=wqs[sl][:, k, :], rhs=h2T[:, k, :], start=(k == 0), stop=(k == 15))
                        return last
                    ph.op("pe", mm, reads=[("wqs", sl), "h2T"], writes=[("qp", j % 2)])
                    issue_wq()
                    if j % 2 == 0:
                        ph.op("act", lambda e, j=j: e.activation(out=qT[:, j, :], in_=qp[j % 2][:], func=AF.Copy),
                              reads=[("qp", j % 2)], writes=[("qT", j)])
                    else:
                        ph.op("dve", lambda e, j=j: e.tensor_copy(out=qT[:, j, :], in_=qp[j % 2][:]),
                              reads=[("qp", j % 2)], writes=[("qT", j)])
                for t in range(4):
                    tok0 = g * 512 + t * 128
                    bs = tn[0] % 2
                    tn[0] += 1

                    def scm(e, t=t):
                        last = None
                        for j in range(16):
                            last = e.matmul(scp[:, j * 128:(j + 1) * 128], lhsT=qT[:, j, t * 128:(t + 1) * 128], rhs=skT[:, j, :],
                                            start=True, stop=True)
                        return last
                    ph.op("pe", scm, reads=[("qT", j) for j in range(16)] + ["skT"], writes=["scp"])
                    ph.op("act", lambda e: e.activation(out=S[:].rearrange("p a b -> p (a b)"), in_=scp[:], func=AF.Copy),
                          reads=["scp"], writes=["S"])
                    for j in range(16):
                        ph.op("dve", lambda e, j=j: e.max(out=vals[:, j, 0:8], in_=S[:, j, :]), reads=["S"], writes=[("v1", j)])
                    pump()
                    for j in range(16):
                        ph.op("dve", lambda e, j=j: e.match_replace(out=S2[:, j, :], in_to_replace=vals[:, j, 0:8], in_values=S[:, j, :], imm_value=-1e30),
                              reads=["S", ("v1", j)], writes=[("S2", j)])
                    pump()
                    for j in range(16):
                        ph.op("dve", lambda e, j=j: e.max(out=vals[:, j, 8:16], in_=S2[:, j, :]), reads=[("S2", j)], writes=[("v2", j)])
                    pump()
                    for j in range(16):
                        ph.op("dve", lambda e, j=j: e.max_index(out=idx[:, j, 0:8], in_max=vals[:, j, 0:8], in_values=S[:, j, :]),
                              reads=["S", ("v1", j)], writes=[("i1", j)])
                    pump()
                    for j in range(16):
                        ph.op("dve", lambda e, j=j: e.max_index(out=idx[:, j, 8:16], in_max=vals[:, j, 8:16], in_values=S2[:, j, :]),
                              reads=[("S2", j), ("v2", j)], writes=[("i2", j)])
                    pump()
                    allv = [("v1", j) for j in range(16)] + [("v2", j) for j in range(16)]
                    alli = [("i1", j) for j in range(16)] + [("i2", j) for j in range(16)]
                    ph.op("pool", lambda e, bs=bs: e.tensor_copy(out=idxfS[bs][:], in_=idx[:]), reads=alli, writes=[("idxf", bs)])
                    v4 = vals[:].rearrange("p (h q) k -> p h q k", q=2)
                    ph.op("pool", lambda e, v4=v4: e.tensor_tensor(
                        out=cand[:].rearrange("p h (a b) -> p h a b", a=16),
                        in0=v4[:, :, 0, :].unsqueeze(3).to_broadcast([128, 8, 16, 16]),
                        in1=v4[:, :, 1, :].unsqueeze(2).to_broadcast([128, 8, 16, 16]), op=ALU.add),
                        reads=allv, writes=["cand"])
                    pump()
                    cvl, cix = cvalS[bs], cidxS[bs]
                    for h in range(8):
                        ph.op("dve", lambda e, h=h, cvl=cvl: e.max(out=cvl[:, h, 0:8], in_=cand[:, h, :]), reads=["cand"], writes=[("c1", bs, h)])
                    pump()
                    for h in range(8):
                        ph.op("dve", lambda e, h=h, cvl=cvl: e.match_replace(out=cand2[:, h, :], in_to_replace=cvl[:, h, 0:8], in_values=cand[:, h, :], imm_value=-1e30),
                              reads=["cand", ("c1", bs, h)], writes=[("cand2", h)])
                    pump()
                    for h in range(8):
                        ph.op("dve", lambda e, h=h, cvl=cvl: e.max(out=cvl[:, h, 8:16], in_=cand2[:, h, :]), reads=[("cand2", h)], writes=[("c2", bs, h)])
                    pump()
                    for h in range(8):
                        ph.op("dve", lambda e, h=h, cvl=cvl, cix=cix: e.max_index(out=cix[:, h, 0:8], in_max=cvl[:, h, 0:8], in_values=cand[:, h, :]),
                              reads=["cand", ("c1", bs, h)], writes=[("ci1", bs, h)])
                    pump()
                    for h in range(8):
                        ph.op("dve", lambda e, h=h, cvl=cvl, cix=cix: e.max_index(out=cix[:, h, 8:16], in_max=cvl[:, h, 8:16], in_values=cand2[:, h, :]),
                              reads=[("cand2", h), ("c2", bs, h)], writes=[("ci2", bs, h)])
                    flush()

                    def make_b(bs=bs, t=t, tok0=tok0):
                        cvl, cix, ixf = cvalS[bs], cidxS[bs], idxfS[bs]
                        allc = [("c1", bs, h) for h in range(8)] + [("c2", bs, h) for h in range(8)]
                        allci = [("ci1", bs, h) for h in range(8)] + [("ci2", bs, h) for h in range(8)]
                        cb = cif[:].unsqueeze(3).to_broadcast([128, 8, 16, 16])
                        lo = k16[:, 1, :].unsqueeze(1).unsqueeze(1).to_broadcast([128, 8, 16, 16])
                        hi = k16[:, 2, :].unsqueeze(1).unsqueeze(1).to_broadcast([128, 8, 16, 16])
                        io = k16[:, 0, :].unsqueeze(1).unsqueeze(1).to_broadcast([128, 8, 16, 16])
                        i4 = ixf[:].rearrange("p (h q) k -> p h q k", q=2)
                        st_ = t % 2
                        steps = []

                        def s1():
                            ph.op("pool", lambda e: e.tensor_tensor(out=ex[:], in0=cvl[:], in1=cvl[:, :, 0:1].to_broadcast([128, 8, 16]), op=ALU.subtract),
                                  reads=allc, writes=["ex"])
                            ph.op("act", lambda e: e.activation(out=ex[:], in_=ex[:], func=AF.Exp), reads=["ex"], writes=["ex"])
                            ph.op("pool", lambda e: e.tensor_copy(out=cif[:], in_=cix[:]), reads=allci, writes=["cif"])

                        def s2():
                            ph.op("dve", lambda e: e.tensor_reduce(out=esum[:], in_=ex[:], axis=AX.X, op=ALU.add), reads=["ex"], writes=["esum"])
                            ph.op("dve", lambda e: e.reciprocal(out=esum[:], in_=esum[:]), reads=["esum"], writes=["esum"])
                            ph.op("pool", lambda e: e.tensor_tensor(out=slot[:, 2, :].rearrange("p (h k) -> p h k", h=8), in0=ex[:],
                                                                    in1=esum[:].unsqueeze(2).to_broadcast([128, 8, 16]), op=ALU.mult),
                                  reads=["ex", "esum"], writes=["slot_g"])
                            ph.op("dve", lambda e: e.tensor_tensor(out=e1[:], in0=cb, in1=lo, op=ALU.is_ge), reads=["cif", "k16b"], writes=["e1"])
                            ph.op("dve", lambda e: e.tensor_tensor(out=e2[:], in0=cb, in1=hi, op=ALU.is_ge), reads=["cif", "k16c"], writes=["e2"])
                            ph.op("dve", lambda e: e.tensor_tensor(out=e1[:], in0=e1[:], in1=e2[:], op=ALU.subtract), reads=["e1", "e2"], writes=["e1"])
                            ph.op("pool", lambda e: e.tensor_tensor(out=e2[:], in0=e1[:], in1=i4[:, :, 0, :].unsqueeze(2).to_broadcast([128, 8, 16, 16]), op=ALU.mult),
                                  reads=["e1", ("idxf", bs)], writes=["e2"])

                        def s3():
                            ph.op("dve", lambda e: e.tensor_reduce(out=slot[:, 0, :].rearrange("p (h k) -> p h k", h=8), in_=e2[:], axis=AX.X, op=ALU.add),
                                  reads=["e2"], writes=["slot_r"])
                            ph.op("pool", lambda e: e.tensor_tensor(out=e2[:], in0=e1[:], in1=io, op=ALU.mult), reads=["e1", "k16a"], writes=["e2"])

                        def s4():
                            ph.op("dve", lambda e: e.tensor_reduce(out=i_f[:], in_=e2[:], axis=AX.X, op=ALU.add), reads=["e2"], writes=["i_f"])
                            ph.op("dve", lambda e: e.scalar_tensor_tensor(out=jjf[:], in0=i_f[:], scalar=-16.0, in1=cif[:], op0=ALU.mult, op1=ALU.add),
                                  reads=["i_f", "cif"], writes=["jjf"])
                            ph.op("dve", lambda e: e.tensor_tensor(out=e1[:], in0=jjf[:].unsqueeze(3).to_broadcast([128, 8, 16, 16]), in1=io, op=ALU.is_equal),
                                  reads=["jjf", "k16a"], writes=["e1"])
                            ph.op("pool", lambda e: e.tensor_tensor(out=e2[:], in0=e1[:], in1=i4[:, :, 1, :].unsqueeze(2).to_broadcast([128, 8, 16, 16]), op=ALU.mult),
                                  reads=["e1", ("idxf", bs)], writes=["e2"])

                        def s5():
                            ph.op("dve", lambda e: e.tensor_reduce(out=slot[:, 1, :].rearrange("p (h k) -> p h k", h=8), in_=e2[:], axis=AX.X, op=ALU.add),
                                  reads=["e2"], writes=["slot_c"])

                            def trs(e):
                                last = None
                                for q in range(3):
                                    last = e.transpose(out=tp[0][:, q * 128:(q + 1) * 128], in_=slot[:, q, :], identity=ident[:])
                                return last
                            ph.op("pe", trs, reads=["slot_r", "slot_c", "slot_g"], writes=[("tp", 0)])
                            ph.op("act", lambda e: e.activation(out=slT[st_][:], in_=tp[0][:, 0:384].rearrange("p (a c) -> p a c", a=3), func=AF.Copy),
                                  reads=[("tp", 0)], writes=[("slT", st_)])
                            ph.op("sp", lambda e: e.dma_start(out=slots_d[:, :, tok0:tok0 + 128].rearrange("q p n -> p q n"), in_=slT[st_][:]),
                                  reads=[("slT", st_)], writes=[("slots_d", tok0)], dma=True, semkey=("st", "slT", st_))
                        return [s1, s2, s3, s4, s5]
                    pending.extend(make_b())
                    if t == 3:
                        flush()
            ph.emit()
        if stop_after == "D":
            _dbg_copy3(nc, dbg, slots_d)
            return nc

        with ExitStack() as ph_es:
            ph = Phase(nc, "E")
            def sbp(nm, shape, dt=F32):
                return ph_es.enter_context(nc.sbuf_tensor("e_" + nm, list(shape), dt))
            def psp(nm, shape, dt=F32):
                return ph_es.enter_context(nc.psum_tensor("e_" + nm, list(shape), dt))
            slT = [sbp(f"slT{i}", [128, 3, 128]) for i in range(2)]
            Ab = [sbp(f"Ab{i}", [128, 128], BF16) for i in range(8)]
            Bb = [sbp(f"Bb{i}", [128, 128], BF16) for i in range(8)]
            Gs = [sbp(f"Gs{i}", [128, 32, 128, 4], BF16) for i in range(2)]
            Gp = [psp(f"Gp{i}", [128, 512]) for i in range(4)]
            for t in range(T // 128):
                s = t % 2
                ph.op("sp", lambda e, s=s, t=t: e.dma_start(out=slT[s][:], in_=slots_d[:, :, t * 128:(t + 1) * 128].rearrange("q p n -> p q n")),
                      writes=[("slT", s)], dma=True)
                for n4 in range(32):
                    bk = n4 % 4
                    for q in range(4):
                        n = n4 * 4 + q
                        a_ = n % 8
                        ph.op("dve", lambda e, s=s, n=n, a_=a_: e.tensor_scalar(out=Ab[a_][:], in0=iota_bf[:], scalar1=slT[s][:, 0, n:n + 1],
                                                                               scalar2=slT[s][:, 2, n:n + 1], op0=ALU.is_equal, op1=ALU.mult),
                              reads=[("slT", s)], writes=[("Ab", a_)])
                        ph.op("dve", lambda e, s=s, n=n, a_=a_: e.tensor_scalar(out=Bb[a_][:], in0=iota_bf[:], scalar1=slT[s][:, 1, n:n + 1],
                                                                               scalar2=None, op0=ALU.is_equal),
                              reads=[("slT", s)], writes=[("Bb", a_)])
                        ph.op("pe", lambda e, a_=a_, bk=bk, q=q: e.matmul(Gp[bk][:, q * 128:(q + 1) * 128], lhsT=Bb[a_][:], rhs=Ab[a_][:], start=True, stop=True),
                              reads=[("Ab", a_), ("Bb", a_)], writes=[("Gp", bk)])
                    ph.op("act", lambda e, s=s, n4=n4, bk=bk: e.activation(
                        out=Gs[s][:, :, n4 * 4:n4 * 4 + 4, :],
                        in_=Gp[bk][:].rearrange("p (q g r) -> p g q r", q=4, g=32, r=4), func=AF.Copy),
                        reads=[("Gp", bk)], writes=[("Gs", s)])
                for gq in range(4):
                    for g2 in range(2):
                        gi = gq * 2 + g2
                        ph.op("sp", lambda e, s=s, t=t, gi=gi: e.dma_start(out=G_ds[gi][:, :, t * 128:(t + 1) * 128, :],
                                                                          in_=Gs[s][:, gi * 4:(gi + 1) * 4, :, :]),
                              reads=[("Gs", s)], writes=[("G_d", t, gi)], dma=True, semkey=("st", "Gs", s, gq))
            ph.emit()

        if stop_after == "E":
            return nc
        y2_d = nc.dram_tensor("y2_d", [T, D], F32).ap()
        RGV = 4
        for hb in range(T // HB):
            with ExitStack() as ph_es:
                ph = Phase(nc, f"F{hb}")
                def sbp(nm, shape, dt=F32):
                    return ph_es.enter_context(nc.sbuf_tensor(f"f{hb}_" + nm, list(shape), dt))
                def psp(nm, shape, dt=F32):
                    return ph_es.enter_context(nc.psum_tensor(f"f{hb}_" + nm, list(shape), dt))
                h2T = sbp("h2T", [128, 16, HB], BF16)
                acc = sbp("acc", [128, HB // 128, D])
                ub = [sbp(f"ub{i}", [128, D], BF16) for i in range(3)]
                uT = [sbp(f"uT{i}", [128, 16, 128], BF16) for i in range(3)]
                vb = [sbp(f"vb{i}", [128, D], BF16) for i in range(8)]
                Gg = [sbp(f"Gg{i}", [128, HB, 4], BF16) for i in range(2)]
                gl = sbp("gl", [128, HB])
                W = [sbp(f"W{i}", [128, HB], BF16) for i in range(8)]
                tpu = [psp(f"tpu{i}", [128, 1024], BF16) for i in range(2)]
                actp = psp("actp", [128, HB])
                yp = [psp(f"yp{i}", [128, 1024]) for i in range(2)]
                ph.op("sp", lambda e: e.dma_start(out=h2T[:], in_=h2T_d[:, :, hb * HB:(hb + 1) * HB].rearrange("k p n -> p k n")),
                      writes=["h2T"], dma=True)
                nch = (F_GROUPS * RGV) if F_GROUPS else NEXP_CH
                ngroups = nch // RGV
                ypc = [0]

                def loadu(r):
                    if r >= nch:
                        return
                    rg, q4 = divmod(r, 4)
                    if q4 == 0:
                        ph.op("sp", lambda e, gs_=rg % 2, rg=rg: e.dma_start(out=Gg[gs_][:], in_=G_ds[rg // 4][:, rg % 4, hb * HB:(hb + 1) * HB, :]),
                              writes=[("Gg", rg % 2)], dma=True)
                    ph.op("pool", lambda e, r=r: e.dma_start(out=ub[r % 3][:], in_=peer_u[r * 128:(r + 1) * 128, :]), writes=[("ub", r % 3)], dma=True)

                def loadv(r):
                    if r >= nch:
                        return
                    ph.op("pool", lambda e, r=r: e.dma_start(out=vb[r % 8][:], in_=peer_v[r * 128:(r + 1) * 128, :]), writes=[("vb", r % 8)], dma=True)

                def front(r):
                    if r >= nch or F_MODE == 'l':
                        return
                    u_ = r % 3
                    for kg in range(2):
                        def trs(e, u_=u_, kg=kg):
                            last = None
                            for kk in range(8):
                                k = 8 * kg + kk
                                last = e.transpose(out=tpu[kg][:, kk * 128:(kk + 1) * 128], in_=ub[u_][:, k * 128:(k + 1) * 128], identity=ident_bf[:])
                            return last
                        ph.op("pe", trs, reads=[("ub", u_)], writes=[("tpu", kg)])
                        if kg == 0:
                            ph.op("act", lambda e, u_=u_, kg=kg: e.activation(
                                out=uT[u_][:, 8 * kg:8 * kg + 8, :], in_=tpu[kg][:].rearrange("p (a c) -> p a c", a=8), func=AF.Copy),
                                reads=[("tpu", kg)], writes=[("uT", u_, kg)])
                        else:
                            ph.op("dve", lambda e, u_=u_, kg=kg: e.tensor_copy(
                                out=uT[u_][:, 8 * kg:8 * kg + 8, :], in_=tpu[kg][:].rearrange("p (a c) -> p a c", a=8)),
                                reads=[("tpu", kg)], writes=[("uT", u_, kg)])

                def mid(r):
                    if F_MODE == 'l':
                        return
                    u_ = r % 3
                    w_ = r % 8
                    rg, q4 = divmod(r, 4)
                    gs_ = rg % 2

                    def mm(e, u_=u_):
                        last = None
                        for k in range(16):
                            for tg in range(HB // 512):
                                last = e.matmul(actp[:, tg * 512:(tg + 1) * 512], lhsT=uT[u_][:, k, :], rhs=h2T[:, k, tg * 512:(tg + 1) * 512],
                                                start=(k == 0), stop=(k == 15))
                        return last
                    ph.op("pe", mm, reads=[("uT", u_, 0), ("uT", u_, 1), "h2T"], writes=[("actp", tg) for tg in range(HB // 512)])
                    ph.op("act", lambda e: e.activation(out=gl[:], in_=actp[:], func=AF.Gelu_apprx_tanh),
                          reads=[("actp", tg) for tg in range(HB // 512)], writes=["gl"])
                    ph.op("dve", lambda e, w_=w_, gs_=gs_, q4=q4: e.tensor_tensor(out=W[w_][:], in0=gl[:], in1=Gg[gs_][:, :, q4], op=ALU.mult),
                          reads=["gl", ("Gg", gs_)], writes=[("W", w_)])

                def vphase(grp):
                    if F_MODE == 'l':
                        return
                    wbase = (grp % 2) * RGV
                    for t in range(HB // 128):
                        for dh in range(2):
                            yb = ypc[0] % 2
                            ypc[0] += 1

                            def vmm(e, t=t, dh=dh, yb=yb, wbase=wbase):
                                last = None
                                for qq in range(RGV):
                                    for dg in range(2):
                                        c0 = dh * 1024 + dg * 512
                                        last = e.matmul(yp[yb][:, dg * 512:(dg + 1) * 512], lhsT=W[wbase + qq][:, t * 128:(t + 1) * 128],
                                                        rhs=vb[wbase + qq][:, c0:c0 + 512], start=(qq == 0), stop=(qq == RGV - 1))
                                return last
                            ph.op("pe", vmm, reads=[("W", wbase + qq) for qq in range(RGV)] + [("vb", wbase + qq) for qq in range(RGV)],
                                  writes=[("yp", yb)])
                            if grp == 0:
                                ph.op("dve", lambda e, t=t, dh=dh, yb=yb: e.tensor_copy(out=acc[:, t, dh * 1024:(dh + 1) * 1024], in_=yp[yb][:]),
                                      reads=[("yp", yb)], writes=[("acc", t, dh)])
                            else:
                                ph.op("dve", lambda e, t=t, dh=dh, yb=yb: e.tensor_tensor(out=acc[:, t, dh * 1024:(dh + 1) * 1024], in0=yp[yb][:],
                                                                                        in1=acc[:, t, dh * 1024:(dh + 1) * 1024], op=ALU.add),
                                      reads=[("yp", yb), ("acc", t, dh)], writes=[("acc", t, dh)])

                for r in range(3):
                    loadu(r)
                front(0)
                front(1)
                for r in range(nch):
                    mid(r)
                    loadv(r)
                    loadu(r + 3)
                    front(r + 2)
                    if r % RGV == 0 and r >= RGV:
                        vphase(r // RGV - 1)
                vphase(ngroups - 1)
                if stop_after == "F":
                    for t in range(HB // 128 if F_MODE == '' else 0):
                        ph.op("sp", lambda e, t=t: e.dma_start(out=y2_d[hb * HB + t * 128:hb * HB + (t + 1) * 128, :], in_=acc[:, t, :]),
                              reads=[("acc", t, 0), ("acc", t, 1)], writes=[("y2_d", t)], dma=True, semkey=("st", "acc", t % 2))
                else:
                    XT = Gg[0][:].rearrange("p a b -> p (a b)").bitcast(F32)
                    GG2 = Gg[1][:].rearrange("p a b -> p (a b)").bitcast(F32)
                    ess = sbp("ess", [128, 4])
                    ers = sbp("ers", [128, 4])
                    ph.op("sp", lambda e: e.dma_start(out=GG2, in_=modbc_d[:, 5 * D:6 * D]), writes=[("Gg", 1)], dma=True)
                    for t in range(HB // 128):
                        s_ = t % 2
                        r0 = hb * HB + t * 128
                        ph.op("sp", lambda e, r0=r0: e.dma_start(out=XT, in_=x1_d[r0:r0 + 128, :]), writes=[("Gg", 0)], dma=True)
                        ph.op("act", lambda e, t=t, s_=s_: e.activation(out=ub[0][:], in_=acc[:, t, :], func=AF.Square, accum_out=ess[:, s_:s_ + 1]),
                              reads=[("acc", t, 0), ("acc", t, 1)], writes=[("ub", 0), ("ess", s_)])
                        ph.op("act", lambda e, s_=s_: e.activation(out=ers[:, s_:s_ + 1], in_=ess[:, s_:s_ + 1], func=AF.Sqrt, scale=1.0 / D, bias=EPS),
                              reads=[("ess", s_)], writes=[("ers", s_)])
                        ph.op("dve", lambda e, s_=s_: e.reciprocal(out=ers[:, 2 + s_:3 + s_], in_=ers[:, s_:s_ + 1]), reads=[("ers", s_)], writes=[("ers2", s_)])
                        ph.op("dve", lambda e, t=t, s_=s_: e.scalar_tensor_tensor(out=acc[:, t, :], in0=acc[:, t, :], scalar=ers[:, 2 + s_:3 + s_], in1=GG2,
                                                                              op0=ALU.mult, op1=ALU.mult),
                              reads=[("acc", t, 0), ("acc", t, 1), ("ers2", s_), ("Gg", 1)], writes=[("acc", t, 0), ("acc", t, 1)])
                        ph.op("pool", lambda e, t=t: e.tensor_tensor(out=acc[:, t, :], in0=acc[:, t, :], in1=XT, op=ALU.add),
                              reads=[("acc", t, 0), ("acc", t, 1), ("Gg", 0)], writes=[("acc", t, 0), ("acc", t, 1)])
                        ph.op("sp", lambda e, t=t, r0=r0: e.dma_start(out=out[r0:r0 + 128, :], in_=acc[:, t, :]),
                              reads=[("acc", t, 0), ("acc", t, 1)], writes=[("out", t)], dma=True, semkey=("st", "acc", t % 2))
                ph.emit()
        if stop_after == "F":
            _dbg_copy(nc, dbg, y2_d, rows=T)
            return nc

        if True:
            return nc
        with ExitStack() as ph_es:
            ph = Phase(nc, "G")
            def sbp(nm, shape, dt=F32):
                return ph_es.enter_context(nc.sbuf_tensor("gq_" + nm, list(shape), dt))
            gg = sbp("gg", [128, D])
            yt = [sbp(f"yt{i}", [128, D]) for i in range(2)]
            xt = [sbp(f"xt{i}", [128, D]) for i in range(2)]
            junk = sbp("junk", [128, D], BF16)
            ss = sbp("ss", [128, 4])
            rs = sbp("rs", [128, 4])
            ph.op("sp", lambda e: e.dma_start(out=gg[:], in_=modbc_d[:, 5 * D:6 * D]), writes=["gg"], dma=True)
            for t in range(T // 128):
                s = t % 2
                ph.op("sp", lambda e, s=s, t=t: e.dma_start(out=yt[s][:], in_=y2_d[t * 128:(t + 1) * 128, :]), writes=[("yt", s)], dma=True)
                ph.op("sp", lambda e, s=s, t=t: e.dma_start(out=xt[s][:], in_=x1_d[t * 128:(t + 1) * 128, :]), writes=[("xt", s)], dma=True)
                ph.op("act", lambda e, s=s: e.activation(out=junk[:], in_=yt[s][:], func=AF.Square, accum_out=ss[:, s:s + 1]),
                      reads=[("yt", s)], writes=["junk", ("ss", s)])
                ph.op("act", lambda e, s=s: e.activation(out=rs[:, s:s + 1], in_=ss[:, s:s + 1], func=AF.Sqrt, scale=1.0 / D, bias=EPS),
                      reads=[("ss", s)], writes=[("rs", s)])
                ph.op("dve", lambda e, s=s: e.reciprocal(out=rs[:, 2 + s:3 + s], in_=rs[:, s:s + 1]), reads=[("rs", s)], writes=[("rs2", s)])
                ph.op("dve", lambda e, s=s: e.scalar_tensor_tensor(out=yt[s][:], in0=yt[s][:], scalar=rs[:, 2 + s:3 + s], in1=gg[:],
                                                                  op0=ALU.mult, op1=ALU.mult),
                      reads=[("yt", s), ("rs2", s), "gg"], writes=[("yt", s)])
                ph.op("pool", lambda e, s=s: e.tensor_tensor(out=yt[s][:], in0=yt[s][:], in1=xt[s][:], op=ALU.add),
                      reads=[("yt", s), ("xt", s)], writes=[("yt", s)])
                ph.op("sp", lambda e, s=s, t=t: e.dma_start(out=out[t * 128:(t + 1) * 128, :], in_=yt[s][:]),
                      reads=[("yt", s)], writes=[("out", t)], dma=True, semkey=("st", "yt", s))
            ph.emit()
    return nc


def _dbg_copy3(nc, dbg, slots_d):
    with ExitStack() as es:
        buf = es.enter_context(nc.sbuf_tensor("dbgbuf3", [128, T], F32))
        s1 = es.enter_context(nc.semaphore("dbg3_s1"))
        with nc.Block() as block:
            @block.sync
            def _(e):
                n = 0
                for q in range(3):
                    e.dma_start(out=buf[:], in_=slots_d[q]).then_inc(s1, 16)
                    n += 16
                    e.wait_ge(s1, n)
                    e.dma_start(out=dbg[q * 128:(q + 1) * 128, :], in_=buf[:]).then_inc(s1, 16)
                    n += 16
                    e.wait_ge(s1, n)


def _dbg_copy(nc, dbg, src, rows):
    with ExitStack() as es:
        buf = es.enter_context(nc.sbuf_tensor("dbgbuf", [128, D], F32))
        s1 = es.enter_context(nc.semaphore("dbg_s1"))
        with nc.Block() as block:
            @block.sync
            def _(e):
                n = 0
                for t in range(rows // 128):
                    e.dma_start(out=buf[:], in_=src[t * 128:(t + 1) * 128, :]).then_inc(s1, 16)
                    n += 16
                    e.wait_ge(s1, n)
                    e.dma_start(out=dbg[t * 128:(t + 1) * 128, :], in_=buf[:]).then_inc(s1, 16)
                    n += 16
                    e.wait_ge(s1, n)


def _make_cols(inp, b, j):
    cols = np.zeros((128, NCOL), np.float32)

    def put(c0, vec):
        v = np.asarray(vec, np.float32).reshape(-1, 128)
        cols[:, c0:c0 + v.shape[0]] = v.T
    put(C_C, inp["c"][b])
    for tap in range(4):
        put(C_LW + tap * 8, inp["lru_conv_w"][0, tap])
    put(C_LB, inp["lru_conv_b"][0])
    put(C_BA, inp["lru_ba"][0])
    put(C_BX, inp["lru_bx"][0])
    put(C_LAM, inp["lru_lambda"][0])
    for tap in range(31):
        put(C_CW + tap * 8, inp["conf_dw_w"][0, tap])
    put(C_CB, inp["conf_dw_b"][0])
    put(C_LG, inp["conf_ln_g"][0])
    put(C_LNB, inp["conf_ln_b"][0])
    nvalid_blocks = (T * j) // TB
    for blk in range(NPRE // TB):
        cols[:, C_FL + blk] = 1.0 if blk >= (NPRE // TB - nvalid_blocks) else 0.0
    return cols


def make_in_maps(inp):
    x = np.ascontiguousarray(inp["x"], dtype=np.float32)
    shared = {
        "ident": np.eye(128, dtype=np.float32),
        "iota": np.tile(np.arange(128, dtype=np.float32)[None, :], (128, 1)),
        "w_mod": np.ascontiguousarray(inp["w_mod"][0]),
        "b_mod": np.ascontiguousarray(inp["b_mod"][0]),
        "g_pre_mix": np.ascontiguousarray(inp["g_pre_mix"][0]),
        "g_post_mix": np.ascontiguousarray(inp["g_post_mix"][0]),
        "g_pre_ffn": np.ascontiguousarray(inp["g_pre_ffn"][0]),
        "g_post_ffn": np.ascontiguousarray(inp["g_post_ffn"][0]),
        "w_in": np.ascontiguousarray(inp["w_in"][0]),
        "lru_wa": np.ascontiguousarray(inp["lru_wa"][0]),
        "lru_wx": np.ascontiguousarray(inp["lru_wx"][0]),
        "w_out": np.ascontiguousarray(inp["w_out"][0]),
        "peer_wq": np.ascontiguousarray(inp["peer_wq"][0]),
        "peer_sk": np.ascontiguousarray(inp["peer_subkeys"][0].reshape(16, 128, 128)),
        "peer_u": np.ascontiguousarray(inp["peer_u"][0]),
        "peer_v": np.ascontiguousarray(inp["peer_v"][0]),
    }
    maps = []
    for c in range(8):
        b, j = divmod(c, 4)
        m = dict(shared)
        m["x_main"] = np.ascontiguousarray(x[b, T * j:T * (j + 1)])
        pre = np.zeros((NPRE, D), np.float32)
        nv = T * j
        if nv:
            pre[NPRE - nv:] = x[b, 0:nv]
        m["x_pre"] = pre
        m["cols"] = _make_cols(inp, b, j)
        maps.append(m)
    return maps


def kernel(**inputs):
    nc = build_program()
    maps = make_in_maps(inputs)
    res = run_bass_kernel_spmd(nc, maps, core_ids=list(range(8)))
    out = np.empty((2, 8192, D), np.float32)
    for c in range(8):
        b, j = divmod(c, 4)
        out[b, T * j:T * (j + 1)] = res.results[c]["out"]
    return out
```
